# Optimizing a Trainium2 kernel written in Bass

```python
import math, functools
import jax, jax.numpy as jnp
from jax import lax
import numpy as np

D_MODEL = 1024
BATCH = 8
SEQ = 2048
DEPTH = 1
DEC_BATCH = 32
DEC_SEQ = 1
PAST_LEN = 8192
PAGE_SIZE = 128

ATT_HEADS = 8
HEAD_DIM = 64
ATT_WIDTH = ATT_HEADS * HEAD_DIM
Q_BLOCK = 128
SSM_GROUP = 16
SSM_WIDTH = 512
SSM_GROUPS = SSM_WIDTH // SSM_GROUP
SSM_STATE = 64
SSM_CHUNK = 128
DT_MIN = 0.001
DT_MAX = 0.1
PEER_HEADS = 8
N_KEYS = 128
N_EXPERTS = N_KEYS * N_KEYS
PEER_TOPK = 16
PEER_KEY_DIM = 256
PEER_HALF = PEER_KEY_DIM // 2
PEER_BLOCK = 128
PLE_DIM = 256
RMS_EPS = 1e-6

IN_SPLITS = (ATT_WIDTH, 2 * ATT_WIDTH, 3 * ATT_WIDTH, 3 * ATT_WIDTH + ATT_HEADS,
             3 * ATT_WIDTH + ATT_HEADS + SSM_WIDTH, 3 * ATT_WIDTH + ATT_HEADS + SSM_WIDTH + D_MODEL)
IN_WIDTH = 3 * ATT_WIDTH + ATT_HEADS + SSM_WIDTH + 2 * D_MODEL

kernel_name = 'fox_s5_peer_hybrid_step'


def rms_norm(x, g):
    x32 = x.astype(jnp.float32)
    y = x32 * lax.rsqrt(jnp.mean(x32 * x32, axis=-1, keepdims=True) + RMS_EPS)
    return (y * g.astype(jnp.float32)).astype(x.dtype)


def ssm_discretise(lam_re, lam_im, log_dt, b_re, b_im):
    f32 = jnp.float32
    dt = jnp.exp(log_dt.astype(f32))[:, None]
    lr, li = lam_re.astype(f32), lam_im.astype(f32)
    mag = jnp.exp(lr * dt)
    ang = li * dt
    a_re, a_im = mag * jnp.cos(ang), mag * jnp.sin(ang)
    den = lr * lr + li * li
    n_re, n_im = a_re - 1.0, a_im
    c_re = (n_re * lr + n_im * li) / den
    c_im = (n_im * lr - n_re * li) / den
    br, bi = b_re.astype(f32), b_im.astype(f32)
    bb_re = c_re[..., None] * br - c_im[..., None] * bi
    bb_im = c_re[..., None] * bi + c_im[..., None] * br
    return a_re, a_im, bb_re, bb_im


def _cplx_combine(e1, e2):
    a1r, a1i, b1r, b1i = e1
    a2r, a2i, b2r, b2i = e2
    return (a2r * a1r - a2i * a1i, a2r * a1i + a2i * a1r,
            a2r * b1r - a2i * b1i + b2r, a2r * b1i + a2i * b1r + b2i)


def ssm_scan(u, h0_re, h0_im, a_re, a_im, bb_re, bb_im, c_re, c_im):
    bsz, L = u.shape[0], u.shape[1]
    lc = SSM_CHUNK if L % SSM_CHUNK == 0 else L
    nc = L // lc
    u_chunks = u.reshape(bsz, nc, lc, SSM_GROUPS, SSM_GROUP).swapaxes(0, 1)
    cr, ci = c_re.astype(jnp.float32), c_im.astype(jnp.float32)

    def step(carry, uc):
        hr, hi = carry
        br = jnp.einsum('blgc,gnc->blgn', uc, bb_re)
        bi = jnp.einsum('blgc,gnc->blgn', uc, bb_im)
        ar = jnp.broadcast_to(a_re, br.shape)
        ai = jnp.broadcast_to(a_im, br.shape)
        pr, pi, sr, si = lax.associative_scan(_cplx_combine, (ar, ai, br, bi), axis=1)
        xr = pr * hr[:, None] - pi * hi[:, None] + sr
        xi = pr * hi[:, None] + pi * hr[:, None] + si
        y = jnp.einsum('blgn,gcn->blgc', xr, cr) - jnp.einsum('blgn,gcn->blgc', xi, ci)
        return (xr[:, -1], xi[:, -1]), y

    (hr, hi), ys = lax.scan(step, (h0_re, h0_im), u_chunks)
    y = ys.swapaxes(0, 1).reshape(bsz, L, SSM_GROUPS, SSM_GROUP)
    return y, hr, hi


def fox_prompt(q, k, v, logf):
    bsz, L = q.shape[0], q.shape[1]
    scale = HEAD_DIM ** -0.5
    c = jnp.cumsum(logf, axis=1)
    c_t = c.transpose(0, 2, 1)
    nb = L // Q_BLOCK
    qb = q.reshape(bsz, nb, Q_BLOCK, ATT_HEADS, HEAD_DIM).swapaxes(0, 1)
    cb = c.reshape(bsz, nb, Q_BLOCK, ATT_HEADS).swapaxes(0, 1)
    kpos = jnp.arange(L)

    def block(args):
        qi, ci, bi = args
        s = jnp.einsum('bqhd,bkhd->bhqk', qi, k).astype(jnp.float32) * scale
        s = s + ci.transpose(0, 2, 1)[..., None] - c_t[:, :, None, :]
        qpos = bi * Q_BLOCK + jnp.arange(Q_BLOCK)
        s = jnp.where(kpos[None, :] <= qpos[:, None], s, -jnp.inf)
        p = jax.nn.softmax(s, axis=-1).astype(v.dtype)
        return jnp.einsum('bhqk,bkhd->bqhd', p, v)

    o = lax.map(block, (qb, cb, jnp.arange(nb)))
    return o.swapaxes(0, 1).reshape(bsz, L, ATT_WIDTH)


def fox_sample(q, k, v, logf, k_past, v_past, logf_past):
    bsz, T = q.shape[0], q.shape[1]
    P = k_past.shape[1]
    scale = HEAD_DIM ** -0.5
    k_all = jnp.concatenate([k_past.astype(k.dtype), k], axis=1)
    v_all = jnp.concatenate([v_past.astype(v.dtype), v], axis=1)
    c = jnp.cumsum(jnp.concatenate([logf_past.astype(jnp.float32), logf], axis=1), axis=1)
    c_t = c.transpose(0, 2, 1)
    s = jnp.einsum('bqhd,bkhd->bhqk', q, k_all).astype(jnp.float32) * scale
    s = s + c_t[:, :, P:, None] - c_t[:, :, None, :]
    qpos = P + jnp.arange(T)
    kpos = jnp.arange(P + T)
    s = jnp.where(kpos[None, :] <= qpos[:, None], s, -jnp.inf)
    p = jax.nn.softmax(s, axis=-1).astype(v.dtype)
    return jnp.einsum('bhqk,bkhd->bqhd', p, v_all).reshape(bsz, T, ATT_WIDTH)


def peer(x, w_q, keys, u_tab, v_tab):
    T = x.shape[0]
    blk = min(PEER_BLOCK, T)
    Tp = -(-T // blk) * blk
    xp = jnp.pad(x, ((0, Tp - T), (0, 0)))

    def block(xb):
        q = (xb @ w_q).reshape(blk, PEER_HEADS, 2, PEER_HALF)
        s = jnp.einsum('thpd,hpkd->thpk', q, keys).astype(jnp.float32)
        s1, i1 = lax.top_k(s[:, :, 0], PEER_TOPK)
        s2, i2 = lax.top_k(s[:, :, 1], PEER_TOPK)
        cand = (s1[..., :, None] + s2[..., None, :]).reshape(blk, PEER_HEADS, PEER_TOPK * PEER_TOPK)
        cidx = (i1[..., :, None] * N_KEYS + i2[..., None, :]).reshape(blk, PEER_HEADS, PEER_TOPK * PEER_TOPK)
        top_s, pos = lax.top_k(cand, PEER_TOPK)
        idx = jnp.take_along_axis(cidx, pos, axis=-1)
        g = jax.nn.softmax(top_s, axis=-1)
        act = jax.nn.gelu(jnp.einsum('td,thkd->thk', xb, u_tab[idx]).astype(jnp.float32))
        w = (g * act).astype(xb.dtype)
        return jnp.einsum('thk,thkd->td', w, v_tab[idx])

    out = lax.map(block, xp.reshape(Tp // blk, blk, D_MODEL))
    return out.reshape(Tp, D_MODEL)[:T]


def trunk_layer(h, p, lw, attend, h0_re, h0_im):
    bsz, L = h.shape[0], h.shape[1]
    f32 = jnp.float32
    hn = rms_norm(h, lw['g_mix'])
    z = hn @ lw['w_in']
    q, k, v, f, u, ga, gs = jnp.split(z, IN_SPLITS, axis=-1)
    q = q.reshape(bsz, L, ATT_HEADS, HEAD_DIM)
    k = k.reshape(bsz, L, ATT_HEADS, HEAD_DIM)
    v = v.reshape(bsz, L, ATT_HEADS, HEAD_DIM)
    logf = jax.nn.log_sigmoid((f + lw['b_forget']).astype(f32))
    attn = attend(q, k, v, logf)
    u32 = u.astype(f32)
    y, hr, hi = ssm_scan(u32.reshape(bsz, L, SSM_GROUPS, SSM_GROUP),
                         h0_re.astype(f32), h0_im.astype(f32), *lw['ssm'])
    y = y.reshape(bsz, L, SSM_WIDTH) + lw['ssm_d'].astype(f32) * u32
    zs = jax.nn.gelu(y).astype(h.dtype)
    ssm_branch = (zs @ lw['w_glu_a']) * jax.nn.sigmoid(zs @ lw['w_glu_b'])
    attn_branch = attn @ lw['w_attn_out']
    merged = jax.nn.sigmoid(ga) * attn_branch + jax.nn.sigmoid(gs) * ssm_branch
    h = h + merged @ lw['w_out']
    hn = rms_norm(h, lw['g_ffn'])
    h = h + peer(hn.reshape(-1, D_MODEL), lw['peer_w_q'], lw['peer_keys'],
                 lw['peer_u'], lw['peer_v']).reshape(h.shape)
    gate = jax.nn.sigmoid(rms_norm(h, lw['g_ple']) @ lw['w_ple_gate'])
    h = h + (p @ lw['w_ple']) * gate
    return h, (k, v, logf.astype(h.dtype), hr.astype(h.dtype), hi.astype(h.dtype))


def setup_inputs(seed: int = 0) -> dict:
    key = jax.random.key(seed)
    ks = iter(jax.random.split(key, 48))
    f32 = jnp.float32

    def nrm(shape, scale):
        return jax.random.normal(next(ks), shape, f32) * scale

    n_pages = PAST_LEN // PAGE_SIZE
    n_used = DEC_BATCH * n_pages
    n_phys = n_used + max(1, n_used // 4)
    x_prompt = nrm((BATCH, SEQ, D_MODEL), 1.0)
    x_sample = nrm((DEC_BATCH, DEC_SEQ, D_MODEL), 1.0)
    cache_k = nrm((n_phys, DEPTH, PAGE_SIZE, ATT_HEADS, HEAD_DIM), 1.0)
    cache_v = nrm((n_phys, DEPTH, PAGE_SIZE, ATT_HEADS, HEAD_DIM), 1.0)
    cache_logf = jax.nn.log_sigmoid(nrm((n_phys, DEPTH, PAGE_SIZE, ATT_HEADS), 1.0) + 4.0)
    state_ssm_re = nrm((DEC_BATCH, DEPTH, SSM_GROUPS, SSM_STATE), 0.1)
    state_ssm_im = nrm((DEC_BATCH, DEPTH, SSM_GROUPS, SSM_STATE), 0.1)
    page_table = jax.random.permutation(next(ks), n_phys)[:n_used].reshape(DEC_BATCH, n_pages).astype(jnp.int32)
    p_prompt = nrm((DEPTH, BATCH, SEQ, PLE_DIM), 1.0)
    p_sample = nrm((DEPTH, DEC_BATCH, DEC_SEQ, PLE_DIM), 1.0)
    w_in = nrm((DEPTH, D_MODEL, IN_WIDTH), D_MODEL ** -0.5)
    b_forget = jnp.broadcast_to(jnp.linspace(1.0, 6.0, ATT_HEADS, dtype=f32), (DEPTH, ATT_HEADS)) + nrm((DEPTH, ATT_HEADS), 0.1)
    w_attn_out = nrm((DEPTH, ATT_WIDTH, D_MODEL), ATT_WIDTH ** -0.5)
    ssm_lam_re = -0.5 + nrm((DEPTH, SSM_GROUPS, SSM_STATE), 0.01)
    ssm_lam_im = jnp.pi * jnp.arange(SSM_STATE, dtype=f32) + nrm((DEPTH, SSM_GROUPS, SSM_STATE), 0.01)
    ssm_log_dt = jax.random.uniform(next(ks), (DEPTH, SSM_GROUPS), f32, math.log(DT_MIN), math.log(DT_MAX))
    ssm_b_re = nrm((DEPTH, SSM_GROUPS, SSM_STATE, SSM_GROUP), (2 * SSM_GROUP) ** -0.5)
    ssm_b_im = nrm((DEPTH, SSM_GROUPS, SSM_STATE, SSM_GROUP), (2 * SSM_GROUP) ** -0.5)
    ssm_c_re = nrm((DEPTH, SSM_GROUPS, SSM_GROUP, SSM_STATE), SSM_STATE ** -0.5)
    ssm_c_im = nrm((DEPTH, SSM_GROUPS, SSM_GROUP, SSM_STATE), SSM_STATE ** -0.5)
    ssm_d = nrm((DEPTH, SSM_WIDTH), 1.0)
    w_glu_a = nrm((DEPTH, SSM_WIDTH, D_MODEL), SSM_WIDTH ** -0.5)
    w_glu_b = nrm((DEPTH, SSM_WIDTH, D_MODEL), SSM_WIDTH ** -0.5)
    w_out = nrm((DEPTH, D_MODEL, D_MODEL), D_MODEL ** -0.5)
    peer_w_q = nrm((DEPTH, D_MODEL, PEER_HEADS * PEER_KEY_DIM), D_MODEL ** -0.5)
    peer_keys = nrm((DEPTH, PEER_HEADS, 2, N_KEYS, PEER_HALF), PEER_HALF ** -0.5)
    peer_u = nrm((DEPTH, N_EXPERTS, D_MODEL), D_MODEL ** -0.5)
    peer_v = nrm((DEPTH, N_EXPERTS, D_MODEL), PEER_HEADS ** -0.5)
    w_ple = nrm((DEPTH, PLE_DIM, D_MODEL), PLE_DIM ** -0.5)
    w_ple_gate = nrm((DEPTH, D_MODEL, D_MODEL), D_MODEL ** -0.5)
    g_mix = 1.0 + nrm((DEPTH, D_MODEL), 0.05)
    g_ffn = 1.0 + nrm((DEPTH, D_MODEL), 0.05)
    g_ple = 1.0 + nrm((DEPTH, D_MODEL), 0.05)
    g_final = 1.0 + nrm((D_MODEL,), 0.05)
    return {'x_prompt': x_prompt, 'x_sample': x_sample, 'cache_k': cache_k, 'cache_v': cache_v,
            'cache_logf': cache_logf, 'state_ssm_re': state_ssm_re, 'state_ssm_im': state_ssm_im,
            'page_table': page_table, 'p_prompt': p_prompt, 'p_sample': p_sample,
            'w_in': w_in, 'b_forget': b_forget, 'w_attn_out': w_attn_out,
            'ssm_lam_re': ssm_lam_re, 'ssm_lam_im': ssm_lam_im, 'ssm_log_dt': ssm_log_dt,
            'ssm_b_re': ssm_b_re, 'ssm_b_im': ssm_b_im, 'ssm_c_re': ssm_c_re, 'ssm_c_im': ssm_c_im,
            'ssm_d': ssm_d, 'w_glu_a': w_glu_a, 'w_glu_b': w_glu_b, 'w_out': w_out,
            'peer_w_q': peer_w_q, 'peer_keys': peer_keys, 'peer_u': peer_u, 'peer_v': peer_v,
            'w_ple': w_ple, 'w_ple_gate': w_ple_gate,
            'g_mix': g_mix, 'g_ffn': g_ffn, 'g_ple': g_ple, 'g_final': g_final}


def reference(x_prompt, x_sample, cache_k, cache_v, cache_logf, state_ssm_re, state_ssm_im, page_table,
              p_prompt, p_sample, w_in, b_forget, w_attn_out, ssm_lam_re, ssm_lam_im, ssm_log_dt,
              ssm_b_re, ssm_b_im, ssm_c_re, ssm_c_im, ssm_d, w_glu_a, w_glu_b, w_out,
              peer_w_q, peer_keys, peer_u, peer_v, w_ple, w_ple_gate,
              g_mix, g_ffn, g_ple, g_final):
    bp, bs = x_prompt.shape[0], x_sample.shape[0]
    hp, hs = x_prompt, x_sample
    kp, vp, lp, rp, ip = [], [], [], [], []
    ksl, vsl, lsl, rsl, isl = [], [], [], [], []
    for i in range(DEPTH):
        lw = {'g_mix': g_mix[i], 'w_in': w_in[i], 'b_forget': b_forget[i],
              'ssm': ssm_discretise(ssm_lam_re[i], ssm_lam_im[i], ssm_log_dt[i], ssm_b_re[i], ssm_b_im[i])
                     + (ssm_c_re[i], ssm_c_im[i]),
              'ssm_d': ssm_d[i], 'w_glu_a': w_glu_a[i], 'w_glu_b': w_glu_b[i],
              'w_attn_out': w_attn_out[i], 'w_out': w_out[i], 'g_ffn': g_ffn[i],
              'peer_w_q': peer_w_q[i], 'peer_keys': peer_keys[i], 'peer_u': peer_u[i], 'peer_v': peer_v[i],
              'g_ple': g_ple[i], 'w_ple_gate': w_ple_gate[i], 'w_ple': w_ple[i]}
        h0 = jnp.zeros((bp, SSM_GROUPS, SSM_STATE), jnp.float32)
        hp, (k_, v_, l_, r_, m_) = trunk_layer(hp, p_prompt[i], lw, fox_prompt, h0, h0)
        kp.append(k_); vp.append(v_); lp.append(l_); rp.append(r_); ip.append(m_)
        k_past = cache_k[page_table, i].reshape(bs, -1, ATT_HEADS, HEAD_DIM)
        v_past = cache_v[page_table, i].reshape(bs, -1, ATT_HEADS, HEAD_DIM)
        l_past = cache_logf[page_table, i].reshape(bs, -1, ATT_HEADS)
        attend = functools.partial(fox_sample, k_past=k_past, v_past=v_past, logf_past=l_past)
        hs, (k_, v_, l_, r_, m_) = trunk_layer(hs, p_sample[i], lw, attend,
                                               state_ssm_re[:, i], state_ssm_im[:, i])
        ksl.append(k_); vsl.append(v_); lsl.append(l_); rsl.append(r_); isl.append(m_)
    y_prompt = rms_norm(hp, g_final)
    y_sample = rms_norm(hs, g_final)
    k_prompt = jnp.stack(kp, axis=1)
    v_prompt = jnp.stack(vp, axis=1)
    logf_prompt = jnp.stack(lp, axis=1)
    ssm_re_prompt = jnp.stack(rp, axis=1)
    ssm_im_prompt = jnp.stack(ip, axis=1)
    k_sample = jnp.stack(ksl, axis=1)
    v_sample = jnp.stack(vsl, axis=1)
    logf_sample = jnp.stack(lsl, axis=1)
    ssm_re_sample = jnp.stack(rsl, axis=1)
    ssm_im_sample = jnp.stack(isl, axis=1)
    return (y_prompt, y_sample, k_prompt, v_prompt, logf_prompt, ssm_re_prompt, ssm_im_prompt,
            k_sample, v_sample, logf_sample, ssm_re_sample, ssm_im_sample)
```

```python
import contextlib
import numpy as np
import concourse.bass as bass
import concourse.mybir as mybir
from concourse.bass_utils import run_bass_kernel_spmd

F32 = mybir.dt.float32
BF16 = mybir.dt.bfloat16
I32 = mybir.dt.int32
U32 = mybir.dt.uint32
ALU = mybir.AluOpType
ACT = mybir.ActivationFunctionType
AX = mybir.AxisListType

N_CORES = 8
NEEDED = ["x_prompt", "x_sample", "w_in", "b_forget", "g_mix", "ssm_lam_re", "ssm_lam_im", "ssm_log_dt", "ssm_b_re", "ssm_b_im", "ssm_c_re", "ssm_c_im", "ssm_d", "state_ssm_re", "state_ssm_im", "w_attn_out", "w_glu_a", "w_glu_b", "w_out", "peer_w_q", "peer_keys", "peer_u", "peer_v", "g_ffn", "g_ple", "g_final", "w_ple", "w_ple_gate", "p_prompt", "p_sample", "cache_k", "cache_v", "cache_logf", "page_table"]
D = 1024
SEQ = 2048
NT = 16
NTT = 17
TOK = NTT * 128
H = 8
HD = 64
AW = 512
IN_W = 4104
NS = 4
NPAGES = 64
NPHYS = 2560
EPS = 1e-6

COMPUTE = ("tensor", "vector", "scalar", "gpsimd")
SEM_SPAN = 30000
N_DMA_SEMS = 16


class Sched:
    def __init__(self, nc, stack):
        self.nc = nc
        self.stack = stack
        self.streams = {e: [] for e in ("tensor", "vector", "scalar", "gpsimd", "sync")}
        self.cnt = {e: 0 for e in self.streams}
        self.esems = {e: [] for e in COMPUTE}
        self.dq = ("sync", "gpsimd")
        self.dsems = [stack.enter_context(nc.semaphore(f"dq{i}")) for i in range(2 * N_DMA_SEMS)]
        self.dcnt = [0] * (2 * N_DMA_SEMS)
        self.dnext = {"sync": 0, "gpsimd": 0}
        self.waited = {}
        self.last_w = {}
        self.readers = {}
        self.n_inst = 0
        self.out_tokens = []

    def _sem_of(self, tok):
        if tok[0] == "e":
            _, eng, n = tok
            idx = (n - 1) // SEM_SPAN
            while len(self.esems[eng]) <= idx:
                self.esems[eng].append(self.stack.enter_context(
                    self.nc.semaphore(f"s_{eng}_{len(self.esems[eng])}")))
            return ("e", eng, idx), self.esems[eng][idx], n - idx * SEM_SPAN
        _, si, val = tok
        return ("d", si), self.dsems[si], val

    def _need_wait(self, eng, tok, same_ok):
        if tok is None:
            return None
        if tok[0] == "e" and tok[1] == eng and same_ok:
            return None
        key, sem, val = self._sem_of(tok)
        if self.waited.get((eng, key), 0) >= val:
            return None
        self.waited[(eng, key)] = val
        return (sem, val)

    def _deps(self, eng, reads, writes):
        waits = []
        pe = eng == "tensor"
        for k in reads:
            w = self._need_wait(eng, self.last_w.get(k), same_ok=pe)
            if w:
                waits.append(w)
        for k in writes:
            w = self._need_wait(eng, self.last_w.get(k), same_ok=pe)
            if w:
                waits.append(w)
            for r in self.readers.get(k, []):
                w = self._need_wait(eng, r, same_ok=pe)
                if w:
                    waits.append(w)
        return waits

    def _commit(self, tok, reads, writes):
        for k in reads:
            self.readers.setdefault(k, []).append(tok)
        for k in writes:
            self.last_w[k] = tok
            self.readers[k] = []

    def op(self, eng, fn, reads=(), writes=()):
        waits = self._deps(eng, reads, writes)
        self.cnt[eng] += 1
        tok = ("e", eng, self.cnt[eng])
        _, sem, _ = self._sem_of(tok)
        self.streams[eng].append((waits, fn, sem, 1))
        self._commit(tok, reads, writes)
        self.n_inst += 1
        return tok

    def dma(self, out, in_, reads=(), writes=(), queue="sync", gather=None, **kw):
        eng = queue
        waits = self._deps(eng, reads, writes)
        si = self.dq.index(eng) * N_DMA_SEMS + self.dnext[eng]
        self.dnext[eng] = (self.dnext[eng] + 1) % N_DMA_SEMS
        if self.dcnt[si] > 0:
            w = self._need_wait(eng, ("d", si, self.dcnt[si]), same_ok=False)
            if w:
                waits.append(w)
        self.dcnt[si] += 16
        tok = ("d", si, self.dcnt[si])
        if gather is None:
            fn = lambda e, o=out, i=in_, kw=kw: e.dma_start(out=o, in_=i, **kw)
        else:
            fn = lambda e, o=out, i=in_, g=gather, kw=kw: e.indirect_dma_start(
                out=o, out_offset=None, in_=i, in_offset=g, **kw)
        self.streams[eng].append((waits, fn, self.dsems[si], 16))
        self._commit(tok, reads, writes)
        self.n_inst += 1
        return tok

    def barrier(self):
        toks = [("e", o, self.cnt[o]) for o in COMPUTE if self.cnt[o] > 0]
        toks += [("d", si, self.dcnt[si]) for si in range(2 * N_DMA_SEMS) if self.dcnt[si] > 0]
        for eng in self.streams:
            waits = []
            for t in toks:
                w = self._need_wait(eng, t, same_ok=True)
                if w:
                    waits.append(w)
            if waits:
                self.streams[eng].append((waits, None, None, 0))
        self.last_w = {}
        self.readers = {}

    def mark_output(self, tok):
        self.out_tokens.append(tok)

    def emit(self, final=False):
        nc = self.nc
        fin = []
        if final:
            for tok in self.out_tokens:
                w = self._need_wait("sync", tok, same_ok=False)
                if w:
                    fin.append(w)
        streams = self.streams
        self.streams = {e: [] for e in streams}

        def run(e, name):
            for waits, fn, sem, inc in streams[name]:
                for (s, v) in waits:
                    e.wait_ge(s, v)
                if fn is not None:
                    fn(e).then_inc(sem, inc)
            if name == "sync":
                for (s, v) in fin:
                    e.wait_ge(s, v)

        with nc.Block() as block:
            @block.sync
            def _(e):
                run(e, "sync")

            @block.tensor
            def _(e):
                run(e, "tensor")

            @block.vector
            def _(e):
                run(e, "vector")

            @block.scalar
            def _(e):
                run(e, "scalar")

            @block.gpsimd
            def _(e):
                run(e, "gpsimd")


class K:
    def __init__(self, stage=99):
        self.stage = stage
        self.nc = bass.Bass("TRN2", target_bir_lowering=False)
        self.stack = contextlib.ExitStack()
        self.S = Sched(self.nc, self.stack)
        self.io = {}

    def din(self, name, shape, dt=F32):
        self.io[name] = self.nc.dram_tensor(name, list(shape), dt, kind="ExternalInput").ap()
        return self.io[name]

    def dout(self, name, shape, dt=F32):
        self.io[name] = self.nc.dram_tensor(name, list(shape), dt, kind="ExternalOutput").ap()
        return self.io[name]

    def sb(self, name, shape, dt=F32, stack=None):
        return (stack or self.stack).enter_context(self.nc.sbuf_tensor(name, list(shape), dt))

    def ps(self, name, shape, dt=F32, stack=None):
        return (stack or self.stack).enter_context(self.nc.psum_tensor(name, list(shape), dt))


def build(stage=99, dbg=None):
    k = K(stage)
    nc, S = k.nc, k.S
    op, dma = S.op, S.dma
    sb, ps = k.sb, k.ps

    def E(eng, method, *args, r=(), w=(), **kw):
        return op(eng, lambda e: getattr(e, method)(*args, **kw), r, w)

    def mm(out, lhsT, rhs, start=True, stop=True, r=(), w=()):
        return op("tensor", lambda e: e.matmul(out, lhsT, rhs, start=start, stop=stop), r, w)

    def act(out, in_, func, r=(), w=(), **kw):
        return op("scalar", lambda e: e.activation(out, in_, func, **kw), r, w)

    x_p = k.din("x_p", [SEQ, D])
    x_s = k.din("x_s", [NS, D])
    w_in = k.din("w_in", [D, IN_W])
    b_forget = k.din("b_forget", [1, H])
    g_mix = k.din("g_mix", [1, D])

    k_p = k.dout("k_p", [SEQ, AW])
    v_p = k.dout("v_p", [SEQ, AW])
    lf_p = k.dout("lf_p", [SEQ, H])
    k_s = k.dout("k_s", [NS, AW])
    v_s = k.dout("v_s", [NS, AW])
    lf_s = k.dout("lf_s", [NS, H])
    dbg_t = k.dout("dbg", list(dbg[1])) if dbg else None
    w_in_c = w_in.rearrange("(c p) n -> p c n", p=128)

    ident_bf = sb("ident_bf", [128, 128], BF16)
    ident_f = sb("ident_f", [128, 128], F32)
    iota_t = sb("iota_t", [128, 128], F32)
    tri = sb("tri", [128, 128], BF16)
    E("gpsimd", "iota", iota_t[:], pattern=[[1, 128]], base=0, channel_multiplier=-1,
      allow_small_or_imprecise_dtypes=True, w=["iota_t"])
    E("vector", "tensor_single_scalar", ident_f[:], iota_t[:], 0.0, ALU.is_equal, r=["iota_t"], w=["ident_f"])
    E("vector", "tensor_copy", ident_bf[:], ident_f[:], r=["ident_f"], w=["ident_bf"])
    E("vector", "tensor_single_scalar", tri[:], iota_t[:], 0.0, ALU.is_ge, r=["iota_t"], w=["tri"])

    pG = contextlib.ExitStack()
    hnT = sb("hnT", [128, 8, TOK], BF16, pG)
    lft = sb("lft", [128, NTT, 3 * H], F32, pG)
    zsT = sb("zsT", [128, 4, TOK], BF16, pG)

    with contextlib.ExitStack() as p1:
        gmix_b = sb("gmix_b", [128, D], F32, p1)
        dma(gmix_b[:], g_mix.partition_broadcast(128), writes=["gmix_b"])
        bf_b = sb("bf_b", [128, H], F32, p1)
        dma(bf_b[:], b_forget.partition_broadcast(128), writes=["bf_b"])
        w_kvf = sb("w_kvf", [128, 8, 1032], BF16, p1)
        for c in range(8):
            dma(w_kvf[:, c, :], w_in_c[:, c, 512:1544], writes=[f"w_kvf{c}"], queue="gpsimd")
        NB = 2
        xt = [sb(f"xt{i}", [128, D], F32, p1) for i in range(NB)]
        hn = [sb(f"hn{i}", [128, D], BF16, p1) for i in range(NB)]
        junk = [sb(f"junk{i}", [128, D], BF16, p1) for i in range(NB)]
        kv = [sb(f"kv{i}", [128, 1024], F32, p1) for i in range(NB)]
        stat = sb("stat", [128, NTT, 4], F32, p1)
        tp_ps = [ps(f"tp_ps{i}", [128, 1024], BF16, p1) for i in range(2)]
        kv_ps = ps("kv_ps", [128, 1536], F32, p1)

        E("vector", "memset", xt[0][:], 0.0, w=["xt0"])
        for t in range(NTT):
            b = t % NB
            X, HN, J, KV = f"xt{b}", f"hn{b}", f"junk{b}", f"kv{b}"
            TP, KP = f"tp_ps{t % 2}", "kv_ps"
            tt = (t - 1) if t > 0 else 16
            cols = slice(tt * 128, (tt + 1) * 128)
            if tt == 16:
                dma(xt[b][0:NS, :], x_s[:, :], writes=[X])
            else:
                dma(xt[b][:], x_p[cols, :], writes=[X])
            st = stat[:, tt, :]
            act(junk[b][:], xt[b][:], ACT.Square, accum_out=st[:, 0:1], r=[X], w=[J, f"st{tt}a"])
            E("vector", "tensor_scalar", st[:, 1:2], st[:, 0:1], 1.0 / D, EPS, ALU.mult, ALU.add,
              r=[f"st{tt}a"], w=[f"st{tt}b"])
            act(st[:, 2:3], st[:, 1:2], ACT.Sqrt, r=[f"st{tt}b"], w=[f"st{tt}c"])
            E("vector", "reciprocal", st[:, 3:4], st[:, 2:3], r=[f"st{tt}c"], w=[f"st{tt}d"])
            E("vector", "scalar_tensor_tensor", hn[b][:], xt[b][:], st[:, 3:4], gmix_b[:], ALU.mult, ALU.mult,
              r=[X, f"st{tt}d", "gmix_b"], w=[HN])
            tp = tp_ps[t % 2]
            for c in range(8):
                E("tensor", "transpose", tp[:, c * 128:(c + 1) * 128], hn[b][:, c * 128:(c + 1) * 128],
                  ident_bf[:], r=[HN, "ident_bf"], w=[TP])
            act(hnT[:, :, cols], tp[:].rearrange("p (c n) -> p c n", c=8), ACT.Copy, r=[TP], w=[f"hnT{tt}"])
            for (lo, hi) in ((0, 512), (512, 1024), (1024, 1032)):
                for c in range(8):
                    mm(kv_ps[:, lo:hi], hnT[:, c, cols], w_kvf[:, c, lo:hi], start=(c == 0), stop=(c == 7),
                       r=[f"hnT{tt}", f"w_kvf{c}"], w=[KP])
            E("vector", "tensor_copy", kv[b][:], kv_ps[:, 0:1024], r=[KP], w=[KV])
            l3 = lft[:, tt, :]
            E("vector", "tensor_tensor", l3[:, 0:H], kv_ps[:, 1024:1032], bf_b[:], ALU.add,
              r=[KP, "bf_b"], w=[f"lf{tt}a"])
            act(l3[:, H:2 * H], l3[:, 0:H], ACT.Exp, scale=-1.0, r=[f"lf{tt}a"], w=[f"lf{tt}b"])
            act(l3[:, 2 * H:3 * H], l3[:, H:2 * H], ACT.Ln, bias=1.0, r=[f"lf{tt}b"], w=[f"lf{tt}c"])
            E("vector", "tensor_scalar", l3[:, 0:H], l3[:, 2 * H:3 * H], -1.0, None, ALU.mult,
              r=[f"lf{tt}c"], w=[f"lf{tt}d"])
            if tt == 16:
                S.mark_output(dma(k_s[:, :], kv[b][0:NS, 0:512], reads=[KV]))
                S.mark_output(dma(v_s[:, :], kv[b][0:NS, 512:1024], reads=[KV]))
                S.mark_output(dma(lf_s[:, :], l3[0:NS, 0:H], reads=[f"lf{tt}d"]))
            else:
                S.mark_output(dma(k_p[cols, :], kv[b][:, 0:512], reads=[KV]))
                S.mark_output(dma(v_p[cols, :], kv[b][:, 512:1024], reads=[KV]))
                S.mark_output(dma(lf_p[cols, :], l3[:, 0:H], reads=[f"lf{tt}d"]))
        S.barrier()
        S.emit()

    if stage >= 3:
      with contextlib.ExitStack() as pS:
        lam_re_d = k.din("lam_re", [32, 64]); lam_im_d = k.din("lam_im", [32, 64])
        log_dt_d = k.din("log_dt", [1, 32])
        b_re_d = k.din("b_re", [32, 64, 16]); b_im_d = k.din("b_im", [32, 64, 16])
        c_re_d = k.din("c_re", [512, 64]); c_im_d = k.din("c_im", [512, 64])
        ssm_d_d = k.din("ssm_d", [512, 1])
        st_re_d = k.din("st_re", [128, 64]); st_im_d = k.din("st_im", [128, 64])
        hr_p = k.dout("hr_p", [32, 64]); hi_p = k.dout("hi_p", [32, 64])
        hr_s = k.dout("hr_s", [128, 64]); hi_s = k.dout("hi_s", [128, 64])

        WS = sb("WS", [128, 4, 16, 2, 2, 64], BF16, pS)
        CA = sb("CA", [64, 17, 2, 32, 16], BF16, pS)
        KM = sb("KM", [128, 4, 16, 128], BF16, pS)
        AL = sb("AL", [64, 2, 2, 2, 32], F32, pS)
        with contextlib.ExitStack() as pW:
            def T(name, shape, dt=F32):
                return sb(name, shape, dt, pW)
            TWO_PI = 2.0 * np.pi
            lnat = T("lnat", [32, 2, 64])
            dma(lnat[:, 0, :], lam_re_d[:, :], writes=["lnat"])
            dma(lnat[:, 1, :], lam_im_d[:, :], writes=["lnat"])
            ldt = T("ldt", [64, 32])
            dma(ldt[:], log_dt_d.partition_broadcast(64), writes=["ldt"])
            Bt = T("Bt", [64, 2, 32, 16])
            dma(Bt[:, 0, :, :], b_re_d.rearrange("g n c -> n g c"), writes=["Bt"])
            dma(Bt[:, 1, :, :], b_im_d.rearrange("g n c -> n g c"), writes=["Bt"])
            Cnat = T("Cnat", [128, 2, 4, 64])
            dma(Cnat[:, 0, :, :], c_re_d.rearrange("(q p) n -> p q n", p=128), writes=["Cnat"])
            dma(Cnat[:, 1, :, :], c_im_d.rearrange("(q p) n -> p q n", p=128), writes=["Cnat"])
            dcol = T("dcol", [128, 4])
            for q in range(4):
                dma(dcol[:, q:q + 1], ssm_d_d[q * 128:(q + 1) * 128, :], writes=["dcol"])
            wps = [ps(f"wps{i}", [128, 512], F32, pW) for i in range(2)]
            wpb = [ps(f"wpb{i}", [128, 1024], BF16, pW) for i in range(2)]
            lam = T("lam", [64, 2, 32])
            for ri in range(2):
                E("tensor", "transpose", wps[0][0:64, ri * 32:(ri + 1) * 32], lnat[:, ri, :], ident_f[0:32, 0:32],
                  r=["lnat", "ident_f"], w=["wps0"])
            E("vector", "tensor_copy", lam[:].rearrange("p a g -> p (a g)"), wps[0][0:64, 0:64], r=["wps0"], w=["lam"])
            CT = T("CT", [64, 2, 512])
            for ri in range(2):
                for q in range(4):
                    E("tensor", "transpose", wps[1][0:64, q * 128:(q + 1) * 128], Cnat[:, ri, q, :], ident_f[:],
                      r=["Cnat", "ident_f"], w=["wps1"])
                E("vector", "tensor_copy", CT[:, ri, :], wps[1][0:64, :], r=["wps1"], w=["CT"])
            sc = T("sc", [64, 16, 32])
            _n = [0]

            def V2(out, a, b, o, r, w):
                E("vector", "tensor_tensor", out, a, b, o, r=r, w=w)

            lr, li = lam[:, 0, :], lam[:, 1, :]
            dt_, lrdt, mag, ang, yv, kk, tmp, rr, sn, cs_, are, aim, den, nre, cre, cim = [sc[:, i, :] for i in range(16)]
            KS = ["sc"]
            act(dt_, ldt[:], ACT.Exp, r=["ldt"], w=KS)
            V2(lrdt, lr, dt_, ALU.mult, ["lam"] + KS, KS)
            act(mag, lrdt, ACT.Exp, r=KS, w=KS)
            V2(ang, li, dt_, ALU.mult, ["lam"] + KS, KS)
            E("vector", "tensor_scalar", yv, ang, 1.0 / TWO_PI, None, ALU.mult, r=KS, w=KS)
            E("vector", "tensor_scalar", kk, yv, 0.5, None, ALU.is_ge, r=KS, w=KS)
            for m in range(2, 8):
                E("vector", "tensor_scalar", tmp, yv, m - 0.5, None, ALU.is_ge, r=KS, w=KS)
                V2(kk, kk, tmp, ALU.add, KS, KS)
            for m in range(1, 3):
                E("vector", "tensor_scalar", tmp, yv, -(m - 0.5), None, ALU.is_le, r=KS, w=KS)
                V2(kk, kk, tmp, ALU.subtract, KS, KS)
            V2(rr, yv, kk, ALU.subtract, KS, KS)
            act(sn, rr, ACT.Sin, scale=TWO_PI, r=KS, w=KS)
            act(tmp, rr, ACT.Abs, r=KS, w=KS)
            hpi = T("hpi", [64, 1])
            E("vector", "memset", hpi[:], float(np.pi / 2), w=["hpi"])
            act(cs_, tmp, ACT.Sin, scale=-TWO_PI, bias=hpi[:, 0:1], r=KS + ["hpi"], w=KS)
            V2(are, mag, cs_, ALU.mult, KS, KS)
            V2(aim, mag, sn, ALU.mult, KS, KS)
            V2(den, lr, lr, ALU.mult, ["lam"] + KS, KS)
            V2(tmp, li, li, ALU.mult, ["lam"] + KS, KS)
            V2(den, den, tmp, ALU.add, KS, KS)
            E("vector", "reciprocal", den, den, r=KS, w=KS)
            E("vector", "tensor_scalar", nre, are, -1.0, None, ALU.add, r=KS, w=KS)
            V2(cre, nre, lr, ALU.mult, ["lam"] + KS, KS)
            V2(tmp, aim, li, ALU.mult, ["lam"] + KS, KS)
            V2(cre, cre, tmp, ALU.add, KS, KS)
            V2(cre, cre, den, ALU.mult, KS, KS)
            V2(cim, aim, lr, ALU.mult, ["lam"] + KS, KS)
            V2(tmp, nre, li, ALU.mult, ["lam"] + KS, KS)
            V2(cim, cim, tmp, ALU.subtract, KS, KS)
            V2(cim, cim, den, ALU.mult, KS, KS)

            PW = T("PW", [64, 17, 2, 32])
            tA = T("tA", [64, 8, 32]); tB = T("tB", [64, 8, 32])
            E("vector", "memset", PW[:, 0, 0, :], 1.0, w=["PW"])
            E("vector", "memset", PW[:, 0, 1, :], 0.0, w=["PW"])
            E("vector", "tensor_copy", PW[:, 1, 0, :], are, r=KS, w=["PW"])
            E("vector", "tensor_copy", PW[:, 1, 1, :], aim, r=KS, w=["PW"])
            for m in (1, 2, 4, 8):
                xr, xi = PW[:, 1:m + 1, 0, :], PW[:, 1:m + 1, 1, :]
                yr = PW[:, m:m + 1, 0, :].to_broadcast([64, m, 32]); yi = PW[:, m:m + 1, 1, :].to_broadcast([64, m, 32])
                orr, oi = PW[:, m + 1:2 * m + 1, 0, :], PW[:, m + 1:2 * m + 1, 1, :]
                P_ = ["PW", "tA", "tB"]
                V2(tA[:, 0:m, :], xr, yr, ALU.mult, P_, ["tA"])
                V2(tB[:, 0:m, :], xi, yi, ALU.mult, P_, ["tB"])
                V2(orr, tA[:, 0:m, :], tB[:, 0:m, :], ALU.subtract, P_, ["PW"])
                V2(tA[:, 0:m, :], xr, yi, ALU.mult, P_, ["tA"])
                V2(tB[:, 0:m, :], xi, yr, ALU.mult, P_, ["tB"])
                V2(oi, tA[:, 0:m, :], tB[:, 0:m, :], ALU.add, P_, ["PW"])
            for wi, e_ in ((0, 16), (1, 1)):
                E("vector", "tensor_copy", AL[:, wi, 0, 0, :], PW[:, e_, 0, :], r=["PW"], w=["AL"])
                E("vector", "tensor_copy", AL[:, wi, 0, 1, :], PW[:, e_, 0, :], r=["PW"], w=["AL"])
                E("vector", "tensor_scalar", AL[:, wi, 1, 0, :], PW[:, e_, 1, :], -1.0, None, ALU.mult, r=["PW"], w=["AL"])
                E("vector", "tensor_copy", AL[:, wi, 1, 1, :], PW[:, e_, 1, :], r=["PW"], w=["AL"])

            BB = T("BB", [64, 2, 32, 16])
            t5 = T("t5", [64, 2, 32, 16]); t6 = T("t6", [64, 2, 32, 16])
            creb = cre.unsqueeze(2).to_broadcast([64, 32, 16]); cimb = cim.unsqueeze(2).to_broadcast([64, 32, 16])
            Q_ = KS + ["Bt", "t5", "t6"]
            V2(t5[:, 0], Bt[:, 0], creb, ALU.mult, Q_, ["t5"])
            V2(t6[:, 0], Bt[:, 1], cimb, ALU.mult, Q_, ["t6"])
            V2(BB[:, 0], t5[:, 0], t6[:, 0], ALU.subtract, Q_, ["BB"])
            V2(t5[:, 0], Bt[:, 1], creb, ALU.mult, Q_, ["t5"])
            V2(t6[:, 0], Bt[:, 0], cimb, ALU.mult, Q_, ["t6"])
            V2(BB[:, 0 + 1], t5[:, 0], t6[:, 0], ALU.add, Q_, ["BB"])

            BA = T("BA", [64, 16, 2, 32, 16], BF16)
            CT4 = CT[:].rearrange("p a (g c) -> p a g c", c=16)

            def cprod(dst, src_re, src_im, e0, ne, neg_im, keys_r, key_w):
                pr = PW[:, e0:e0 + ne, 0, :].unsqueeze(3).to_broadcast([64, ne, 32, 16])
                pi_ = PW[:, e0:e0 + ne, 1, :].unsqueeze(3).to_broadcast([64, ne, 32, 16])
                sr = src_re.unsqueeze(1).to_broadcast([64, ne, 32, 16])
                si = src_im.unsqueeze(1).to_broadcast([64, ne, 32, 16])
                R_ = ["PW", "t5", "t6"] + keys_r
                V2(t5[:, 0:ne], sr, pr, ALU.mult, R_, ["t5"])
                V2(t6[:, 0:ne], si, pi_, ALU.mult, R_, ["t6"])
                V2(dst[:, e0:e0 + ne, 0], t5[:, 0:ne], t6[:, 0:ne], ALU.subtract, R_, [key_w])
                V2(t5[:, 0:ne], sr, pi_, ALU.mult, R_, ["t5"])
                V2(t6[:, 0:ne], si, pr, ALU.mult, R_, ["t6"])
                if neg_im:
                    E("vector", "scalar_tensor_tensor", dst[:, e0:e0 + ne, 1], t5[:, 0:ne], -1.0, t6[:, 0:ne],
                      ALU.mult, ALU.subtract, r=R_, w=[key_w])
                else:
                    V2(dst[:, e0:e0 + ne, 1], t5[:, 0:ne], t6[:, 0:ne], ALU.add, R_, [key_w])

            for e0 in range(0, 16, 2):
                cprod(BA, BB[:, 0], BB[:, 1], e0, 2, False, ["BB"], "BA")
            for e0 in range(0, 16, 2):
                cprod(CA, CT4[:, 0], CT4[:, 1], e0, 2, True, ["CT"], "CA")
            cprod(CA, CT4[:, 0], CT4[:, 1], 16, 1, True, ["CT"], "CA")

            pmask = T("pmask", [128, 2])
            pidx = T("pidx", [128, 1])
            E("gpsimd", "iota", pidx[:], pattern=[[0, 1]], base=0, channel_multiplier=1,
              allow_small_or_imprecise_dtypes=True, w=["pidx"])
            pi32 = T("pi32", [128, 2], I32)
            E("vector", "tensor_copy", pi32[:, 0:1], pidx[:], r=["pidx"], w=["pi32"])
            E("vector", "tensor_scalar", pi32[:, 1:2], pi32[:, 0:1], 4, 1, ALU.logical_shift_right, ALU.bitwise_and,
              r=["pi32"], w=["pi32b"])
            E("vector", "tensor_copy", pmask[:, 1:2], pi32[:, 1:2], r=["pi32b"], w=["pmask1"])
            E("vector", "tensor_scalar", pmask[:, 0:1], pmask[:, 1:2], -1.0, 1.0, ALU.mult, ALU.add,
              r=["pmask1"], w=["pmask"])
            nb_ = 0
            for q in range(4):
                for j0 in range(0, 16, 8):
                    wp, wk = wpb[nb_ % 2], f"wpb{nb_ % 2}"
                    nb_ += 1
                    for jj in range(8):
                        j = j0 + jj
                        for ri in range(2):
                            E("tensor", "transpose", wp[:, (jj * 2 + ri) * 64:(jj * 2 + ri + 1) * 64],
                              BA[:, 15 - j, ri, 8 * q:8 * q + 8, :].rearrange("p g c -> p (g c)"), ident_bf[0:64, 0:64],
                              r=["BA", "ident_bf"], w=[wk])
                    src = wp[:].rearrange("p (j r n) -> p j r n", j=8, r=2)
                    for par in range(2):
                        E("vector" if par == 0 else "gpsimd" if False else "vector", "tensor_scalar",
                          WS[:, q, j0:j0 + 8, :, par, :], src, pmask[:, par:par + 1], None, ALU.mult,
                          r=[wk, "pmask", "pmask1"], w=["WS"])
            CTb = T("CTb", [64, 2, 512], BF16)
            E("vector", "tensor_copy", CTb[:, 0, :], CT[:, 0, :], r=["CT"], w=["CTb"])
            E("vector", "tensor_scalar", CTb[:, 1, :], CT[:, 1, :], -1.0, None, ALU.mult, r=["CT"], w=["CTb"])
            bdm = T("bdm", [128, 128])
            io2 = T("io2", [128, 128], I32)
            E("gpsimd", "iota", io2[:], pattern=[[1, 128]], base=0, channel_multiplier=0, w=["io2"])
            E("vector", "tensor_scalar", io2[:], io2[:], 4, None, ALU.logical_shift_right, r=["io2"], w=["io2"])
            gcol = T("gcol", [128, 128])
            E("vector", "tensor_copy", gcol[:], io2[:], r=["io2"], w=["gcol"])
            prow = T("prow", [128, 2], I32)
            E("vector", "tensor_scalar", prow[:, 0:1], pi32[:, 0:1], 4, None, ALU.logical_shift_right, r=["pi32"], w=["prow"])
            prowf = T("prowf", [128, 1])
            E("vector", "tensor_copy", prowf[:], prow[:, 0:1], r=["prow"], w=["prowf"])
            E("vector", "tensor_scalar", bdm[:], gcol[:], prowf[:, 0:1], None, ALU.is_equal, r=["gcol", "prowf"], w=["bdm"])
            bdm4 = bdm[:].unsqueeze(1).to_broadcast([128, 4, 128])
            for q in range(4):
                for d0 in range(0, 16, 4):
                    wp, wk = wps[nb_ % 2], f"wps{nb_ % 2}"
                    nb_ += 1
                    for dd in range(4):
                        for ri in range(2):
                            mm(wp[:, dd * 128:(dd + 1) * 128],
                               BA[:, d0 + dd, ri, 8 * q:8 * q + 8, :].rearrange("p g c -> p (g c)"),
                               CTb[:, ri, q * 128:(q + 1) * 128], start=(ri == 0), stop=(ri == 1),
                               r=["BA", "CTb"], w=[wk])
                    V2(KM[:, q, d0:d0 + 4, :], wp[:].rearrange("p (d n) -> p d n", d=4), bdm4, ALU.mult,
                       [wk, "bdm"], [f"KM{q}"])
                dg = T(f"dg{q}", [128, 128])
                E("vector", "tensor_scalar", dg[:], ident_f[:], dcol[:, q:q + 1], None, ALU.mult,
                  r=["ident_f", "dcol"], w=[f"dg{q}"])
                V2(KM[:, q, 0, :], KM[:, q, 0, :], dg[:], ALU.add, [f"KM{q}", f"dg{q}"], [f"KM{q}"])
            S.barrier()
            S.emit()

        uT = sb("uT", [128, 4, TOK], BF16, pS)
        Hb = sb("Hb", [64, 129, 2, 32], BF16, pS)
        with contextlib.ExitStack() as pU:
            w_u = sb("w_u", [128, 8, 512], BF16, pU)
            for c in range(8):
                dma(w_u[:, c, :], w_in_c[:, c, 1544:2056], writes=[f"w_u{c}"], queue="gpsimd")
            u_ps = [ps(f"u_ps{i}", [128, 512], F32, pU) for i in range(2)]
            nu = 0
            for q in range(4):
                for nb in range(5):
                    blk = slice(nb * 512, min((nb + 1) * 512, TOK))
                    n = blk.stop - blk.start
                    up, uk = u_ps[nu % 2], f"u_ps{nu % 2}"
                    for c in range(8):
                        mm(up[:, 0:n], w_u[:, c, q * 128:(q + 1) * 128], hnT[:, c, blk], start=(c == 0), stop=(c == 7),
                           r=[f"w_u{c}"], w=[uk])
                    if nu % 2 == 0:
                        act(uT[:, q, blk], up[:, 0:n], ACT.Copy, r=[uk], w=[f"uT{q}"])
                    else:
                        E("vector", "tensor_copy", uT[:, q, blk], up[:, 0:n], r=[uk], w=[f"uT{q}"])
                    nu += 1
            S.barrier()
            S.emit()

        E("gpsimd", "memset", Hb[:, 0, :, :], 0.0, w=["Hb0"])
        uTj = [uT[:, q, 0:SEQ].rearrange("p (k j) -> p j k", j=16) for q in range(4)]
        h0b = sb("h0b", [64, 2, 32, NS], BF16, pS)
        with contextlib.ExitStack() as pH:
          s_ps = [ps(f"ss_ps{i}", [128, 512], F32, pH) for i in range(2)]
          fps = ps("fps", [128, 512], F32, pH)
          with contextlib.ExitStack() as pHi:
            Hf = sb("Hf", [64, 128, 2, 32], F32, pHi)
            nsp = 0
            for gp in range(16):
                q, pp = gp // 4, gp % 4
                sp_, sk = s_ps[nsp % 2], f"ss_ps{nsp % 2}"
                nsp += 1
                for ri in range(2):
                    for par in range(2):
                        o = (ri * 2 + par) * 128
                        for j in range(16):
                            op("tensor", lambda e, o=o, sp_=sp_, q=q, pp=pp, j=j, ri=ri, par=par: e.matmul(
                                sp_[0:64, o:o + 128], WS[32 * pp:32 * pp + 32, q, j, ri, par, :],
                                uTj[q][32 * pp:32 * pp + 32, j, :], start=(j == 0), stop=(j == 15),
                                tile_position=(32 * pp, 0)), [f"uT{q}", "WS"], [sk])
                E("vector" if gp % 2 == 0 else "scalar", "tensor_copy" if gp % 2 == 0 else "activation",
                  Hf[:, :, :, 2 * gp:2 * gp + 2].rearrange("p k r g -> p r g k"),
                  sp_[0:64, :].rearrange("p (r g k) -> p r g k", r=2, g=2),
                  *(() if gp % 2 == 0 else (ACT.Copy,)), r=[sk], w=[f"Hf_g{gp}"])
            rt = [sb(f"rt{i}", [64, 2, 32], F32, pHi) for i in range(2)]
            allg = [f"Hf_g{gp}" for gp in range(16)]
            prev_key = allg
            for kc in range(1, 128):
                cur = f"Hk{kc}"
                P_ = Hf[:, kc - 1, :, :]
                V2(rt[0][:], P_, AL[:, 0, 0, :, :], ALU.mult, prev_key + ["AL", "rt0"], ["rt0"])
                V2(rt[1][:, 0, :], P_[:, 1, :], AL[:, 0, 1, 0, :], ALU.mult, prev_key + ["AL", "rt1"], ["rt1"])
                V2(rt[1][:, 1, :], P_[:, 0, :], AL[:, 0, 1, 1, :], ALU.mult, prev_key + ["AL", "rt1"], ["rt1"])
                V2(rt[0][:], rt[0][:], rt[1][:], ALU.add, ["rt0", "rt1"], ["rt0"])
                V2(Hf[:, kc, :, :], Hf[:, kc, :, :], rt[0][:], ALU.add, ["rt0"] + (allg if kc == 1 else []), [cur])
                prev_key = [cur]
            E("vector", "tensor_copy", Hb[:, 1:129, :, :], Hf[:], r=prev_key + allg, w=["Hb"])
            fin = sb("fin", [32, 2, 64], F32, pHi)
            for ri in range(2):
                E("tensor", "transpose", fps[0:32, ri * 64:(ri + 1) * 64], Hf[:, 127, ri, :], ident_f[0:64, 0:64],
                  r=prev_key + ["ident_f"], w=["fps"])
            E("vector", "tensor_copy", fin[:].rearrange("p a n -> p (a n)"), fps[0:32, 0:128], r=["fps"], w=["fin"])
            S.mark_output(dma(hr_p[:, :], fin[:, 0, :], reads=["fin"]))
            S.mark_output(dma(hi_p[:, :], fin[:, 1, :], reads=["fin"]))
            S.barrier()
            S.emit()
          if True:

            h0n = sb("h0n", [128, 2, 64], F32, pH)
            dma(h0n[:, 0, :], st_re_d[:, :], writes=["h0n"])
            dma(h0n[:, 1, :], st_im_d[:, :], writes=["h0n"])
            h0 = sb("h0", [64, 2, NS, 32], F32, pH)
            h1 = sb("h1", [64, 2, NS, 32], F32, pH)
            for ri in range(2):
                E("tensor", "transpose", fps[0:64, 128 + ri * 128:256 + ri * 128], h0n[:, ri, :], ident_f[:],
                  r=["h0n", "ident_f"], w=["fps2"])
            E("vector", "tensor_copy", h0[:].rearrange("p r s g -> p (r s g)"), fps[0:64, 128:384], r=["fps2"], w=["h0"])
            E("vector", "tensor_copy", h0b[:].rearrange("p r g s -> p r s g"), h0[:], r=["h0"], w=["h0b"])
            ssp = s_ps[0]
            for g in range(32):
                q, pp, par = g // 8, (g % 8) // 2, g % 2
                for ri in range(2):
                    o = (ri * 32 + g) * NS
                    op("tensor", lambda e, o=o, q=q, pp=pp, ri=ri, par=par: e.matmul(
                        ssp[0:64, o:o + NS], WS[32 * pp:32 * pp + 32, q, 15, ri, par, :],
                        uT[32 * pp:32 * pp + 32, q, SEQ:SEQ + NS], start=True, stop=True,
                        tile_position=(32 * pp, 0)), [f"uT{q}", "WS"], ["ss_ps0"])
            a1b = [AL[:, 1, i, :, :].unsqueeze(2).to_broadcast([64, 2, NS, 32]) for i in range(2)]
            t7 = sb("t7", [64, 2, NS, 32], F32, pH); t8 = sb("t8", [64, 2, NS, 32], F32, pH)
            V2(t7[:], h0[:], a1b[0], ALU.mult, ["h0", "AL"], ["t7"])
            V2(t8[:, 0], h0[:, 1], a1b[1][:, 0], ALU.mult, ["h0", "AL"], ["t8"])
            V2(t8[:, 1], h0[:, 0], a1b[1][:, 1], ALU.mult, ["h0", "AL"], ["t8"])
            V2(t7[:], t7[:], t8[:], ALU.add, ["t7", "t8"], ["t7"])
            V2(h1[:], t7[:], ssp[0:64, 0:2 * 32 * NS].rearrange("p (r g s) -> p r s g", r=2, g=32), ALU.add,
               ["t7", "ss_ps0"], ["h1"])
            for ri in range(2):
                E("tensor", "transpose", fps[:, 384 + ri * 64:448 + ri * 64], h1[:, ri, :, :].rearrange("p s g -> p (s g)"),
                  ident_f[0:64, 0:64], r=["h1", "ident_f"], w=["fps3"])
            h1o = sb("h1o", [128, 2, 64], F32, pH)
            E("vector", "tensor_copy", h1o[:].rearrange("p a n -> p (a n)"), fps[:, 384:512], r=["fps3"], w=["h1o"])
            S.mark_output(dma(hr_s[:, :], h1o[:, 0, :], reads=["h1o"]))
            S.mark_output(dma(hi_s[:, :], h1o[:, 1, :], reads=["h1o"]))
            S.barrier()
            S.emit()

        with contextlib.ExitStack() as pY:
            y_ps = ps("y_ps", [128, 2048], F32, pY)
            ys_ps = ps("ys_ps", [128, 512], F32, pY)
            t_ps2 = ps("t_ps2", [128, 2560], BF16, pY)
            ysb = [sb(f"ysb{i}", [128, 2048], BF16, pY) for i in range(2)]
            yss = sb("yss", [128, 512], BF16, pY)
            g1 = sb("g1", [128, 2176], F32, pY); g2 = sb("g2", [128, 2176], F32, pY)
            for q in range(4):
                mm(ys_ps[0:NS, q * 128:(q + 1) * 128], uT[:, q, SEQ:SEQ + NS], KM[:, q, 0, :], start=(q == 0), stop=False,
                   r=[f"uT{q}", f"KM{q}"], w=["ys_ps"])
            for g in range(32):
                for ri in range(2):
                    mm(ys_ps[0:NS, g * 16:(g + 1) * 16], h0b[:, ri, g, :], CA[:, 1, ri, g, :], start=False,
                       stop=(g == 31 and ri == 1), r=["h0b", "CA"], w=["ys_ps"])
            E("vector", "memset", yss[:], 0.0, w=["yss"])
            E("vector", "tensor_copy", yss[0:NS, :], ys_ps[0:NS, :], r=["ys_ps"], w=["yss"])
            for q in range(4):
                yk = f"y_ps"
                for j in range(16):
                    for i in range(j + 1):
                        mm(y_ps[:, j * 128:(j + 1) * 128], uTj[q][:, i, :], KM[:, q, j - i, :], start=(i == 0), stop=False,
                           r=[f"uT{q}", f"KM{q}"], w=[yk])
                    for gl in range(8):
                        g = 8 * q + gl
                        for ri in range(2):
                            mm(y_ps[:, j * 128 + gl * 16:j * 128 + (gl + 1) * 16], Hb[:, 0:128, ri, g], CA[:, j + 1, ri, g, :],
                               start=False, stop=(gl == 7 and ri == 1), r=["Hb", "Hb0", "CA"], w=[yk])
                yb, ybk = ysb[q % 2], f"ysb{q % 2}"
                act(yb[:], y_ps[:], ACT.Copy, r=[yk], w=[ybk])
                for j in range(16):
                    E("tensor", "transpose", t_ps2[:, j * 128:(j + 1) * 128], yb[:, j * 128:(j + 1) * 128], ident_bf[:],
                      r=[ybk, "ident_bf"], w=["t_ps2"])
                E("tensor", "transpose", t_ps2[:, 2048:2176], yss[:, q * 128:(q + 1) * 128], ident_bf[:],
                  r=["yss", "ident_bf"], w=["t_ps2"])
                xin = t_ps2[:, 0:2176]
                act(g1[:], xin, ACT.Square, r=["t_ps2"], w=["g1"])
                E("vector", "tensor_scalar", g1[:], g1[:], 0.044715, 1.0, ALU.mult, ALU.add, r=["g1"], w=["g1"])
                E("vector", "tensor_tensor", g1[:], g1[:], xin, ALU.mult, r=["g1", "t_ps2"], w=["g1"])
                act(g2[:], g1[:], ACT.Sigmoid, scale=1.5957691216057308, r=["g1"], w=["g2"])
                E("vector", "tensor_tensor", zsT[:, q, 0:SEQ].rearrange("p (k j) -> p j k", j=16),
                  g2[:, 0:SEQ].rearrange("p (j k) -> p j k", j=16), t_ps2[:, 0:SEQ].rearrange("p (j k) -> p j k", j=16),
                  ALU.mult, r=["g2", "t_ps2"], w=[f"zsT{q}"])
                E("vector", "tensor_tensor", zsT[:, q, SEQ:TOK], g2[:, SEQ:TOK], t_ps2[:, SEQ:TOK], ALU.mult,
                  r=["g2", "t_ps2"], w=[f"zsT{q}"])
            if dbg and dbg[0] == "zs":
                dt_ = sb("dbgt", [128, TOK], F32, pY)
                dv = dbg_t.rearrange("p (a n) -> p a n", a=4)
                for a in range(4):
                    E("vector", "tensor_copy", dt_[:], zsT[:, a, :], r=[f"zsT{a}"], w=["dbgt"])
                    S.mark_output(dma(dv[:, a, :], dt_[:], reads=["dbgt"]))
            S.barrier()
            S.emit()

    pA = contextlib.ExitStack()
    attnT = sb("attnT", [128, 4, TOK], BF16, pA)
    E("gpsimd", "memset", attnT[:, :, SEQ:TOK], 0.0, w=["attnT_s"])
    if stage == 2 or stage >= 4:
      with contextlib.ExitStack() as p2:
        v_bf = sb("v_bf", [128, NT, AW], BF16, p2)
        dma(v_bf[:], v_p.rearrange("(t p) c -> p t c", p=128), writes=["v_bf"], queue="gpsimd")
        w_qk = sb("w_qk", [128, 8, 1024], BF16, p2)
        for c in range(8):
            dma(w_qk[:, c, :], w_in_c[:, c, 0:1024], writes=[f"w_qk{c}"], queue="gpsimd")
        w_f = sb("w_f", [128, 8, 8], BF16, p2)
        dma(w_f[:], w_in_c[:, :, 1536:1544], writes=["w_f"], queue="gpsimd")
        negb8 = sb("negb8", [8, 1], F32, p2)
        dma(negb8[:], b_forget.rearrange("o h -> h o"), writes=["negb8"])
        E("vector", "tensor_scalar", negb8[:], negb8[:], -1.0, None, ALU.mult, r=["negb8"], w=["negb8"])
        ones8 = sb("ones8", [8, SEQ], F32, p2)
        E("gpsimd", "memset", ones8[:], 1.0, w=["ones8"])
        spl = sb("spl", [8, SEQ], F32, p2)
        cs = sb("cs", [8, SEQ], F32, p2)
        e1 = [sb(f"e1_{i}", [8, 512], F32, p2) for i in range(2)]
        c_split = sb("c_split", [8, 3, SEQ], BF16, p2)
        tmpf = [sb(f"tmpf{i}", [8, SEQ], F32, p2) for i in range(3)]
        negc_tok = sb("negc_tok", [128, NT * H], F32, p2)
        with contextlib.ExitStack() as p2a:
            f_ps = [ps(f"f_ps{i}", [128, 512], F32, p2a) for i in range(2)]
            t_ps = ps("t_ps", [128, 512], F32, p2a)
            for nb in range(4):
                fp = f_ps[nb % 2]
                blk = slice(nb * 512, (nb + 1) * 512)
                for c in range(8):
                    mm(fp[0:8, :], w_f[:, c, :], hnT[:, c, blk], start=(c == 0), stop=(c == 7),
                       r=["w_f"], w=[f"f_ps{nb % 2}"])
                act(e1[nb % 2][:], fp[0:8, :], ACT.Exp, scale=-1.0, bias=negb8[:, 0:1],
                    r=[f"f_ps{nb % 2}", "negb8"], w=[f"e1_{nb % 2}"])
                act(spl[:, blk], e1[nb % 2][:], ACT.Ln, bias=1.0, r=[f"e1_{nb % 2}"], w=[f"spl{nb}"])
            E("vector", "tensor_tensor_scan", cs[:], ones8[:], spl[:], 0.0, ALU.mult, ALU.add,
              r=["ones8"] + [f"spl{i}" for i in range(4)], w=["cs"])
            E("vector", "tensor_scalar", c_split[:, 0, :], cs[:], -8.0, None, ALU.mult, r=["cs"], w=["c_hi"])
            E("vector", "tensor_copy", tmpf[0][:], c_split[:, 0, :], r=["c_hi"], w=["tmpf0"])
            E("vector", "scalar_tensor_tensor", tmpf[1][:], cs[:], -8.0, tmpf[0][:], ALU.mult, ALU.subtract,
              r=["cs", "tmpf0"], w=["tmpf1"])
            E("vector", "tensor_copy", c_split[:, 1, :], tmpf[1][:], r=["tmpf1"], w=["c_mid"])
            E("vector", "tensor_copy", tmpf[2][:], c_split[:, 1, :], r=["c_mid"], w=["tmpf2"])
            E("vector", "tensor_tensor", tmpf[0][:], tmpf[1][:], tmpf[2][:], ALU.subtract,
              r=["tmpf1", "tmpf2"], w=["tmpf0"])
            E("vector", "tensor_copy", c_split[:, 2, :], tmpf[0][:], r=["tmpf0"], w=["c_lo"])
            for t in range(NT):
                E("tensor", "transpose", t_ps[:, t * 8:(t + 1) * 8], cs[0:8, t * 128:(t + 1) * 128],
                  ident_f[0:8, 0:8], r=["cs", "ident_f"], w=["t_ps"])
            E("vector", "tensor_copy", negc_tok[:], t_ps[:, 0:NT * H], r=["t_ps"], w=["negc_tok"])
            S.barrier()
            S.emit()

        qa = [sb(f"qa{i}", [128, SEQ], BF16, p2) for i in range(2)]
        ka = [sb(f"ka{i}", [128, SEQ], BF16, p2) for i in range(2)]
        vp = [sb(f"vp{i}", [128, NT, 128], BF16, p2) for i in range(2)]
        onesp = [sb(f"onesp{i}", [128, 128], BF16, p2) for i in range(2)]
        pT = [sb(f"pT{i}", [128, 512], BF16, p2) for i in range(3)]
        rl = [sb(f"rl{i}", [128, 512], F32, p2) for i in range(2)]
        for i in range(2):
            E("gpsimd", "memset", ka[i][64:67, :], 1.0, w=[f"ka{i}"])
            E("gpsimd", "memset", vp[i][:], 0.0, w=[f"vp{i}"])
            E("gpsimd", "memset", onesp[i][:], 0.0, w=[f"onesp{i}"])
            E("gpsimd", "memset", onesp[i][:, i * 64:(i + 1) * 64], 1.0, w=[f"onesp{i}"])
        with contextlib.ExitStack() as p2b:
            pj_ps = [ps(f"pj_ps{i}", [128, 512], F32, p2b) for i in range(2)]
            s_ps = [ps(f"s_ps{i}", [128, 512], F32, p2b) for i in range(2)]
            o_ps = [ps(f"o_ps{i}", [128, 512], F32, p2b) for i in range(2)]
            l_ps = [ps(f"l_ps{i}", [128, 512], F32, p2b) for i in range(2)]
            npj = 0
            nsc = 0
            ngr = 0
            for pr in range(4):
                for hh in range(2):
                    h = 2 * pr + hh
                    for (dst, dk, col0) in ((qa[hh], f"qa{hh}", h * 64), (ka[hh], f"ka{hh}", 512 + h * 64)):
                        for nb in range(4):
                            blk = slice(nb * 512, (nb + 1) * 512)
                            pp, pk = pj_ps[npj % 2], f"pj_ps{npj % 2}"
                            for c in range(8):
                                mm(pp[0:64, :], w_qk[:, c, col0:col0 + 64], hnT[:, c, blk],
                                   start=(c == 0), stop=(c == 7), r=[f"w_qk{c}"], w=[pk])
                            if npj % 2 == 0:
                                act(dst[0:64, blk], pp[0:64, :], ACT.Copy, r=[pk], w=[dk])
                            else:
                                E("vector", "tensor_copy", dst[0:64, blk], pp[0:64, :], r=[pk], w=[dk])
                            npj += 1
                    for i in range(3):
                        dma(qa[hh][64 + i:65 + i, :], c_split[h:h + 1, i, :], reads=["c_hi", "c_mid", "c_lo"],
                            writes=[f"qa{hh}"])
                    E("vector", "tensor_copy", vp[hh][:, :, hh * 64:(hh + 1) * 64], v_bf[:, :, h * 64:(h + 1) * 64],
                      r=["v_bf"], w=[f"vp{hh}"])
                for g in range(4):
                    gb = ngr % 2
                    OP, LP = f"o_ps{gb}", f"l_ps{gb}"
                    first = True
                    for hh in range(2):
                        h = 2 * pr + hh
                        for j in range(4 * g + 4):
                            rr = j - 4 * g
                            c0 = max(rr, 0) * 128
                            sp_, sk = s_ps[nsc % 2], f"s_ps{nsc % 2}"
                            pt, pk = pT[nsc % 3], f"pT{nsc % 3}"
                            nsc += 1
                            mm(sp_[:, c0:512], ka[hh][0:67, j * 128:(j + 1) * 128],
                               qa[hh][0:67, g * 512 + c0:(g + 1) * 512], r=[f"ka{hh}", f"qa{hh}"], w=[sk])
                            act(pt[:, c0:512], sp_[:, c0:512], ACT.Exp, scale=0.125,
                                bias=negc_tok[:, j * H + h:j * H + h + 1], r=[sk, "negc_tok"], w=[pk])
                            if rr >= 0:
                                E("gpsimd", "tensor_tensor", pt[:, c0:c0 + 128], pt[:, c0:c0 + 128], tri[:], ALU.mult,
                                  r=[pk, "tri"], w=[pk])
                            last = (hh == 1 and j == 4 * g + 3)
                            mm(o_ps[gb][:, c0:512], vp[hh][:, j, :], pt[:, c0:512], start=first, stop=last,
                               r=[f"vp{hh}", pk], w=[OP])
                            mm(l_ps[gb][:, c0:512], onesp[hh][:], pt[:, c0:512], start=first, stop=last,
                               r=[f"onesp{hh}", pk], w=[LP])
                            first = False
                    E("vector", "reciprocal", rl[gb][:], l_ps[gb][:], r=[LP], w=[f"rl{gb}"])
                    E("vector", "tensor_tensor", attnT[:, pr, g * 512:(g + 1) * 512], o_ps[gb][:], rl[gb][:], ALU.mult,
                      r=[OP, f"rl{gb}"], w=[f"attnT{pr}_{g}"])
                    ngr += 1
            if dbg and dbg[0] == "attn":
                dt_ = sb("dbgt", [128, SEQ], F32, p2b)
                dv = dbg_t.rearrange("p (a n) -> p a n", a=4)
                for a in range(4):
                    E("vector", "tensor_copy", dt_[:], attnT[:, a, 0:SEQ],
                      r=[f"attnT{a}_{b_}" for b_ in range(4)], w=["dbgt"])
                    S.mark_output(dma(dv[:, a, :], dt_[:], reads=["dbgt"]))
            S.barrier()
            S.emit()

    if stage >= 4:
      with contextlib.ExitStack() as pq:
        ck_d = k.din("cache_k", [NPHYS * 128, AW]); cv_d = k.din("cache_v", [NPHYS * 128, AW])
        cl_d = k.din("cache_logf", [NPHYS, 128 * H])
        pt_d = k.din("page_table", [1, NS * NPAGES], I32)
        NEG = -1.0e30
        w_qs = sb("w_qs", [128, 8, AW], BF16, pq)
        for c in range(8):
            dma(w_qs[:, c, :], w_in_c[:, c, 0:512], writes=["w_qs"], queue="gpsimd")
        ptb = sb("ptb", [128, NS * NPAGES], I32, pq)
        dma(ptb[:], pt_d.partition_broadcast(128), writes=["ptb"])
        ptf = sb("ptf", [128, NS * NPAGES], F32, pq)
        pidx = sb("pidx_q", [128, 1], F32, pq)
        E("gpsimd", "iota", pidx[:], pattern=[[0, 1]], base=0, channel_multiplier=1,
          allow_small_or_imprecise_dtypes=True, w=["pidx"])
        E("vector", "tensor_copy", ptf[:], ptb[:], r=["ptb"], w=["ptf"])
        E("vector", "tensor_scalar", ptf[:], ptf[:], 128.0, None, ALU.mult, r=["ptf"], w=["ptf"])
        E("vector", "tensor_scalar", ptf[:], ptf[:], pidx[:, 0:1], None, ALU.add, r=["ptf", "pidx"], w=["ptf"])
        rowi = sb("rowi", [128, NS * NPAGES], I32, pq)
        E("vector", "tensor_copy", rowi[:], ptf[:], r=["ptf"], w=["rowi"])
        mext = sb("mext", [128, 1], F32, pq)
        E("vector", "tensor_scalar", mext[:], pidx[:], 0.0, NEG, ALU.is_gt, ALU.mult, r=["pidx"], w=["mext"])
        pg2 = sb("pg2", [128, 2], I32, pq)
        for m in range(2):
            dma(pg2[:, m:m + 1], pt_d[0:1, m * 128:(m + 1) * 128].rearrange("o n -> n o"), writes=["pg2"])
        ones_f = sb("ones_f", [128, 128], F32, pq)
        E("gpsimd", "memset", ones_f[:], 1.0, w=["ones_f"])
        lt2 = sb("lt2", [128, 128], F32, pq)
        bo2 = sb("bo2", [128, 128], F32, pq)
        E("vector", "tensor_single_scalar", lt2[:], iota_t[:], 0.0, ALU.is_gt, r=["iota_t"], w=["lt2"])
        E("vector", "memset", lt2[0:64, 64:128], 0.0, w=["lt2"])
        E("vector", "memset", bo2[:], 0.0, w=["bo2"])
        E("vector", "memset", bo2[0:64, 0:64], 1.0, w=["bo2"])
        E("vector", "memset", bo2[64:128, 64:128], 1.0, w=["bo2"])
        q_ps = ps("q_ps", [128, 512], F32, pq)
        m_ps = ps("m_ps", [128, 512], F32, pq)
        bt_ps = [ps(f"bt_ps{i}", [128, 512], F32, pq) for i in range(2)]
        o_ps = ps("os_ps", [128, 512], F32, pq)
        qb = sb("qb", [128, NS, AW], F32, pq)
        hb = [sb(f"hb{i}", [128, 8, 128], BF16, pq) for i in range(2)]
        for s_ in range(NS):
            col = SEQ + s_
            E("vector", "tensor_copy", hb[s_ % 2][:], hnT[:, :, col:col + 1].to_broadcast([128, 8, 128]), w=[f"hb{s_ % 2}"])
            for c in range(8):
                mm(q_ps[:], hb[s_ % 2][:, c, :], w_qs[:, c, :], start=(c == 0), stop=(c == 7), r=[f"hb{s_ % 2}", "w_qs"], w=["q_ps"])
            act(qb[:, s_, :], q_ps[:], ACT.Copy, scale=0.125, r=["q_ps"], w=["qb"])
        BT = sb("BT", [128, 2, H, 128], F32, pq)
        lfn = sb("lfn", [128, 2, H], F32, pq)
        for m in range(2):
            for s2 in range(2):
                dma(lfn[s2 * 64:(s2 + 1) * 64, m, :], lf_s[2 * m + s2:2 * m + s2 + 1, :].partition_broadcast(64),
                    writes=["lfn"])
        with contextlib.ExitStack() as pl:
            Lg = [sb(f"Lg{i}", [128, 128 * H], F32, pl) for i in range(2)]
            Cg = [sb(f"Cg{i}", [128, 128 * H], F32, pl) for i in range(2)]
            base = sb("base", [128, 2, H], F32, pl)
            for m in range(2):
                dma(Lg[m][:], cl_d[:, :], reads=["pg2"], writes=[f"Lg{m}"], queue="gpsimd",
                    gather=bass.IndirectOffsetOnAxis(ap=pg2[:, m:m + 1], axis=0))
                for h in range(H):
                    E("vector", "tensor_tensor_scan", Cg[m][:, h:128 * H:H], ones_f[:], Lg[m][:, h:128 * H:H], 0.0,
                      ALU.mult, ALU.add, r=[f"Lg{m}", "ones_f"], w=[f"Cg{m}"])
                tot = Cg[m][:, 127 * H:128 * H]
                mm(m_ps[:, m * 16:m * 16 + 8], lt2[:], tot, r=["lt2", f"Cg{m}"], w=["m_ps"])
                mm(m_ps[:, m * 16 + 8:m * 16 + 16], bo2[:], tot, r=["bo2", f"Cg{m}"], w=["m_ps"])
                E("vector", "tensor_tensor", base[:, m, :], m_ps[:, m * 16 + 8:m * 16 + 16], lfn[:, m, :], ALU.add,
                  r=["m_ps", "lfn"], w=["base"])
                E("vector", "tensor_tensor", base[:, m, :], base[:, m, :], m_ps[:, m * 16:m * 16 + 8], ALU.subtract,
                  r=["m_ps", "base"], w=["base"])
                E("vector", "scalar_tensor_tensor", Cg[m][:].rearrange("p (r h) -> p r h", h=H),
                  Cg[m][:].rearrange("p (r h) -> p r h", h=H), -1.0,
                  base[:, m, :].unsqueeze(1).to_broadcast([128, 128, H]), ALU.mult, ALU.add,
                  r=[f"Cg{m}", "base"], w=[f"Cg{m}"])
                for h4 in range(2):
                    bp = bt_ps[h4]
                    for hh in range(4):
                        h = h4 * 4 + hh
                        E("tensor", "transpose", bp[:, hh * 128:(hh + 1) * 128], Cg[m][:, h:128 * H:H], ident_f[:],
                          r=[f"Cg{m}", "ident_f"], w=[f"bt_ps{h4}"])
                    E("vector", "tensor_copy", BT[:, m, h4 * 4:h4 * 4 + 4, :].rearrange("p h x -> p (h x)"), bp[:],
                      r=[f"bt_ps{h4}"], w=["BT"])
            S.barrier()
            S.emit()
        Sc = sb("Sc", [128, NS, NPAGES + 1, H], F32, pq)
        NKB = 4
        Kb = [sb(f"Kb{i}", [128, 4, AW], F32, pq) for i in range(NKB)]
        Vb = [sb(f"Vb{i}", [128, 4, AW], F32, pq) for i in range(NKB)]
        prod = sb("prod", [128, 4, AW], F32, pq)
        Kx = sb("Kx", [128, NS, AW], F32, pq)
        Vx = sb("Vx", [128, NS, AW], F32, pq)
        E("gpsimd", "memset", Kx[:], 0.0, w=["Kx"])
        E("gpsimd", "memset", Vx[:], 0.0, w=["Vx"])
        dma(Kx[0:1, :, :], k_s.rearrange("(o s) c -> o s c", o=1), writes=["Kx"])
        dma(Vx[0:1, :, :], v_s.rearrange("(o s) c -> o s c", o=1), writes=["Vx"])
        ng = 0
        for s_ in range(NS):
            m, s2 = s_ // 2, s_ % 2
            for g4 in range(NPAGES // 4):
                kb, kk = Kb[ng % NKB], f"Kb{ng % NKB}"
                ng += 1
                for pi in range(4):
                    pg = g4 * 4 + pi
                    dma(kb[:, pi, :], ck_d[:, :], reads=["rowi"], writes=[kk], queue="gpsimd",
                        gather=bass.IndirectOffsetOnAxis(ap=rowi[:, s_ * NPAGES + pg:s_ * NPAGES + pg + 1], axis=0))
                E("vector", "tensor_tensor", prod[:], kb[:], qb[:, s_, :].unsqueeze(1).to_broadcast([128, 4, AW]), ALU.mult,
                  r=[kk, "qb"], w=["prod"])
                E("vector", "tensor_reduce", Sc[:, s_, g4 * 4:g4 * 4 + 4, :], prod[:].rearrange("p g (h d) -> p g h d", h=H),
                  AX.X, ALU.add, r=["prod"], w=[f"Sc{s_}"])
            E("vector", "tensor_tensor", prod[:, 0, :], Kx[:, s_, :], qb[:, s_, :], ALU.mult, r=["Kx", "qb"], w=["prod"])
            E("vector", "tensor_reduce", Sc[:, s_, NPAGES, :], prod[:, 0, :].rearrange("p (h d) -> p h d", h=H),
              AX.X, ALU.add, r=["prod"], w=[f"Sc{s_}"])
            E("vector", "tensor_scalar", Sc[:, s_, NPAGES, :], Sc[:, s_, NPAGES, :], mext[:, 0:1], None, ALU.add,
              r=[f"Sc{s_}", "mext"], w=[f"Sc{s_}"])
            btv = BT[:, m, :, s2 * 64:(s2 + 1) * 64].rearrange("p h g -> p g h")
            E("vector", "tensor_tensor", Sc[:, s_, 0:NPAGES, :], Sc[:, s_, 0:NPAGES, :], btv, ALU.add,
              r=[f"Sc{s_}", "BT"], w=[f"Sc{s_}"])
        SCK = [f"Sc{i}" for i in range(NS)]
        mx = sb("mx", [128, NS * H], F32, pq)
        E("vector", "tensor_reduce", mx[:].rearrange("p (s h) -> p s h", h=H), Sc[:].rearrange("p s g h -> p s h g"),
          AX.X, ALU.max, r=SCK, w=["mx"])
        E("tensor", "transpose", m_ps[0:32, 128:256], mx[:], ident_f[:], r=["mx", "ident_f"], w=["m_ps"])
        gm = sb("gm", [32, 1], F32, pq)
        E("vector", "tensor_reduce", gm[:], m_ps[0:32, 128:256], AX.X, ALU.max, r=["m_ps"], w=["gm"])
        dgm = sb("dgm", [32, 32], F32, pq)
        E("vector", "tensor_scalar", dgm[:], ident_f[0:32, 0:32], gm[:, 0:1], None, ALU.mult, r=["gm", "ident_f"], w=["dgm"])
        mm(m_ps[:, 256:288], ones_f[0:32, :], dgm[:], r=["ones_f", "dgm"], w=["m_ps2"])
        gmb = sb("gmb", [128, NS * H], F32, pq)
        E("vector", "tensor_copy", gmb[:], m_ps[:, 256:288], r=["m_ps2"], w=["gmb"])
        E("vector", "tensor_tensor", Sc[:], Sc[:],
          gmb[:].rearrange("p (s h) -> p s h", h=H).unsqueeze(2).to_broadcast([128, NS, NPAGES + 1, H]), ALU.subtract,
          r=SCK + ["gmb"], w=["ScA"])
        act(Sc[:].rearrange("p s g h -> p (s g h)"), Sc[:].rearrange("p s g h -> p (s g h)"), ACT.Exp, r=["ScA"], w=["ScP"])
        ls = sb("ls", [128, NS * H], F32, pq)
        E("vector", "tensor_reduce", ls[:].rearrange("p (s h) -> p s h", h=H), Sc[:].rearrange("p s g h -> p s h g"),
          AX.X, ALU.add, r=["ScP"], w=["ls"])
        mm(m_ps[:, 320:352], ones_f[:], ls[:], r=["ones_f", "ls"], w=["m_ps3"])
        rlb = sb("rlb", [128, NS * H], F32, pq)
        E("vector", "reciprocal", rlb[:], m_ps[:, 320:352], r=["m_ps3"], w=["rlb"])
        first = True
        for s_ in range(NS):
            for g4 in range(NPAGES // 4 + 1):
                if g4 < NPAGES // 4:
                    vb, vk = Vb[ng % NKB], f"Vb{ng % NKB}"
                    ng += 1
                    npg = 4
                    for pi in range(4):
                        pg = g4 * 4 + pi
                        dma(vb[:, pi, :], cv_d[:, :], reads=["rowi"], writes=[vk], queue="gpsimd",
                            gather=bass.IndirectOffsetOnAxis(ap=rowi[:, s_ * NPAGES + pg:s_ * NPAGES + pg + 1], axis=0))
                else:
                    npg = 1
                for pi in range(npg):
                    pg = g4 * 4 + pi
                    src = vb[:, pi, :] if g4 < NPAGES // 4 else Vx[:, s_, :]
                    sk = vk if g4 < NPAGES // 4 else "Vx"
                    for c4 in range(4):
                        last = (s_ == NS - 1 and g4 == NPAGES // 4 and c4 == 3)
                        o0 = (s_ * 4 + c4) * H
                        mm(o_ps[:, o0:o0 + H], src[:, c4 * 128:(c4 + 1) * 128], Sc[:, s_, pg, :], start=first, stop=last,
                           r=[sk, "ScP"], w=["os_ps"])
                        first = False
        ov = o_ps[:, 0:NS * 4 * H].rearrange("p (s c h) -> p s c h", s=NS, c=4)
        rv = rlb[:].rearrange("p (s h) -> p s h", h=H)
        for pr in range(4):
            for hh in range(2):
                rws = slice(hh * 64, (hh + 1) * 64)
                E("vector", "tensor_tensor", attnT[rws, pr, SEQ:SEQ + NS], ov[rws, :, pr, 2 * pr + hh], rv[rws, :, 2 * pr + hh],
                  ALU.mult, r=["os_ps", "rlb", "attnT_s"], w=["attnT_s"])
        S.barrier()
        S.emit()

    h2_d = nc.dram_tensor("h2_scratch", [TOK, D], F32, kind="Internal").ap()
    uv_d = nc.dram_tensor("uv_scratch", [16384, 2 * D], BF16, kind="Internal").ap()
    if stage >= 5:
      with contextlib.ExitStack() as p3:
        w_ao_d = k.din("w_ao", [AW, D]); w_ga_d = k.din("w_glu_a", [AW, D]); w_gb_d = k.din("w_glu_b", [AW, D])
        w_out_d = k.din("w_out", [D, D])
        w_ao = sb("w_aos", [128, 4, D], BF16, p3); w_ga = sb("w_gla", [128, 4, D], BF16, p3); w_gb = sb("w_glb", [128, 4, D], BF16, p3)
        w_gta = sb("w_gta", [128, 8, D], BF16, p3); w_gts = sb("w_gts", [128, 8, D], BF16, p3)
        w_o = sb("w_o", [128, 8, D], BF16, p3)
        for (dst, src, nm) in ((w_ao, w_ao_d, "w_ao"), (w_ga, w_ga_d, "w_gla"), (w_gb, w_gb_d, "w_glb")):
            sv = src.rearrange("(c p) n -> p c n", p=128)
            for c in range(4):
                dma(dst[:, c, :], sv[:, c, :], writes=[nm], queue="gpsimd")
        w_out_c = w_out_d.rearrange("(c p) n -> p c n", p=128)
        for c in range(8):
            dma(w_gta[:, c, :], w_in_c[:, c, 2056:3080], writes=["w_gta"], queue="gpsimd")
            dma(w_gts[:, c, :], w_in_c[:, c, 3080:4104], writes=["w_gts"], queue="gpsimd")
            dma(w_o[:, c, :], w_out_c[:, c, :], writes=["w_o"], queue="gpsimd")
        pu_d = k.din("peer_u", [16384, D]); pv_d = k.din("peer_v", [16384, D])
        uvs = [sb(f"uvs{i}", [128, 2, 2 * D], BF16, p3) for i in range(2)]
        for i in range(64):
            b = i % 2
            rs = slice(i * 256, (i + 1) * 256)
            dma(uvs[b][:, :, 0:D], pu_d[rs, :].rearrange("(p r) d -> p r d", r=2), writes=[f"uvs{b}"], queue="gpsimd")
            dma(uvs[b][:, :, D:2 * D], pv_d[rs, :].rearrange("(p r) d -> p r d", r=2), writes=[f"uvs{b}"], queue="gpsimd")
            dma(uv_d[rs, :].rearrange("(p r) d -> p r d", r=2), uvs[b][:], reads=[f"uvs{b}"], writes=["uv_d"], queue="gpsimd")
        mT = sb("mT", [128, 8, 512], BF16, p3)
        sg = [sb(f"sg{i}", [128, 512], F32, p3) for i in range(3)]
        tm = [sb(f"tm{i}", [128, 512], F32, p3) for i in range(2)]
        xr = [sb(f"xr{i}", [128, D], F32, p3) for i in range(2)]
        h2t = [sb(f"h2t{i}", [128, D], F32, p3) for i in range(2)]
        E("vector", "memset", xr[0][:], 0.0, w=["xr0"])
        E("vector", "memset", xr[1][:], 0.0, w=["xr1"])
        b_ps = [ps(f"b_ps{i}", [128, 512], F32, p3) for i in range(5)]
        h_ps = ps("h_ps", [128, 1024], F32, p3)
        ntile = 0
        for nb in (4, 0, 1, 2, 3):
            blk = slice(nb * 512, min((nb + 1) * 512, TOK))
            n = blk.stop - blk.start
            for oc in range(8):
                ocs = slice(oc * 128, (oc + 1) * 128)
                for (pi, wt, src, nk, wk, rk) in ((0, w_ao, attnT, 4, "w_ao", "attnT"), (1, w_ga, zsT, 4, "w_gla", "zsT"),
                                                  (2, w_gb, zsT, 4, "w_glb", "zsT"), (3, w_gta, hnT, 8, "w_gta", "hnT"),
                                                  (4, w_gts, hnT, 8, "w_gts", "hnT")):
                    for c in range(nk):
                        mm(b_ps[pi][:, 0:n], wt[:, c, ocs], src[:, c, blk], start=(c == 0), stop=(c == nk - 1),
                           r=[wk, rk], w=[f"b_ps{pi}"])
                act(sg[0][:, 0:n], b_ps[3][:, 0:n], ACT.Sigmoid, r=["b_ps3"], w=["sg0"])
                act(sg[1][:, 0:n], b_ps[4][:, 0:n], ACT.Sigmoid, r=["b_ps4"], w=["sg1"])
                act(sg[2][:, 0:n], b_ps[2][:, 0:n], ACT.Sigmoid, r=["b_ps2"], w=["sg2"])
                E("vector", "tensor_tensor", tm[0][:, 0:n], b_ps[1][:, 0:n], sg[2][:, 0:n], ALU.mult,
                  r=["b_ps1", "sg2"], w=["tm0"])
                E("vector", "tensor_tensor", tm[0][:, 0:n], tm[0][:, 0:n], sg[1][:, 0:n], ALU.mult, r=["tm0", "sg1"], w=["tm0"])
                E("vector", "tensor_tensor", tm[1][:, 0:n], b_ps[0][:, 0:n], sg[0][:, 0:n], ALU.mult,
                  r=["b_ps0", "sg0"], w=["tm1"])
                E("vector", "tensor_tensor", mT[:, oc, 0:n], tm[0][:, 0:n], tm[1][:, 0:n], ALU.add,
                  r=["tm0", "tm1"], w=[f"mT{oc}"])
            for ti in range(n // 128):
                tt = nb * 4 + ti
                b = ntile % 2
                ntile += 1
                if tt == 16:
                    dma(xr[b][0:NS, :], x_s[:, :], writes=[f"xr{b}"])
                else:
                    dma(xr[b][:], x_p[tt * 128:(tt + 1) * 128, :], writes=[f"xr{b}"])
                for half in range(2):
                    for c in range(8):
                        mm(h_ps[:, half * 512:(half + 1) * 512], mT[:, c, ti * 128:(ti + 1) * 128],
                           w_o[:, c, half * 512:(half + 1) * 512], start=(c == 0), stop=(c == 7),
                           r=[f"mT{c}", "w_o"], w=["h_ps"])
                E("vector", "tensor_tensor", h2t[b][:], h_ps[:], xr[b][:], ALU.add, r=["h_ps", f"xr{b}"], w=[f"h2t{b}"])
                tk = dma(h2_d[tt * 128:(tt + 1) * 128, :], h2t[b][:], reads=[f"h2t{b}"], writes=[f"h2d{tt}"])
                if dbg and dbg[0] == "h2":
                    S.mark_output(dma(dbg_t[tt * 128:(tt + 1) * 128, :], h2t[b][:], reads=[f"h2t{b}"]))
        S.barrier()
        S.emit()

    pA.close()
    pG.close()
    if stage >= 6:
      with contextlib.ExitStack() as p4:
        wq_d = k.din("peer_w_q", [D, 2048]); keys_d = k.din("peer_keys", [16, 128, 128])
        gffn_d = k.din("g_ffn", [1, D]); gple_d = k.din("g_ple", [1, D]); gfin_d = k.din("g_final", [1, D])
        wple_d = k.din("w_ple", [256, D]); wpg_d = k.din("w_ple_gate", [D, D])
        pp_d = k.din("p_p", [SEQ, 256]); ps_d = k.din("p_s", [NS, 256])
        y_p = k.dout("y_p", [SEQ, D]); y_s = k.dout("y_s", [NS, D])

        w_q = sb("w_q", [128, 8, 2048], BF16, p4)
        wq_c = wq_d.rearrange("(c p) n -> p c n", p=128)
        for c in range(8):
            dma(w_q[:, c, :], wq_c[:, c, :], writes=["w_q"], queue="gpsimd")
        w_pg = sb("w_pg", [128, 8, D], BF16, p4)
        wpg_c = wpg_d.rearrange("(c p) n -> p c n", p=128)
        for c in range(8):
            dma(w_pg[:, c, :], wpg_c[:, c, :], writes=["w_pg"], queue="gpsimd")
        w_pl = sb("w_pl", [128, 2, D], BF16, p4)
        wpl_c = wple_d.rearrange("(c p) n -> p c n", p=128)
        for c in range(2):
            dma(w_pl[:, c, :], wpl_c[:, c, :], writes=["w_pl"], queue="gpsimd")
        gv = sb("gv", [128, 3, D], F32, p4)
        for i, gd in enumerate((gffn_d, gple_d, gfin_d)):
            dma(gv[:, i, :], gd.partition_broadcast(128), writes=["gv"])
        io16 = sb("io16", [128, 16], F32, p4)
        E("gpsimd", "iota", io16[:], pattern=[[1, 16]], base=0, channel_multiplier=0,
          allow_small_or_imprecise_dtypes=True, w=["io16"])
        keysT = sb("keysT", [128, 16, 128], BF16, p4)
        A_ps = ps("A_ps", [128, 1024], BF16, p4)
        B_ps = ps("B_ps", [128, 512], F32, p4)
        C_ps = ps("C_ps", [128, 2048], F32, p4)
        D_ps = ps("D_ps", [128, 1024], F32, p4)
        with contextlib.ExitStack() as pk:
            kn = sb("kn", [128, 16, 128], BF16, pk)
            dma(kn[:], keys_d.rearrange("a k d -> k a d"), writes=["kn"], queue="gpsimd")
            for half in range(2):
                for a in range(8):
                    E("tensor", "transpose", A_ps[:, a * 128:(a + 1) * 128], kn[:, half * 8 + a, :], ident_bf[:],
                      r=["kn", "ident_bf"], w=["A_ps"])
                E("vector", "tensor_copy", keysT[:, half * 8:half * 8 + 8, :].rearrange("p a k -> p (a k)"), A_ps[:],
                  r=["A_ps"], w=["keysT"])
            S.barrier()
            S.emit()

        NU = 4
        uvb = [sb(f"uvb{i}", [128, 4, 2 * D], BF16, p4) for i in range(NU)]
        h2t = sb("h2t4", [128, D], F32, p4)
        hn2f = sb("hn2f", [128, D], F32, p4)
        hnb = sb("hnb", [128, D], BF16, p4)
        jk = sb("jk4", [128, D], BF16, p4)
        hT = sb("hT4", [128, 8, 128], BF16, p4)
        qpT = sb("qpT", [128, 16, 128], BF16, p4)
        S1 = sb("S1", [128, 4096], F32, p4)
        S2 = sb("S2", [128, 2048], F32, p4)
        vals = sb("vals", [128, 16, 16], F32, p4)
        ixu = sb("ixu", [128, 16, 16], U32, p4)
        ixf = sb("ixf", [128, 16, 16], F32, p4)
        tops = sb("tops", [128, 8, 16], F32, p4)
        posu = sb("posu", [128, 8, 16], U32, p4)
        abi = sb("abi", [128, 2, 8, 16], U32, p4)
        abf = sb("abf", [128, 2, 8, 16], F32, p4)
        ijf = sb("ijf", [128, 2, 8, 16], F32, p4)
        eidf = sb("eidf", [128, 128], F32, p4)
        eidx = sb("eidx", [128, 128], I32, p4)
        gat = sb("gat", [128, 8, 16], F32, p4)
        gsm = sb("gsm", [128, 8, 2], F32, p4)
        scr = sb("scr", [128, 128], F32, p4)
        wsl = sb("wsl", [128, 128], F32, p4)
        gt = [sb(f"gt{i}", [128, 128], F32, p4) for i in range(2)]
        dg4 = [sb(f"dg4_{i}", [128, 128], BF16, p4) for i in range(4)]
        g5 = [sb(f"g5_{i}", [128, 2, 4], F32, p4) for i in range(2)]
        st4 = sb("st4", [128, 3, 4], F32, p4)
        h3 = sb("h3", [128, D], F32, p4)
        gate = sb("gate", [128, D], F32, p4)
        pt_f = sb("pt_f", [128, 256], F32, p4)
        pt_b = sb("pt_b", [128, 256], BF16, p4)
        pT4 = sb("pT4", [128, 2, 128], BF16, p4)
        yo = sb("yo", [128, D], F32, p4)
        E("vector", "memset", pt_f[:], 0.0, w=["pt_f"])
        NEG = -1.0e30

        def rms(src, src_keys, gi, out_f, out_b, tag):
            st = st4[:, gi, :]
            act(jk[:], src, ACT.Square, accum_out=st[:, 0:1], r=src_keys, w=["jk4", f"st4{gi}"])
            E("vector", "tensor_scalar", st[:, 1:2], st[:, 0:1], 1.0 / D, EPS, ALU.mult, ALU.add, r=[f"st4{gi}"], w=[f"st4{gi}"])
            act(st[:, 2:3], st[:, 1:2], ACT.Sqrt, r=[f"st4{gi}"], w=[f"st4{gi}"])
            E("vector", "reciprocal", st[:, 3:4], st[:, 2:3], r=[f"st4{gi}"], w=[f"st4{gi}"])
            if out_f is not None:
                E("vector", "scalar_tensor_tensor", out_f[0], src, st[:, 3:4], gv[:, gi, :], ALU.mult, ALU.mult,
                  r=src_keys + [f"st4{gi}", "gv"], w=[out_f[1]])
            if out_b is not None:
                if out_f is not None:
                    E("gpsimd", "tensor_copy", out_b[0], out_f[0], r=[out_f[1]], w=[out_b[1]])
                else:
                    E("vector", "scalar_tensor_tensor", out_b[0], src, st[:, 3:4], gv[:, gi, :], ALU.mult, ALU.mult,
                      r=src_keys + [f"st4{gi}", "gv"], w=[out_b[1]])

        order = [16] + list(range(16))
        if stage == 6:
            order = [16, 0]
        for tt in order:
            rows = slice(tt * 128, (tt + 1) * 128)
            dma(h2t[:], h2_d[rows, :], reads=[f"h2d{tt}"], writes=["h2t4"])
            rms(h2t[:], ["h2t4"], 0, (hn2f[:], "hn2f"), (hnb[:], "hnb"), "a")
            for c in range(8):
                E("tensor", "transpose", A_ps[:, c * 128:(c + 1) * 128], hnb[:, c * 128:(c + 1) * 128], ident_bf[:],
                  r=["hnb", "ident_bf"], w=["A_ps"])
            act(hT[:].rearrange("p c n -> p (c n)"), A_ps[:], ACT.Copy, r=["A_ps"], w=["hT4"])
            for g4 in range(4):
                for a in range(4):
                    hp = g4 * 4 + a
                    for c in range(8):
                        mm(B_ps[:, a * 128:(a + 1) * 128], w_q[:, c, hp * 128:(hp + 1) * 128], hT[:, c, :],
                           start=(c == 0), stop=(c == 7), r=["w_q", "hT4"], w=["B_ps"])
                if g4 % 2 == 0:
                    act(qpT[:, g4 * 4:g4 * 4 + 4, :].rearrange("p a n -> p (a n)"), B_ps[:], ACT.Copy, r=["B_ps"], w=["qpT"])
                else:
                    E("vector", "tensor_copy", qpT[:, g4 * 4:g4 * 4 + 4, :].rearrange("p a n -> p (a n)"), B_ps[:],
                      r=["B_ps"], w=["qpT"])
            for hp in range(16):
                mm(C_ps[:, hp * 128:(hp + 1) * 128], qpT[:, hp, :], keysT[:, hp, :], r=["qpT", "keysT"], w=["C_ps"])
            sc = S1[:, 0:2048]; sc2 = S1[:, 2048:4096]
            act(sc, C_ps[:], ACT.Copy, r=["C_ps"], w=["S1a"])
            for hp in range(16):
                s_ = sc[:, hp * 128:(hp + 1) * 128]; s2_ = sc2[:, hp * 128:(hp + 1) * 128]
                E("vector", "max", vals[:, hp, 0:8], s_, r=["S1a"], w=["vals"])
                E("vector", "match_replace", s2_, vals[:, hp, 0:8], s_, NEG, r=["S1a", "vals"], w=["S1b"])
                E("vector", "max", vals[:, hp, 8:16], s2_, r=["S1b"], w=["vals"])
                E("vector", "max_index", ixu[:, hp, 0:8], vals[:, hp, 0:8], s_, r=["S1a", "vals"], w=["ixu"])
                E("vector", "max_index", ixu[:, hp, 8:16], vals[:, hp, 8:16], s2_, r=["S1b", "vals"], w=["ixu"])
            E("vector", "tensor_copy", ixf[:], ixu[:], r=["ixu"], w=["ixf"])
            v4 = vals[:].rearrange("p (h q) k -> p h q k", q=2)
            cand = S2[:].rearrange("p (h a b) -> p h a b", h=8, a=16)
            E("vector", "tensor_tensor", cand, v4[:, :, 0, :].unsqueeze(3).to_broadcast([128, 8, 16, 16]),
              v4[:, :, 1, :].unsqueeze(2).to_broadcast([128, 8, 16, 16]), ALU.add, r=["vals"], w=["S2"])
            c2 = S1[:, 0:2048]
            for h in range(8):
                c_ = S2[:, h * 256:(h + 1) * 256]; c2_ = c2[:, h * 256:(h + 1) * 256]
                E("vector", "max", tops[:, h, 0:8], c_, r=["S2"], w=["tops"])
                E("vector", "match_replace", c2_, tops[:, h, 0:8], c_, NEG, r=["S2", "tops", "S1a"], w=["S1a"])
                E("vector", "max", tops[:, h, 8:16], c2_, r=["S1a"], w=["tops"])
                E("vector", "max_index", posu[:, h, 0:8], tops[:, h, 0:8], c_, r=["S2", "tops"], w=["posu"])
                E("vector", "max_index", posu[:, h, 8:16], tops[:, h, 8:16], c2_, r=["S1a", "tops"], w=["posu"])
            E("vector", "tensor_single_scalar", abi[:, 0], posu[:], 4, ALU.logical_shift_right, r=["posu"], w=["abi"])
            E("vector", "tensor_single_scalar", abi[:, 1], posu[:], 15, ALU.bitwise_and, r=["posu"], w=["abi"])
            E("vector", "tensor_copy", abf[:], abi[:], r=["abi"], w=["abf"])
            eq = S1[:, 2048:4096].rearrange("p (h k a) -> p h k a", h=8, k=16)
            ix4 = ixf[:].rearrange("p (h q) k -> p h q k", q=2)
            io_b = io16[:].unsqueeze(1).unsqueeze(1).to_broadcast([128, 8, 16, 16])
            for w_ in range(2):
                E("vector", "tensor_tensor", eq, abf[:, w_].unsqueeze(3).to_broadcast([128, 8, 16, 16]), io_b, ALU.is_equal,
                  r=["abf", "io16", "S1b"], w=["S1b"])
                E("vector", "tensor_tensor", eq, eq, ix4[:, :, w_, :].unsqueeze(2).to_broadcast([128, 8, 16, 16]), ALU.mult,
                  r=["S1b", "ixf"], w=["S1b"])
                E("vector", "tensor_reduce", ijf[:, w_], eq, AX.X, ALU.add, r=["S1b"], w=["ijf"])
            E("vector", "scalar_tensor_tensor", eidf[:].rearrange("p (h k) -> p h k", h=8), ijf[:, 0], 128.0, ijf[:, 1],
              ALU.mult, ALU.add, r=["ijf"], w=["eidf"])
            E("vector", "tensor_copy", eidx[:], eidf[:], r=["eidf"], w=["eidx"])
            E("vector", "tensor_tensor", gat[:], tops[:], tops[:, :, 0:1].to_broadcast([128, 8, 16]), ALU.subtract,
              r=["tops"], w=["gat"])
            act(gat[:], gat[:], ACT.Exp, r=["gat"], w=["gat"])
            E("vector", "tensor_reduce", gsm[:, :, 0], gat[:], AX.X, ALU.add, r=["gat"], w=["gsm"])
            E("vector", "reciprocal", gsm[:, :, 1], gsm[:, :, 0], r=["gsm"], w=["gsm"])
            E("vector", "tensor_tensor", gat[:], gat[:], gsm[:, :, 1:2].to_broadcast([128, 8, 16]), ALU.mult,
              r=["gat", "gsm"], w=["gat"])
            gatf = gat[:].rearrange("p h k -> p (h k)")
            for grp in range(32):
                bi = grp % NU
                buf, bk = uvb[bi], f"uvb{bi}"
                sls = slice(grp * 4, grp * 4 + 4)
                for i in range(4):
                    sl = grp * 4 + i
                    dma(buf[:, i, :], uv_d[:, :], reads=["eidx", "uv_d"], writes=[bk], queue="gpsimd",
                        gather=bass.IndirectOffsetOnAxis(ap=eidx[:, sl:sl + 1], axis=0))
                for i in range(4):
                    sl = grp * 4 + i
                    E("vector", "scalar_tensor_tensor", jk[:], buf[:, i, 0:D], 1.0, hn2f[:], ALU.mult, ALU.mult,
                      accum_out=scr[:, sl:sl + 1], r=[bk, "hn2f"], w=["jk4", f"scr{grp}"])
                ga_, gk = g5[grp % 2], f"g5_{grp % 2}"
                E("vector", "tensor_tensor", ga_[:, 0, :], scr[:, sls], scr[:, sls], ALU.mult, r=[f"scr{grp}"], w=[gk])
                E("vector", "tensor_scalar", ga_[:, 0, :], ga_[:, 0, :], 0.044715, 1.0, ALU.mult, ALU.add, r=[gk], w=[gk])
                E("vector", "tensor_tensor", ga_[:, 0, :], ga_[:, 0, :], scr[:, sls], ALU.mult, r=[gk, f"scr{grp}"], w=[gk])
                act(ga_[:, 1, :], ga_[:, 0, :], ACT.Sigmoid, scale=1.5957691216057308, r=[gk], w=[gk])
                E("vector", "tensor_tensor", ga_[:, 1, :], ga_[:, 1, :], scr[:, sls], ALU.mult, r=[gk, f"scr{grp}"], w=[gk])
                E("vector", "tensor_tensor", wsl[:, sls], ga_[:, 1, :], gatf[:, sls], ALU.mult, r=[gk, "gat"], w=[f"wsl{grp}"])
                for i in range(4):
                    sl = grp * 4 + i
                    d3 = sl % 4
                    act(dg4[d3][:], ident_f[:], ACT.Copy, scale=wsl[:, sl:sl + 1], r=["ident_f", f"wsl{grp}"], w=[f"dg4_{d3}"])
                    for half in range(2):
                        mm(D_ps[:, half * 512:(half + 1) * 512], dg4[d3][:], buf[:, i, D + half * 512:D + (half + 1) * 512],
                           start=(sl == 0), stop=(sl == 127), r=[f"dg4_{d3}", bk], w=["D_ps"])
            E("vector", "tensor_tensor", h3[:], D_ps[:], h2t[:], ALU.add, r=["D_ps", "h2t4"], w=["h3"])
            if dbg and dbg[0] == "h3":
                S.mark_output(dma(dbg_t[rows, :], h3[:], reads=["h3"]))
            rms(h3[:], ["h3"], 1, None, (hnb[:], "hnb"), "b")
            for c in range(8):
                E("tensor", "transpose", A_ps[:, c * 128:(c + 1) * 128], hnb[:, c * 128:(c + 1) * 128], ident_bf[:],
                  r=["hnb", "ident_bf"], w=["A_ps"])
            act(hT[:].rearrange("p c n -> p (c n)"), A_ps[:], ACT.Copy, r=["A_ps"], w=["hT4"])
            for half in range(2):
                for c in range(8):
                    mm(C_ps[:, half * 512:(half + 1) * 512], hT[:, c, :], w_pg[:, c, half * 512:(half + 1) * 512],
                       start=(c == 0), stop=(c == 7), r=["hT4", "w_pg"], w=["C_ps"])
            act(gate[:], C_ps[:, 0:1024], ACT.Sigmoid, r=["C_ps"], w=["gate"])
            if tt == 16:
                dma(pt_f[0:NS, :], ps_d[:, :], writes=["pt_f"])
            else:
                dma(pt_f[:], pp_d[rows, :], writes=["pt_f"])
            E("vector", "tensor_copy", pt_b[:], pt_f[:], r=["pt_f"], w=["pt_b"])
            for c in range(2):
                E("tensor", "transpose", A_ps[:, c * 128:(c + 1) * 128], pt_b[:, c * 128:(c + 1) * 128], ident_bf[:],
                  r=["pt_b", "ident_bf"], w=["A_ps"])
            E("vector", "tensor_copy", pT4[:].rearrange("p c n -> p (c n)"), A_ps[:, 0:256], r=["A_ps"], w=["pT4"])
            for half in range(2):
                for c in range(2):
                    mm(C_ps[:, 1024 + half * 512:1024 + (half + 1) * 512], pT4[:, c, :], w_pl[:, c, half * 512:(half + 1) * 512],
                       start=(c == 0), stop=(c == 1), r=["pT4", "w_pl"], w=["C_ps2"])
            E("vector", "tensor_tensor", gate[:], C_ps[:, 1024:2048], gate[:], ALU.mult, r=["C_ps2", "gate"], w=["gate"])
            E("vector", "tensor_tensor", h3[:], h3[:], gate[:], ALU.add, r=["h3", "gate"], w=["h3"])
            rms(h3[:], ["h3"], 2, (yo[:], "yo"), None, "c")
            if tt == 16:
                S.mark_output(dma(y_s[:, :], yo[0:NS, :], reads=["yo"]))
            else:
                S.mark_output(dma(y_p[rows, :], yo[:], reads=["yo"]))
        S.barrier()
        S.emit()

    S.barrier()
    S.emit(final=True)

    k.stack.close()
    return k


def make_in_maps(inputs):
    maps = []
    for c in range(N_CORES):
        m = {
            "x_p": np.ascontiguousarray(inputs["x_prompt"][c]),
            "x_s": np.ascontiguousarray(inputs["x_sample"][4 * c:4 * c + 4, 0]),
            "w_in": np.ascontiguousarray(inputs["w_in"][0]),
            "b_forget": np.ascontiguousarray(inputs["b_forget"]),
            "g_mix": np.ascontiguousarray(inputs["g_mix"]),
            "lam_re": np.ascontiguousarray(inputs["ssm_lam_re"][0]),
            "lam_im": np.ascontiguousarray(inputs["ssm_lam_im"][0]),
            "log_dt": np.ascontiguousarray(inputs["ssm_log_dt"]),
            "b_re": np.ascontiguousarray(inputs["ssm_b_re"][0]),
            "b_im": np.ascontiguousarray(inputs["ssm_b_im"][0]),
            "c_re": np.ascontiguousarray(inputs["ssm_c_re"][0].reshape(512, 64)),
            "c_im": np.ascontiguousarray(inputs["ssm_c_im"][0].reshape(512, 64)),
            "ssm_d": np.ascontiguousarray(inputs["ssm_d"].reshape(512, 1)),
            "st_re": np.ascontiguousarray(inputs["state_ssm_re"][4 * c:4 * c + 4, 0].reshape(128, 64)),
            "st_im": np.ascontiguousarray(inputs["state_ssm_im"][4 * c:4 * c + 4, 0].reshape(128, 64)),
            "w_ao": np.ascontiguousarray(inputs["w_attn_out"][0]),
            "w_glu_a": np.ascontiguousarray(inputs["w_glu_a"][0]),
            "w_glu_b": np.ascontiguousarray(inputs["w_glu_b"][0]),
            "w_out": np.ascontiguousarray(inputs["w_out"][0]),
            "peer_w_q": np.ascontiguousarray(inputs["peer_w_q"][0]),
            "peer_keys": np.ascontiguousarray(inputs["peer_keys"][0].reshape(16, 128, 128)),
            "peer_u": inputs["peer_u"][0],
            "peer_v": inputs["peer_v"][0],
            "g_ffn": np.ascontiguousarray(inputs["g_ffn"]),
            "g_ple": np.ascontiguousarray(inputs["g_ple"]),
            "g_final": np.ascontiguousarray(inputs["g_final"].reshape(1, D)),
            "w_ple": np.ascontiguousarray(inputs["w_ple"][0]),
            "w_ple_gate": np.ascontiguousarray(inputs["w_ple_gate"][0]),
            "p_p": np.ascontiguousarray(inputs["p_prompt"][0, c]),
            "p_s": np.ascontiguousarray(inputs["p_sample"][0, 4 * c:4 * c + 4, 0]),
            "cache_k": inputs["cache_k"].reshape(NPHYS * 128, AW),
            "cache_v": inputs["cache_v"].reshape(NPHYS * 128, AW),
            "cache_logf": inputs["cache_logf"].reshape(NPHYS, 128 * H),
            "page_table": np.ascontiguousarray(inputs["page_table"][4 * c:4 * c + 4].reshape(1, NS * NPAGES)),
        }
        maps.append(m)
    return maps


def run(inputs, stage=99, trace=False, dbg=None):
    k = build(stage, dbg)
    names = set(k.io.keys())
    maps = [{n: v for n, v in m.items() if n in names} for m in make_in_maps(inputs)]
    res = run_bass_kernel_spmd(k.nc, maps, core_ids=list(range(N_CORES)), trace=trace)
    return res


def kernel(**inputs):
    inputs = {n: np.asarray(v) for n, v in inputs.items()}
    res = run(inputs).results
    B, DB = 8, 32
    f32 = np.float32

    def cat(name, shape):
        return np.stack([np.asarray(r[name]) for r in res], 0).reshape(shape).astype(f32, copy=False)

    return (cat("y_p", (B, SEQ, D)), cat("y_s", (DB, 1, D)),
            cat("k_p", (B, 1, SEQ, H, HD)), cat("v_p", (B, 1, SEQ, H, HD)), cat("lf_p", (B, 1, SEQ, H)),
            cat("hr_p", (B, 1, 32, 64)), cat("hi_p", (B, 1, 32, 64)),
            cat("k_s", (DB, 1, 1, H, HD)), cat("v_s", (DB, 1, 1, H, HD)), cat("lf_s", (DB, 1, 1, H)),
            cat("hr_s", (DB, 1, 32, 64)), cat("hi_s", (DB, 1, 32, 64)))
```

```python
import contextlib
import numpy as np
import concourse.bass as bass
import concourse.mybir as mybir
from concourse.bass_utils import run_bass_kernel_spmd

F32 = mybir.dt.float32
BF16 = mybir.dt.bfloat16
I32 = mybir.dt.int32
U32 = mybir.dt.uint32
ALU = mybir.AluOpType
ACT = mybir.ActivationFunctionType
AX = mybir.AxisListType

N_CORES = 8
NEEDED = ["x_prompt", "x_sample", "w_in", "b_forget", "g_mix", "ssm_lam_re", "ssm_lam_im", "ssm_log_dt", "ssm_b_re", "ssm_b_im", "ssm_c_re", "ssm_c_im", "ssm_d", "state_ssm_re", "state_ssm_im", "w_attn_out", "w_glu_a", "w_glu_b", "w_out", "peer_w_q", "peer_keys", "peer_u", "peer_v", "g_ffn", "g_ple", "g_final", "w_ple", "w_ple_gate", "p_prompt", "p_sample", "cache_k", "cache_v", "cache_logf", "page_table"]
D = 1024
SEQ = 2048
NT = 16
NTT = 17
TOK = NTT * 128
H = 8
HD = 64
AW = 512
IN_W = 4104
NS = 4
NPAGES = 64
NPHYS = 2560
EPS = 1e-6

COMPUTE = ("tensor", "vector", "scalar", "gpsimd")
SEM_SPAN = 30000
N_DMA_SEMS = 16


class Sched:
    def __init__(self, nc, stack):
        self.nc = nc
        self.stack = stack
        self.streams = {e: [] for e in ("tensor", "vector", "scalar", "gpsimd", "sync")}
        self.cnt = {e: 0 for e in self.streams}
        self.esems = {e: [] for e in COMPUTE}
        self.dq = ("sync", "gpsimd")
        self.dsems = [stack.enter_context(nc.semaphore(f"dq{i}")) for i in range(2 * N_DMA_SEMS)]
        self.dcnt = [0] * (2 * N_DMA_SEMS)
        self.dnext = {"sync": 0, "gpsimd": 0}
        self.waited = {}
        self.last_w = {}
        self.readers = {}
        self.n_inst = 0
        self.out_tokens = []

    def _sem_of(self, tok):
        if tok[0] == "e":
            _, eng, n = tok
            idx = (n - 1) // SEM_SPAN
            while len(self.esems[eng]) <= idx:
                self.esems[eng].append(self.stack.enter_context(
                    self.nc.semaphore(f"s_{eng}_{len(self.esems[eng])}")))
            return ("e", eng, idx), self.esems[eng][idx], n - idx * SEM_SPAN
        _, si, val = tok
        return ("d", si), self.dsems[si], val

    def _need_wait(self, eng, tok, same_ok):
        if tok is None:
            return None
        if tok[0] == "e" and tok[1] == eng and same_ok:
            return None
        key, sem, val = self._sem_of(tok)
        if self.waited.get((eng, key), 0) >= val:
            return None
        self.waited[(eng, key)] = val
        return (sem, val)

    def _deps(self, eng, reads, writes, merge=False):
        waits = []
        pe = eng == "tensor"
        for k in reads:
            for t in self.last_w.get(k, []):
                w = self._need_wait(eng, t, same_ok=pe)
                if w:
                    waits.append(w)
        for k in writes:
            if not merge:
                for t in self.last_w.get(k, []):
                    w = self._need_wait(eng, t, same_ok=pe)
                    if w:
                        waits.append(w)
            for r in self.readers.get(k, []):
                w = self._need_wait(eng, r, same_ok=pe)
                if w:
                    waits.append(w)
        return waits

    def _commit(self, tok, reads, writes, merge=False):
        for k in reads:
            self.readers.setdefault(k, []).append(tok)
        for k in writes:
            if merge:
                self.last_w.setdefault(k, []).append(tok)
            else:
                self.last_w[k] = [tok]
                self.readers[k] = []

    def op(self, eng, fn, reads=(), writes=()):
        waits = self._deps(eng, reads, writes)
        self.cnt[eng] += 1
        tok = ("e", eng, self.cnt[eng])
        _, sem, _ = self._sem_of(tok)
        self.streams[eng].append((waits, fn, sem, 1))
        self._commit(tok, reads, writes)
        self.n_inst += 1
        return tok

    def dma(self, out, in_, reads=(), writes=(), queue="sync", gather=None, merge=False, **kw):
        eng = queue
        waits = self._deps(eng, reads, writes, merge)
        si = self.dq.index(eng) * N_DMA_SEMS + self.dnext[eng]
        self.dnext[eng] = (self.dnext[eng] + 1) % N_DMA_SEMS
        if self.dcnt[si] > 0:
            w = self._need_wait(eng, ("d", si, self.dcnt[si]), same_ok=False)
            if w:
                waits.append(w)
        self.dcnt[si] += 16
        tok = ("d", si, self.dcnt[si])
        if gather is None:
            fn = lambda e, o=out, i=in_, kw=kw: e.dma_start(out=o, in_=i, **kw)
        else:
            fn = lambda e, o=out, i=in_, g=gather, kw=kw: e.indirect_dma_start(
                out=o, out_offset=None, in_=i, in_offset=g, **kw)
        self.streams[eng].append((waits, fn, self.dsems[si], 16))
        self._commit(tok, reads, writes, merge)
        self.n_inst += 1
        return tok

    def barrier(self):
        toks = [("e", o, self.cnt[o]) for o in COMPUTE if self.cnt[o] > 0]
        toks += [("d", si, self.dcnt[si]) for si in range(2 * N_DMA_SEMS) if self.dcnt[si] > 0]
        for eng in self.streams:
            waits = []
            for t in toks:
                w = self._need_wait(eng, t, same_ok=True)
                if w:
                    waits.append(w)
            if waits:
                self.streams[eng].append((waits, None, None, 0))
        self.last_w = {}
        self.readers = {}

    def mark_output(self, tok):
        self.out_tokens.append(tok)

    def emit(self, final=False):
        nc = self.nc
        fin = []
        if final:
            for tok in self.out_tokens:
                w = self._need_wait("sync", tok, same_ok=False)
                if w:
                    fin.append(w)
        streams = self.streams
        self.streams = {e: [] for e in streams}

        def run(e, name):
            for waits, fn, sem, inc in streams[name]:
                for (s, v) in waits:
                    e.wait_ge(s, v)
                if fn is not None:
                    fn(e).then_inc(sem, inc)
            if name == "sync":
                for (s, v) in fin:
                    e.wait_ge(s, v)

        with nc.Block() as block:
            @block.sync
            def _(e):
                run(e, "sync")

            @block.tensor
            def _(e):
                run(e, "tensor")

            @block.vector
            def _(e):
                run(e, "vector")

            @block.scalar
            def _(e):
                run(e, "scalar")

            @block.gpsimd
            def _(e):
                run(e, "gpsimd")


class K:
    def __init__(self, stage=99):
        self.stage = stage
        self.nc = bass.Bass("TRN2", target_bir_lowering=False)
        self.stack = contextlib.ExitStack()
        self.S = Sched(self.nc, self.stack)
        self.io = {}

    def din(self, name, shape, dt=F32):
        self.io[name] = self.nc.dram_tensor(name, list(shape), dt, kind="ExternalInput").ap()
        return self.io[name]

    def dout(self, name, shape, dt=F32):
        self.io[name] = self.nc.dram_tensor(name, list(shape), dt, kind="ExternalOutput").ap()
        return self.io[name]

    def sb(self, name, shape, dt=F32, stack=None):
        return (stack or self.stack).enter_context(self.nc.sbuf_tensor(name, list(shape), dt))

    def ps(self, name, shape, dt=F32, stack=None):
        return (stack or self.stack).enter_context(self.nc.psum_tensor(name, list(shape), dt))


def build(stage=99, dbg=None):
    k = K(stage)
    nc, S = k.nc, k.S
    op, dma = S.op, S.dma
    sb, ps = k.sb, k.ps

    def E(eng, method, *args, r=(), w=(), **kw):
        return op(eng, lambda e: getattr(e, method)(*args, **kw), r, w)

    def mm(out, lhsT, rhs, start=True, stop=True, r=(), w=()):
        return op("tensor", lambda e: e.matmul(out, lhsT, rhs, start=start, stop=stop), r, w)

    def act(out, in_, func, r=(), w=(), **kw):
        return op("scalar", lambda e: e.activation(out, in_, func, **kw), r, w)

    x_p = k.din("x_p", [SEQ, D])
    x_s = k.din("x_s", [NS, D])
    w_in = k.din("w_in", [D, IN_W])
    b_forget = k.din("b_forget", [1, H])
    g_mix = k.din("g_mix", [1, D])

    k_p = k.dout("k_p", [SEQ, AW])
    v_p = k.dout("v_p", [SEQ, AW])
    lf_p = k.dout("lf_p", [SEQ, H])
    k_s = k.dout("k_s", [NS, AW])
    v_s = k.dout("v_s", [NS, AW])
    lf_s = k.dout("lf_s", [NS, H])
    dbg_t = k.dout("dbg", list(dbg[1])) if dbg else None
    w_in_c = w_in.rearrange("(c p) n -> p c n", p=128)

    ident_bf = sb("ident_bf", [128, 128], BF16)
    ident_f = sb("ident_f", [128, 128], F32)
    iota_t = sb("iota_t", [128, 128], F32)
    tri = sb("tri", [128, 128], BF16)
    E("gpsimd", "iota", iota_t[:], pattern=[[1, 128]], base=0, channel_multiplier=-1,
      allow_small_or_imprecise_dtypes=True, w=["iota_t"])
    E("vector", "tensor_single_scalar", ident_f[:], iota_t[:], 0.0, ALU.is_equal, r=["iota_t"], w=["ident_f"])
    E("vector", "tensor_copy", ident_bf[:], ident_f[:], r=["ident_f"], w=["ident_bf"])
    E("vector", "tensor_single_scalar", tri[:], iota_t[:], 0.0, ALU.is_ge, r=["iota_t"], w=["tri"])

    pG = contextlib.ExitStack()
    hnT = sb("hnT", [128, 8, TOK], BF16, pG)
    lft = sb("lft", [128, NTT, 3 * H], F32, pG)
    zsT = sb("zsT", [128, 4, TOK], BF16, pG)

    with contextlib.ExitStack() as p1:
        gmix_b = sb("gmix_b", [128, D], F32, p1)
        dma(gmix_b[:], g_mix.partition_broadcast(128), writes=["gmix_b"])
        bf_b = sb("bf_b", [128, H], F32, p1)
        dma(bf_b[:], b_forget.partition_broadcast(128), writes=["bf_b"])
        w_kvf = sb("w_kvf", [128, 8, 1032], BF16, p1)
        for c in range(8):
            dma(w_kvf[:, c, :], w_in_c[:, c, 512:1544], writes=[f"w_kvf{c}"], queue="gpsimd")
        NB = 2
        xt = [sb(f"xt{i}", [128, D], F32, p1) for i in range(NB)]
        hn = [sb(f"hn{i}", [128, D], BF16, p1) for i in range(NB)]
        junk = [sb(f"junk{i}", [128, D], BF16, p1) for i in range(NB)]
        kv = [sb(f"kv{i}", [128, 1024], F32, p1) for i in range(NB)]
        stat = sb("stat", [128, NTT, 4], F32, p1)
        tp_ps = [ps(f"tp_ps{i}", [128, 1024], BF16, p1) for i in range(2)]
        kv_ps = ps("kv_ps", [128, 1536], F32, p1)

        E("vector", "memset", xt[0][:], 0.0, w=["xt0"])
        for t in range(NTT):
            b = t % NB
            X, HN, J, KV = f"xt{b}", f"hn{b}", f"junk{b}", f"kv{b}"
            TP, KP = f"tp_ps{t % 2}", "kv_ps"
            tt = (t - 1) if t > 0 else 16
            cols = slice(tt * 128, (tt + 1) * 128)
            if tt == 16:
                dma(xt[b][0:NS, :], x_s[:, :], writes=[X])
            else:
                dma(xt[b][:], x_p[cols, :], writes=[X])
            st = stat[:, tt, :]
            act(junk[b][:], xt[b][:], ACT.Square, accum_out=st[:, 0:1], r=[X], w=[J, f"st{tt}a"])
            E("vector", "tensor_scalar", st[:, 1:2], st[:, 0:1], 1.0 / D, EPS, ALU.mult, ALU.add,
              r=[f"st{tt}a"], w=[f"st{tt}b"])
            act(st[:, 2:3], st[:, 1:2], ACT.Sqrt, r=[f"st{tt}b"], w=[f"st{tt}c"])
            E("vector", "reciprocal", st[:, 3:4], st[:, 2:3], r=[f"st{tt}c"], w=[f"st{tt}d"])
            E("vector", "scalar_tensor_tensor", hn[b][:], xt[b][:], st[:, 3:4], gmix_b[:], ALU.mult, ALU.mult,
              r=[X, f"st{tt}d", "gmix_b"], w=[HN])
            tp = tp_ps[t % 2]
            for c in range(8):
                E("tensor", "transpose", tp[:, c * 128:(c + 1) * 128], hn[b][:, c * 128:(c + 1) * 128],
                  ident_bf[:], r=[HN, "ident_bf"], w=[TP])
            act(hnT[:, :, cols], tp[:].rearrange("p (c n) -> p c n", c=8), ACT.Copy, r=[TP], w=[f"hnT{tt}"])
            for (lo, hi) in ((0, 512), (512, 1024), (1024, 1032)):
                for c in range(8):
                    mm(kv_ps[:, lo:hi], hnT[:, c, cols], w_kvf[:, c, lo:hi], start=(c == 0), stop=(c == 7),
                       r=[f"hnT{tt}", f"w_kvf{c}"], w=[KP])
            E("vector", "tensor_copy", kv[b][:], kv_ps[:, 0:1024], r=[KP], w=[KV])
            l3 = lft[:, tt, :]
            E("vector", "tensor_tensor", l3[:, 0:H], kv_ps[:, 1024:1032], bf_b[:], ALU.add,
              r=[KP, "bf_b"], w=[f"lf{tt}a"])
            act(l3[:, H:2 * H], l3[:, 0:H], ACT.Exp, scale=-1.0, r=[f"lf{tt}a"], w=[f"lf{tt}b"])
            act(l3[:, 2 * H:3 * H], l3[:, H:2 * H], ACT.Ln, bias=1.0, r=[f"lf{tt}b"], w=[f"lf{tt}c"])
            E("vector", "tensor_scalar", l3[:, 0:H], l3[:, 2 * H:3 * H], -1.0, None, ALU.mult,
              r=[f"lf{tt}c"], w=[f"lf{tt}d"])
            if tt == 16:
                S.mark_output(dma(k_s[:, :], kv[b][0:NS, 0:512], reads=[KV]))
                S.mark_output(dma(v_s[:, :], kv[b][0:NS, 512:1024], reads=[KV]))
                S.mark_output(dma(lf_s[:, :], l3[0:NS, 0:H], reads=[f"lf{tt}d"]))
            else:
                S.mark_output(dma(k_p[cols, :], kv[b][:, 0:512], reads=[KV]))
                S.mark_output(dma(v_p[cols, :], kv[b][:, 512:1024], reads=[KV]))
                S.mark_output(dma(lf_p[cols, :], l3[:, 0:H], reads=[f"lf{tt}d"]))
        S.barrier()
        S.emit()

    if stage >= 3:
      with contextlib.ExitStack() as pS:
        lam_re_d = k.din("lam_re", [32, 64]); lam_im_d = k.din("lam_im", [32, 64])
        log_dt_d = k.din("log_dt", [1, 32])
        b_re_d = k.din("b_re", [32, 64, 16]); b_im_d = k.din("b_im", [32, 64, 16])
        c_re_d = k.din("c_re", [512, 64]); c_im_d = k.din("c_im", [512, 64])
        ssm_d_d = k.din("ssm_d", [512, 1])
        st_re_d = k.din("st_re", [128, 64]); st_im_d = k.din("st_im", [128, 64])
        hr_p = k.dout("hr_p", [32, 64]); hi_p = k.dout("hi_p", [32, 64])
        hr_s = k.dout("hr_s", [128, 64]); hi_s = k.dout("hi_s", [128, 64])

        WS = sb("WS", [128, 4, 16, 2, 2, 64], BF16, pS)
        CA = sb("CA", [64, 17, 2, 32, 16], BF16, pS)
        KM = sb("KM", [128, 4, 16, 128], BF16, pS)
        AL = sb("AL", [64, 2, 2, 2, 32], F32, pS)
        with contextlib.ExitStack() as pW:
            def T(name, shape, dt=F32):
                return sb(name, shape, dt, pW)
            TWO_PI = 2.0 * np.pi
            lnat = T("lnat", [32, 2, 64])
            dma(lnat[:, 0, :], lam_re_d[:, :], writes=["lnat"])
            dma(lnat[:, 1, :], lam_im_d[:, :], writes=["lnat"])
            ldt = T("ldt", [64, 32])
            dma(ldt[:], log_dt_d.partition_broadcast(64), writes=["ldt"])
            Bt = T("Bt", [64, 2, 32, 16])
            dma(Bt[:, 0, :, :], b_re_d.rearrange("g n c -> n g c"), writes=["Bt"])
            dma(Bt[:, 1, :, :], b_im_d.rearrange("g n c -> n g c"), writes=["Bt"])
            Cnat = T("Cnat", [128, 2, 4, 64])
            dma(Cnat[:, 0, :, :], c_re_d.rearrange("(q p) n -> p q n", p=128), writes=["Cnat"])
            dma(Cnat[:, 1, :, :], c_im_d.rearrange("(q p) n -> p q n", p=128), writes=["Cnat"])
            dcol = T("dcol", [128, 4])
            for q in range(4):
                dma(dcol[:, q:q + 1], ssm_d_d[q * 128:(q + 1) * 128, :], writes=["dcol"])
            wps = [ps(f"wps{i}", [128, 512], F32, pW) for i in range(2)]
            wpb = [ps(f"wpb{i}", [128, 1024], BF16, pW) for i in range(2)]
            lam = T("lam", [64, 2, 32])
            for ri in range(2):
                E("tensor", "transpose", wps[0][0:64, ri * 32:(ri + 1) * 32], lnat[:, ri, :], ident_f[0:32, 0:32],
                  r=["lnat", "ident_f"], w=["wps0"])
            E("vector", "tensor_copy", lam[:].rearrange("p a g -> p (a g)"), wps[0][0:64, 0:64], r=["wps0"], w=["lam"])
            CT = T("CT", [64, 2, 512])
            for ri in range(2):
                for q in range(4):
                    E("tensor", "transpose", wps[1][0:64, q * 128:(q + 1) * 128], Cnat[:, ri, q, :], ident_f[:],
                      r=["Cnat", "ident_f"], w=["wps1"])
                E("vector", "tensor_copy", CT[:, ri, :], wps[1][0:64, :], r=["wps1"], w=["CT"])
            sc = T("sc", [64, 16, 32])
            _n = [0]

            def V2(out, a, b, o, r, w):
                E("vector", "tensor_tensor", out, a, b, o, r=r, w=w)

            lr, li = lam[:, 0, :], lam[:, 1, :]
            dt_, lrdt, mag, ang, yv, kk, tmp, rr, sn, cs_, are, aim, den, nre, cre, cim = [sc[:, i, :] for i in range(16)]
            KS = ["sc"]
            act(dt_, ldt[:], ACT.Exp, r=["ldt"], w=KS)
            V2(lrdt, lr, dt_, ALU.mult, ["lam"] + KS, KS)
            act(mag, lrdt, ACT.Exp, r=KS, w=KS)
            V2(ang, li, dt_, ALU.mult, ["lam"] + KS, KS)
            E("vector", "tensor_scalar", yv, ang, 1.0 / TWO_PI, None, ALU.mult, r=KS, w=KS)
            E("vector", "tensor_scalar", kk, yv, 0.5, None, ALU.is_ge, r=KS, w=KS)
            for m in range(2, 8):
                E("vector", "tensor_scalar", tmp, yv, m - 0.5, None, ALU.is_ge, r=KS, w=KS)
                V2(kk, kk, tmp, ALU.add, KS, KS)
            for m in range(1, 3):
                E("vector", "tensor_scalar", tmp, yv, -(m - 0.5), None, ALU.is_le, r=KS, w=KS)
                V2(kk, kk, tmp, ALU.subtract, KS, KS)
            V2(rr, yv, kk, ALU.subtract, KS, KS)
            act(sn, rr, ACT.Sin, scale=TWO_PI, r=KS, w=KS)
            act(tmp, rr, ACT.Abs, r=KS, w=KS)
            hpi = T("hpi", [64, 1])
            E("vector", "memset", hpi[:], float(np.pi / 2), w=["hpi"])
            act(cs_, tmp, ACT.Sin, scale=-TWO_PI, bias=hpi[:, 0:1], r=KS + ["hpi"], w=KS)
            V2(are, mag, cs_, ALU.mult, KS, KS)
            V2(aim, mag, sn, ALU.mult, KS, KS)
            V2(den, lr, lr, ALU.mult, ["lam"] + KS, KS)
            V2(tmp, li, li, ALU.mult, ["lam"] + KS, KS)
            V2(den, den, tmp, ALU.add, KS, KS)
            E("vector", "reciprocal", den, den, r=KS, w=KS)
            E("vector", "tensor_scalar", nre, are, -1.0, None, ALU.add, r=KS, w=KS)
            V2(cre, nre, lr, ALU.mult, ["lam"] + KS, KS)
            V2(tmp, aim, li, ALU.mult, ["lam"] + KS, KS)
            V2(cre, cre, tmp, ALU.add, KS, KS)
            V2(cre, cre, den, ALU.mult, KS, KS)
            V2(cim, aim, lr, ALU.mult, ["lam"] + KS, KS)
            V2(tmp, nre, li, ALU.mult, ["lam"] + KS, KS)
            V2(cim, cim, tmp, ALU.subtract, KS, KS)
            V2(cim, cim, den, ALU.mult, KS, KS)

            PW = T("PW", [64, 17, 2, 32])
            tA = T("tA", [64, 8, 32]); tB = T("tB", [64, 8, 32])
            E("vector", "memset", PW[:, 0, 0, :], 1.0, w=["PW"])
            E("vector", "memset", PW[:, 0, 1, :], 0.0, w=["PW"])
            E("vector", "tensor_copy", PW[:, 1, 0, :], are, r=KS, w=["PW"])
            E("vector", "tensor_copy", PW[:, 1, 1, :], aim, r=KS, w=["PW"])
            for m in (1, 2, 4, 8):
                xr, xi = PW[:, 1:m + 1, 0, :], PW[:, 1:m + 1, 1, :]
                yr = PW[:, m:m + 1, 0, :].to_broadcast([64, m, 32]); yi = PW[:, m:m + 1, 1, :].to_broadcast([64, m, 32])
                orr, oi = PW[:, m + 1:2 * m + 1, 0, :], PW[:, m + 1:2 * m + 1, 1, :]
                P_ = ["PW", "tA", "tB"]
                V2(tA[:, 0:m, :], xr, yr, ALU.mult, P_, ["tA"])
                V2(tB[:, 0:m, :], xi, yi, ALU.mult, P_, ["tB"])
                V2(orr, tA[:, 0:m, :], tB[:, 0:m, :], ALU.subtract, P_, ["PW"])
                V2(tA[:, 0:m, :], xr, yi, ALU.mult, P_, ["tA"])
                V2(tB[:, 0:m, :], xi, yr, ALU.mult, P_, ["tB"])
                V2(oi, tA[:, 0:m, :], tB[:, 0:m, :], ALU.add, P_, ["PW"])
            for wi, e_ in ((0, 16), (1, 1)):
                E("vector", "tensor_copy", AL[:, wi, 0, 0, :], PW[:, e_, 0, :], r=["PW"], w=["AL"])
                E("vector", "tensor_copy", AL[:, wi, 0, 1, :], PW[:, e_, 0, :], r=["PW"], w=["AL"])
                E("vector", "tensor_scalar", AL[:, wi, 1, 0, :], PW[:, e_, 1, :], -1.0, None, ALU.mult, r=["PW"], w=["AL"])
                E("vector", "tensor_copy", AL[:, wi, 1, 1, :], PW[:, e_, 1, :], r=["PW"], w=["AL"])

            BB = T("BB", [64, 2, 32, 16])
            t5 = T("t5", [64, 2, 32, 16]); t6 = T("t6", [64, 2, 32, 16])
            creb = cre.unsqueeze(2).to_broadcast([64, 32, 16]); cimb = cim.unsqueeze(2).to_broadcast([64, 32, 16])
            Q_ = KS + ["Bt", "t5", "t6"]
            V2(t5[:, 0], Bt[:, 0], creb, ALU.mult, Q_, ["t5"])
            V2(t6[:, 0], Bt[:, 1], cimb, ALU.mult, Q_, ["t6"])
            V2(BB[:, 0], t5[:, 0], t6[:, 0], ALU.subtract, Q_, ["BB"])
            V2(t5[:, 0], Bt[:, 1], creb, ALU.mult, Q_, ["t5"])
            V2(t6[:, 0], Bt[:, 0], cimb, ALU.mult, Q_, ["t6"])
            V2(BB[:, 0 + 1], t5[:, 0], t6[:, 0], ALU.add, Q_, ["BB"])

            BA = T("BA", [64, 16, 2, 32, 16], BF16)
            CT4 = CT[:].rearrange("p a (g c) -> p a g c", c=16)

            def cprod(dst, src_re, src_im, e0, ne, neg_im, keys_r, key_w):
                pr = PW[:, e0:e0 + ne, 0, :].unsqueeze(3).to_broadcast([64, ne, 32, 16])
                pi_ = PW[:, e0:e0 + ne, 1, :].unsqueeze(3).to_broadcast([64, ne, 32, 16])
                sr = src_re.unsqueeze(1).to_broadcast([64, ne, 32, 16])
                si = src_im.unsqueeze(1).to_broadcast([64, ne, 32, 16])
                R_ = ["PW", "t5", "t6"] + keys_r
                V2(t5[:, 0:ne], sr, pr, ALU.mult, R_, ["t5"])
                V2(t6[:, 0:ne], si, pi_, ALU.mult, R_, ["t6"])
                V2(dst[:, e0:e0 + ne, 0], t5[:, 0:ne], t6[:, 0:ne], ALU.subtract, R_, [key_w])
                V2(t5[:, 0:ne], sr, pi_, ALU.mult, R_, ["t5"])
                V2(t6[:, 0:ne], si, pr, ALU.mult, R_, ["t6"])
                if neg_im:
                    E("vector", "scalar_tensor_tensor", dst[:, e0:e0 + ne, 1], t5[:, 0:ne], -1.0, t6[:, 0:ne],
                      ALU.mult, ALU.subtract, r=R_, w=[key_w])
                else:
                    V2(dst[:, e0:e0 + ne, 1], t5[:, 0:ne], t6[:, 0:ne], ALU.add, R_, [key_w])

            for e0 in range(0, 16, 2):
                cprod(BA, BB[:, 0], BB[:, 1], e0, 2, False, ["BB"], "BA")
            for e0 in range(0, 16, 2):
                cprod(CA, CT4[:, 0], CT4[:, 1], e0, 2, True, ["CT"], "CA")
            cprod(CA, CT4[:, 0], CT4[:, 1], 16, 1, True, ["CT"], "CA")

            pmask = T("pmask", [128, 2])
            pidx = T("pidx", [128, 1])
            E("gpsimd", "iota", pidx[:], pattern=[[0, 1]], base=0, channel_multiplier=1,
              allow_small_or_imprecise_dtypes=True, w=["pidx"])
            pi32 = T("pi32", [128, 2], I32)
            E("vector", "tensor_copy", pi32[:, 0:1], pidx[:], r=["pidx"], w=["pi32"])
            E("vector", "tensor_scalar", pi32[:, 1:2], pi32[:, 0:1], 4, 1, ALU.logical_shift_right, ALU.bitwise_and,
              r=["pi32"], w=["pi32b"])
            E("vector", "tensor_copy", pmask[:, 1:2], pi32[:, 1:2], r=["pi32b"], w=["pmask1"])
            E("vector", "tensor_scalar", pmask[:, 0:1], pmask[:, 1:2], -1.0, 1.0, ALU.mult, ALU.add,
              r=["pmask1"], w=["pmask"])
            nb_ = 0
            for q in range(4):
                for j0 in range(0, 16, 8):
                    wp, wk = wpb[nb_ % 2], f"wpb{nb_ % 2}"
                    nb_ += 1
                    for jj in range(8):
                        j = j0 + jj
                        for ri in range(2):
                            E("tensor", "transpose", wp[:, (jj * 2 + ri) * 64:(jj * 2 + ri + 1) * 64],
                              BA[:, 15 - j, ri, 8 * q:8 * q + 8, :].rearrange("p g c -> p (g c)"), ident_bf[0:64, 0:64],
                              r=["BA", "ident_bf"], w=[wk])
                    src = wp[:].rearrange("p (j r n) -> p j r n", j=8, r=2)
                    for par in range(2):
                        E("vector" if par == 0 else "gpsimd" if False else "vector", "tensor_scalar",
                          WS[:, q, j0:j0 + 8, :, par, :], src, pmask[:, par:par + 1], None, ALU.mult,
                          r=[wk, "pmask", "pmask1"], w=["WS"])
            CTb = T("CTb", [64, 2, 512], BF16)
            E("vector", "tensor_copy", CTb[:, 0, :], CT[:, 0, :], r=["CT"], w=["CTb"])
            E("vector", "tensor_scalar", CTb[:, 1, :], CT[:, 1, :], -1.0, None, ALU.mult, r=["CT"], w=["CTb"])
            bdm = T("bdm", [128, 128])
            io2 = T("io2", [128, 128], I32)
            E("gpsimd", "iota", io2[:], pattern=[[1, 128]], base=0, channel_multiplier=0, w=["io2"])
            E("vector", "tensor_scalar", io2[:], io2[:], 4, None, ALU.logical_shift_right, r=["io2"], w=["io2"])
            gcol = T("gcol", [128, 128])
            E("vector", "tensor_copy", gcol[:], io2[:], r=["io2"], w=["gcol"])
            prow = T("prow", [128, 2], I32)
            E("vector", "tensor_scalar", prow[:, 0:1], pi32[:, 0:1], 4, None, ALU.logical_shift_right, r=["pi32"], w=["prow"])
            prowf = T("prowf", [128, 1])
            E("vector", "tensor_copy", prowf[:], prow[:, 0:1], r=["prow"], w=["prowf"])
            E("vector", "tensor_scalar", bdm[:], gcol[:], prowf[:, 0:1], None, ALU.is_equal, r=["gcol", "prowf"], w=["bdm"])
            bdm4 = bdm[:].unsqueeze(1).to_broadcast([128, 4, 128])
            for q in range(4):
                for d0 in range(0, 16, 4):
                    wp, wk = wps[nb_ % 2], f"wps{nb_ % 2}"
                    nb_ += 1
                    for dd in range(4):
                        for ri in range(2):
                            mm(wp[:, dd * 128:(dd + 1) * 128],
                               BA[:, d0 + dd, ri, 8 * q:8 * q + 8, :].rearrange("p g c -> p (g c)"),
                               CTb[:, ri, q * 128:(q + 1) * 128], start=(ri == 0), stop=(ri == 1),
                               r=["BA", "CTb"], w=[wk])
                    V2(KM[:, q, d0:d0 + 4, :], wp[:].rearrange("p (d n) -> p d n", d=4), bdm4, ALU.mult,
                       [wk, "bdm"], [f"KM{q}"])
                dg = T(f"dg{q}", [128, 128])
                E("vector", "tensor_scalar", dg[:], ident_f[:], dcol[:, q:q + 1], None, ALU.mult,
                  r=["ident_f", "dcol"], w=[f"dg{q}"])
                V2(KM[:, q, 0, :], KM[:, q, 0, :], dg[:], ALU.add, [f"KM{q}", f"dg{q}"], [f"KM{q}"])
            S.barrier()
            S.emit()

        uT = sb("uT", [128, 4, TOK], BF16, pS)
        Hb = sb("Hb", [64, 129, 2, 32], BF16, pS)
        with contextlib.ExitStack() as pU:
            w_u = sb("w_u", [128, 8, 512], BF16, pU)
            for c in range(8):
                dma(w_u[:, c, :], w_in_c[:, c, 1544:2056], writes=[f"w_u{c}"], queue="gpsimd")
            u_ps = [ps(f"u_ps{i}", [128, 512], F32, pU) for i in range(2)]
            nu = 0
            for q in range(4):
                for nb in range(5):
                    blk = slice(nb * 512, min((nb + 1) * 512, TOK))
                    n = blk.stop - blk.start
                    up, uk = u_ps[nu % 2], f"u_ps{nu % 2}"
                    for c in range(8):
                        mm(up[:, 0:n], w_u[:, c, q * 128:(q + 1) * 128], hnT[:, c, blk], start=(c == 0), stop=(c == 7),
                           r=[f"w_u{c}"], w=[uk])
                    if nu % 2 == 0:
                        act(uT[:, q, blk], up[:, 0:n], ACT.Copy, r=[uk], w=[f"uT{q}"])
                    else:
                        E("vector", "tensor_copy", uT[:, q, blk], up[:, 0:n], r=[uk], w=[f"uT{q}"])
                    nu += 1
            S.barrier()
            S.emit()

        E("gpsimd", "memset", Hb[:, 0, :, :], 0.0, w=["Hb0"])
        uTj = [uT[:, q, 0:SEQ].rearrange("p (k j) -> p j k", j=16) for q in range(4)]
        h0b = sb("h0b", [64, 2, 32, NS], BF16, pS)
        with contextlib.ExitStack() as pH:
          s_ps = [ps(f"ss_ps{i}", [128, 512], F32, pH) for i in range(2)]
          fps = ps("fps", [128, 512], F32, pH)
          with contextlib.ExitStack() as pHi:
            Hf = sb("Hf", [64, 128, 2, 32], F32, pHi)
            nsp = 0
            for gp in range(16):
                q, pp = gp // 4, gp % 4
                sp_, sk = s_ps[nsp % 2], f"ss_ps{nsp % 2}"
                nsp += 1
                for ri in range(2):
                    for par in range(2):
                        o = (ri * 2 + par) * 128
                        for j in range(16):
                            op("tensor", lambda e, o=o, sp_=sp_, q=q, pp=pp, j=j, ri=ri, par=par: e.matmul(
                                sp_[0:64, o:o + 128], WS[32 * pp:32 * pp + 32, q, j, ri, par, :],
                                uTj[q][32 * pp:32 * pp + 32, j, :], start=(j == 0), stop=(j == 15),
                                tile_position=(32 * pp, 0)), [f"uT{q}", "WS"], [sk])
                E("vector" if gp % 2 == 0 else "scalar", "tensor_copy" if gp % 2 == 0 else "activation",
                  Hf[:, :, :, 2 * gp:2 * gp + 2].rearrange("p k r g -> p r g k"),
                  sp_[0:64, :].rearrange("p (r g k) -> p r g k", r=2, g=2),
                  *(() if gp % 2 == 0 else (ACT.Copy,)), r=[sk], w=[f"Hf_g{gp}"])
            rt = [sb(f"rt{i}", [64, 2, 32], F32, pHi) for i in range(2)]
            allg = [f"Hf_g{gp}" for gp in range(16)]
            prev_key = allg
            for kc in range(1, 128):
                cur = f"Hk{kc}"
                P_ = Hf[:, kc - 1, :, :]
                V2(rt[0][:], P_, AL[:, 0, 0, :, :], ALU.mult, prev_key + ["AL", "rt0"], ["rt0"])
                V2(rt[1][:, 0, :], P_[:, 1, :], AL[:, 0, 1, 0, :], ALU.mult, prev_key + ["AL", "rt1"], ["rt1"])
                V2(rt[1][:, 1, :], P_[:, 0, :], AL[:, 0, 1, 1, :], ALU.mult, prev_key + ["AL", "rt1"], ["rt1"])
                V2(rt[0][:], rt[0][:], rt[1][:], ALU.add, ["rt0", "rt1"], ["rt0"])
                V2(Hf[:, kc, :, :], Hf[:, kc, :, :], rt[0][:], ALU.add, ["rt0"] + (allg if kc == 1 else []), [cur])
                prev_key = [cur]
            E("vector", "tensor_copy", Hb[:, 1:129, :, :], Hf[:], r=prev_key + allg, w=["Hb"])
            fin = sb("fin", [32, 2, 64], F32, pHi)
            for ri in range(2):
                E("tensor", "transpose", fps[0:32, ri * 64:(ri + 1) * 64], Hf[:, 127, ri, :], ident_f[0:64, 0:64],
                  r=prev_key + ["ident_f"], w=["fps"])
            E("vector", "tensor_copy", fin[:].rearrange("p a n -> p (a n)"), fps[0:32, 0:128], r=["fps"], w=["fin"])
            S.mark_output(dma(hr_p[:, :], fin[:, 0, :], reads=["fin"]))
            S.mark_output(dma(hi_p[:, :], fin[:, 1, :], reads=["fin"]))
            S.barrier()
            S.emit()
          if True:

            h0n = sb("h0n", [128, 2, 64], F32, pH)
            dma(h0n[:, 0, :], st_re_d[:, :], writes=["h0n"])
            dma(h0n[:, 1, :], st_im_d[:, :], writes=["h0n"])
            h0 = sb("h0", [64, 2, NS, 32], F32, pH)
            h1 = sb("h1", [64, 2, NS, 32], F32, pH)
            for ri in range(2):
                E("tensor", "transpose", fps[0:64, 128 + ri * 128:256 + ri * 128], h0n[:, ri, :], ident_f[:],
                  r=["h0n", "ident_f"], w=["fps2"])
            E("vector", "tensor_copy", h0[:].rearrange("p r s g -> p (r s g)"), fps[0:64, 128:384], r=["fps2"], w=["h0"])
            E("vector", "tensor_copy", h0b[:].rearrange("p r g s -> p r s g"), h0[:], r=["h0"], w=["h0b"])
            ssp = s_ps[0]
            for g in range(32):
                q, pp, par = g // 8, (g % 8) // 2, g % 2
                for ri in range(2):
                    o = (ri * 32 + g) * NS
                    op("tensor", lambda e, o=o, q=q, pp=pp, ri=ri, par=par: e.matmul(
                        ssp[0:64, o:o + NS], WS[32 * pp:32 * pp + 32, q, 15, ri, par, :],
                        uT[32 * pp:32 * pp + 32, q, SEQ:SEQ + NS], start=True, stop=True,
                        tile_position=(32 * pp, 0)), [f"uT{q}", "WS"], ["ss_ps0"])
            a1b = [AL[:, 1, i, :, :].unsqueeze(2).to_broadcast([64, 2, NS, 32]) for i in range(2)]
            t7 = sb("t7", [64, 2, NS, 32], F32, pH); t8 = sb("t8", [64, 2, NS, 32], F32, pH)
            V2(t7[:], h0[:], a1b[0], ALU.mult, ["h0", "AL"], ["t7"])
            V2(t8[:, 0], h0[:, 1], a1b[1][:, 0], ALU.mult, ["h0", "AL"], ["t8"])
            V2(t8[:, 1], h0[:, 0], a1b[1][:, 1], ALU.mult, ["h0", "AL"], ["t8"])
            V2(t7[:], t7[:], t8[:], ALU.add, ["t7", "t8"], ["t7"])
            V2(h1[:], t7[:], ssp[0:64, 0:2 * 32 * NS].rearrange("p (r g s) -> p r s g", r=2, g=32), ALU.add,
               ["t7", "ss_ps0"], ["h1"])
            for ri in range(2):
                E("tensor", "transpose", fps[:, 384 + ri * 64:448 + ri * 64], h1[:, ri, :, :].rearrange("p s g -> p (s g)"),
                  ident_f[0:64, 0:64], r=["h1", "ident_f"], w=["fps3"])
            h1o = sb("h1o", [128, 2, 64], F32, pH)
            E("vector", "tensor_copy", h1o[:].rearrange("p a n -> p (a n)"), fps[:, 384:512], r=["fps3"], w=["h1o"])
            S.mark_output(dma(hr_s[:, :], h1o[:, 0, :], reads=["h1o"]))
            S.mark_output(dma(hi_s[:, :], h1o[:, 1, :], reads=["h1o"]))
            S.barrier()
            S.emit()

        with contextlib.ExitStack() as pY:
            y_ps = ps("y_ps", [128, 2048], F32, pY)
            ys_ps = ps("ys_ps", [128, 512], F32, pY)
            t_ps2 = ps("t_ps2", [128, 2560], BF16, pY)
            ysb = [sb(f"ysb{i}", [128, 2048], BF16, pY) for i in range(2)]
            yss = sb("yss", [128, 512], BF16, pY)
            g1 = sb("g1", [128, 2176], F32, pY); g2 = sb("g2", [128, 2176], F32, pY)
            for q in range(4):
                mm(ys_ps[0:NS, q * 128:(q + 1) * 128], uT[:, q, SEQ:SEQ + NS], KM[:, q, 0, :], start=(q == 0), stop=False,
                   r=[f"uT{q}", f"KM{q}"], w=["ys_ps"])
            for g in range(32):
                for ri in range(2):
                    mm(ys_ps[0:NS, g * 16:(g + 1) * 16], h0b[:, ri, g, :], CA[:, 1, ri, g, :], start=False,
                       stop=(g == 31 and ri == 1), r=["h0b", "CA"], w=["ys_ps"])
            E("vector", "memset", yss[:], 0.0, w=["yss"])
            E("vector", "tensor_copy", yss[0:NS, :], ys_ps[0:NS, :], r=["ys_ps"], w=["yss"])
            for q in range(4):
                yk = f"y_ps"
                for j in range(16):
                    for i in range(j + 1):
                        mm(y_ps[:, j * 128:(j + 1) * 128], uTj[q][:, i, :], KM[:, q, j - i, :], start=(i == 0), stop=False,
                           r=[f"uT{q}", f"KM{q}"], w=[yk])
                    for gl in range(8):
                        g = 8 * q + gl
                        for ri in range(2):
                            mm(y_ps[:, j * 128 + gl * 16:j * 128 + (gl + 1) * 16], Hb[:, 0:128, ri, g], CA[:, j + 1, ri, g, :],
                               start=False, stop=(gl == 7 and ri == 1), r=["Hb", "Hb0", "CA"], w=[yk])
                yb, ybk = ysb[q % 2], f"ysb{q % 2}"
                act(yb[:], y_ps[:], ACT.Copy, r=[yk], w=[ybk])
                for j in range(16):
                    E("tensor", "transpose", t_ps2[:, j * 128:(j + 1) * 128], yb[:, j * 128:(j + 1) * 128], ident_bf[:],
                      r=[ybk, "ident_bf"], w=["t_ps2"])
                E("tensor", "transpose", t_ps2[:, 2048:2176], yss[:, q * 128:(q + 1) * 128], ident_bf[:],
                  r=["yss", "ident_bf"], w=["t_ps2"])
                xin = t_ps2[:, 0:2176]
                act(g1[:], xin, ACT.Square, r=["t_ps2"], w=["g1"])
                E("vector", "tensor_scalar", g1[:], g1[:], 0.044715, 1.0, ALU.mult, ALU.add, r=["g1"], w=["g1"])
                E("vector", "tensor_tensor", g1[:], g1[:], xin, ALU.mult, r=["g1", "t_ps2"], w=["g1"])
                act(g2[:], g1[:], ACT.Sigmoid, scale=1.5957691216057308, r=["g1"], w=["g2"])
                E("vector", "tensor_tensor", zsT[:, q, 0:SEQ].rearrange("p (k j) -> p j k", j=16),
                  g2[:, 0:SEQ].rearrange("p (j k) -> p j k", j=16), t_ps2[:, 0:SEQ].rearrange("p (j k) -> p j k", j=16),
                  ALU.mult, r=["g2", "t_ps2"], w=[f"zsT{q}"])
                E("vector", "tensor_tensor", zsT[:, q, SEQ:TOK], g2[:, SEQ:TOK], t_ps2[:, SEQ:TOK], ALU.mult,
                  r=["g2", "t_ps2"], w=[f"zsT{q}"])
            if dbg and dbg[0] == "zs":
                dt_ = sb("dbgt", [128, TOK], F32, pY)
                dv = dbg_t.rearrange("p (a n) -> p a n", a=4)
                for a in range(4):
                    E("vector", "tensor_copy", dt_[:], zsT[:, a, :], r=[f"zsT{a}"], w=["dbgt"])
                    S.mark_output(dma(dv[:, a, :], dt_[:], reads=["dbgt"]))
            S.barrier()
            S.emit()

    pA = contextlib.ExitStack()
    attnT = sb("attnT", [128, 4, TOK], BF16, pA)
    E("gpsimd", "memset", attnT[:, :, SEQ:TOK], 0.0, w=["attnT_s"])
    if stage == 2 or stage >= 4:
      with contextlib.ExitStack() as p2:
        v_bf = sb("v_bf", [128, NT, AW], BF16, p2)
        dma(v_bf[:], v_p.rearrange("(t p) c -> p t c", p=128), writes=["v_bf"], queue="gpsimd")
        w_qk = sb("w_qk", [128, 8, 1024], BF16, p2)
        for c in range(8):
            dma(w_qk[:, c, :], w_in_c[:, c, 0:1024], writes=[f"w_qk{c}"], queue="gpsimd")
        w_f = sb("w_f", [128, 8, 8], BF16, p2)
        dma(w_f[:], w_in_c[:, :, 1536:1544], writes=["w_f"], queue="gpsimd")
        negb8 = sb("negb8", [8, 1], F32, p2)
        dma(negb8[:], b_forget.rearrange("o h -> h o"), writes=["negb8"])
        E("vector", "tensor_scalar", negb8[:], negb8[:], -1.0, None, ALU.mult, r=["negb8"], w=["negb8"])
        ones8 = sb("ones8", [8, SEQ], F32, p2)
        E("gpsimd", "memset", ones8[:], 1.0, w=["ones8"])
        spl = sb("spl", [8, SEQ], F32, p2)
        cs = sb("cs", [8, SEQ], F32, p2)
        e1 = [sb(f"e1_{i}", [8, 512], F32, p2) for i in range(2)]
        c_split = sb("c_split", [8, 3, SEQ], BF16, p2)
        tmpf = [sb(f"tmpf{i}", [8, SEQ], F32, p2) for i in range(3)]
        negc_tok = sb("negc_tok", [128, NT * H], F32, p2)
        with contextlib.ExitStack() as p2a:
            f_ps = [ps(f"f_ps{i}", [128, 512], F32, p2a) for i in range(2)]
            t_ps = ps("t_ps", [128, 512], F32, p2a)
            for nb in range(4):
                fp = f_ps[nb % 2]
                blk = slice(nb * 512, (nb + 1) * 512)
                for c in range(8):
                    mm(fp[0:8, :], w_f[:, c, :], hnT[:, c, blk], start=(c == 0), stop=(c == 7),
                       r=["w_f"], w=[f"f_ps{nb % 2}"])
                act(e1[nb % 2][:], fp[0:8, :], ACT.Exp, scale=-1.0, bias=negb8[:, 0:1],
                    r=[f"f_ps{nb % 2}", "negb8"], w=[f"e1_{nb % 2}"])
                act(spl[:, blk], e1[nb % 2][:], ACT.Ln, bias=1.0, r=[f"e1_{nb % 2}"], w=[f"spl{nb}"])
            E("vector", "tensor_tensor_scan", cs[:], ones8[:], spl[:], 0.0, ALU.mult, ALU.add,
              r=["ones8"] + [f"spl{i}" for i in range(4)], w=["cs"])
            E("vector", "tensor_scalar", c_split[:, 0, :], cs[:], -8.0, None, ALU.mult, r=["cs"], w=["c_hi"])
            E("vector", "tensor_copy", tmpf[0][:], c_split[:, 0, :], r=["c_hi"], w=["tmpf0"])
            E("vector", "scalar_tensor_tensor", tmpf[1][:], cs[:], -8.0, tmpf[0][:], ALU.mult, ALU.subtract,
              r=["cs", "tmpf0"], w=["tmpf1"])
            E("vector", "tensor_copy", c_split[:, 1, :], tmpf[1][:], r=["tmpf1"], w=["c_mid"])
            E("vector", "tensor_copy", tmpf[2][:], c_split[:, 1, :], r=["c_mid"], w=["tmpf2"])
            E("vector", "tensor_tensor", tmpf[0][:], tmpf[1][:], tmpf[2][:], ALU.subtract,
              r=["tmpf1", "tmpf2"], w=["tmpf0"])
            E("vector", "tensor_copy", c_split[:, 2, :], tmpf[0][:], r=["tmpf0"], w=["c_lo"])
            for t in range(NT):
                E("tensor", "transpose", t_ps[:, t * 8:(t + 1) * 8], cs[0:8, t * 128:(t + 1) * 128],
                  ident_f[0:8, 0:8], r=["cs", "ident_f"], w=["t_ps"])
            E("vector", "tensor_copy", negc_tok[:], t_ps[:, 0:NT * H], r=["t_ps"], w=["negc_tok"])
            S.barrier()
            S.emit()

        qa = [sb(f"qa{i}", [128, SEQ], BF16, p2) for i in range(2)]
        ka = [sb(f"ka{i}", [128, SEQ], BF16, p2) for i in range(2)]
        vp = [sb(f"vp{i}", [128, NT, 128], BF16, p2) for i in range(2)]
        onesp = [sb(f"onesp{i}", [128, 128], BF16, p2) for i in range(2)]
        pT = [sb(f"pT{i}", [128, 512], BF16, p2) for i in range(3)]
        rl = [sb(f"rl{i}", [128, 512], F32, p2) for i in range(2)]
        for i in range(2):
            E("gpsimd", "memset", ka[i][64:67, :], 1.0, w=[f"ka{i}"])
            E("gpsimd", "memset", vp[i][:], 0.0, w=[f"vp{i}"])
            E("gpsimd", "memset", onesp[i][:], 0.0, w=[f"onesp{i}"])
            E("gpsimd", "memset", onesp[i][:, i * 64:(i + 1) * 64], 1.0, w=[f"onesp{i}"])
        with contextlib.ExitStack() as p2b:
            pj_ps = [ps(f"pj_ps{i}", [128, 512], F32, p2b) for i in range(2)]
            s_ps = [ps(f"s_ps{i}", [128, 512], F32, p2b) for i in range(2)]
            o_ps = [ps(f"o_ps{i}", [128, 512], F32, p2b) for i in range(2)]
            l_ps = [ps(f"l_ps{i}", [128, 512], F32, p2b) for i in range(2)]
            npj = 0
            nsc = 0
            ngr = 0
            for pr in range(4):
                for hh in range(2):
                    h = 2 * pr + hh
                    for (dst, dk, col0) in ((qa[hh], f"qa{hh}", h * 64), (ka[hh], f"ka{hh}", 512 + h * 64)):
                        for nb in range(4):
                            blk = slice(nb * 512, (nb + 1) * 512)
                            pp, pk = pj_ps[npj % 2], f"pj_ps{npj % 2}"
                            for c in range(8):
                                mm(pp[0:64, :], w_qk[:, c, col0:col0 + 64], hnT[:, c, blk],
                                   start=(c == 0), stop=(c == 7), r=[f"w_qk{c}"], w=[pk])
                            if npj % 2 == 0:
                                act(dst[0:64, blk], pp[0:64, :], ACT.Copy, r=[pk], w=[dk])
                            else:
                                E("vector", "tensor_copy", dst[0:64, blk], pp[0:64, :], r=[pk], w=[dk])
                            npj += 1
                    for i in range(3):
                        dma(qa[hh][64 + i:65 + i, :], c_split[h:h + 1, i, :], reads=["c_hi", "c_mid", "c_lo"],
                            writes=[f"qa{hh}"])
                    E("vector", "tensor_copy", vp[hh][:, :, hh * 64:(hh + 1) * 64], v_bf[:, :, h * 64:(h + 1) * 64],
                      r=["v_bf"], w=[f"vp{hh}"])
                for g in range(4):
                    gb = ngr % 2
                    OP, LP = f"o_ps{gb}", f"l_ps{gb}"
                    first = True
                    for hh in range(2):
                        h = 2 * pr + hh
                        for j in range(4 * g + 4):
                            rr = j - 4 * g
                            c0 = max(rr, 0) * 128
                            sp_, sk = s_ps[nsc % 2], f"s_ps{nsc % 2}"
                            pt, pk = pT[nsc % 3], f"pT{nsc % 3}"
                            nsc += 1
                            mm(sp_[:, c0:512], ka[hh][0:67, j * 128:(j + 1) * 128],
                               qa[hh][0:67, g * 512 + c0:(g + 1) * 512], r=[f"ka{hh}", f"qa{hh}"], w=[sk])
                            act(pt[:, c0:512], sp_[:, c0:512], ACT.Exp, scale=0.125,
                                bias=negc_tok[:, j * H + h:j * H + h + 1], r=[sk, "negc_tok"], w=[pk])
                            if rr >= 0:
                                E("gpsimd", "tensor_tensor", pt[:, c0:c0 + 128], pt[:, c0:c0 + 128], tri[:], ALU.mult,
                                  r=[pk, "tri"], w=[pk])
                            last = (hh == 1 and j == 4 * g + 3)
                            mm(o_ps[gb][:, c0:512], vp[hh][:, j, :], pt[:, c0:512], start=first, stop=last,
                               r=[f"vp{hh}", pk], w=[OP])
                            mm(l_ps[gb][:, c0:512], onesp[hh][:], pt[:, c0:512], start=first, stop=last,
                               r=[f"onesp{hh}", pk], w=[LP])
                            first = False
                    E("vector", "reciprocal", rl[gb][:], l_ps[gb][:], r=[LP], w=[f"rl{gb}"])
                    E("vector", "tensor_tensor", attnT[:, pr, g * 512:(g + 1) * 512], o_ps[gb][:], rl[gb][:], ALU.mult,
                      r=[OP, f"rl{gb}"], w=[f"attnT{pr}_{g}"])
                    ngr += 1
            if dbg and dbg[0] == "attn":
                dt_ = sb("dbgt", [128, SEQ], F32, p2b)
                dv = dbg_t.rearrange("p (a n) -> p a n", a=4)
                for a in range(4):
                    E("vector", "tensor_copy", dt_[:], attnT[:, a, 0:SEQ],
                      r=[f"attnT{a}_{b_}" for b_ in range(4)], w=["dbgt"])
                    S.mark_output(dma(dv[:, a, :], dt_[:], reads=["dbgt"]))
            S.barrier()
            S.emit()

    if stage >= 4:
      with contextlib.ExitStack() as pq:
        ck_d = k.din("cache_k", [NPHYS * 128, AW]); cv_d = k.din("cache_v", [NPHYS * 128, AW])
        cl_d = k.din("cache_logf", [NPHYS, 128 * H])
        pt_d = k.din("page_table", [1, NS * NPAGES], I32)
        NEG = -1.0e30
        w_qs = sb("w_qs", [128, 8, AW], BF16, pq)
        for c in range(8):
            dma(w_qs[:, c, :], w_in_c[:, c, 0:512], writes=["w_qs"], queue="gpsimd", merge=True)
        ptb = sb("ptb", [128, NS * NPAGES], I32, pq)
        dma(ptb[:], pt_d.partition_broadcast(128), writes=["ptb"])
        ptf = sb("ptf", [128, NS * NPAGES], F32, pq)
        pidx = sb("pidx_q", [128, 1], F32, pq)
        E("gpsimd", "iota", pidx[:], pattern=[[0, 1]], base=0, channel_multiplier=1,
          allow_small_or_imprecise_dtypes=True, w=["pidx"])
        E("vector", "tensor_copy", ptf[:], ptb[:], r=["ptb"], w=["ptf"])
        E("vector", "tensor_scalar", ptf[:], ptf[:], 128.0, None, ALU.mult, r=["ptf"], w=["ptf"])
        E("vector", "tensor_scalar", ptf[:], ptf[:], pidx[:, 0:1], None, ALU.add, r=["ptf", "pidx"], w=["ptf"])
        rowi = sb("rowi", [128, NS * NPAGES], I32, pq)
        E("vector", "tensor_copy", rowi[:], ptf[:], r=["ptf"], w=["rowi"])
        mext = sb("mext", [128, 1], F32, pq)
        E("vector", "tensor_scalar", mext[:], pidx[:], 0.0, NEG, ALU.is_gt, ALU.mult, r=["pidx"], w=["mext"])
        pg2 = sb("pg2", [128, 2], I32, pq)
        for m in range(2):
            dma(pg2[:, m:m + 1], pt_d[0:1, m * 128:(m + 1) * 128].rearrange("o n -> n o"), writes=["pg2"])
        ones_f = sb("ones_f", [128, 128], F32, pq)
        E("gpsimd", "memset", ones_f[:], 1.0, w=["ones_f"])
        lt2 = sb("lt2", [128, 128], F32, pq)
        bo2 = sb("bo2", [128, 128], F32, pq)
        E("vector", "tensor_single_scalar", lt2[:], iota_t[:], 0.0, ALU.is_gt, r=["iota_t"], w=["lt2"])
        E("vector", "memset", lt2[0:64, 64:128], 0.0, w=["lt2"])
        E("vector", "memset", bo2[:], 0.0, w=["bo2"])
        E("vector", "memset", bo2[0:64, 0:64], 1.0, w=["bo2"])
        E("vector", "memset", bo2[64:128, 64:128], 1.0, w=["bo2"])
        q_ps = ps("q_ps", [128, 512], F32, pq)
        m_ps = ps("m_ps", [128, 512], F32, pq)
        bt_ps = [ps(f"bt_ps{i}", [128, 512], F32, pq) for i in range(2)]
        o_ps = ps("os_ps", [128, 512], F32, pq)
        qb = sb("qb", [128, NS, AW], F32, pq)
        hb = [sb(f"hb{i}", [128, 8, 128], BF16, pq) for i in range(2)]
        for s_ in range(NS):
            col = SEQ + s_
            E("vector", "tensor_copy", hb[s_ % 2][:], hnT[:, :, col:col + 1].to_broadcast([128, 8, 128]), w=[f"hb{s_ % 2}"])
            for c in range(8):
                mm(q_ps[:], hb[s_ % 2][:, c, :], w_qs[:, c, :], start=(c == 0), stop=(c == 7), r=[f"hb{s_ % 2}", "w_qs"], w=["q_ps"])
            act(qb[:, s_, :], q_ps[:], ACT.Copy, scale=0.125, r=["q_ps"], w=["qb"])
        BT = sb("BT", [128, 2, H, 128], F32, pq)
        lfn = sb("lfn", [128, 2, H], F32, pq)
        for m in range(2):
            for s2 in range(2):
                dma(lfn[s2 * 64:(s2 + 1) * 64, m, :], lf_s[2 * m + s2:2 * m + s2 + 1, :].partition_broadcast(64),
                    writes=["lfn"])
        with contextlib.ExitStack() as pl:
            Lg = [sb(f"Lg{i}", [128, 128 * H], F32, pl) for i in range(2)]
            Cg = [sb(f"Cg{i}", [128, 128 * H], F32, pl) for i in range(2)]
            base = sb("base", [128, 2, H], F32, pl)
            for m in range(2):
                dma(Lg[m][:], cl_d[:, :], reads=["pg2"], writes=[f"Lg{m}"], queue="gpsimd",
                    gather=bass.IndirectOffsetOnAxis(ap=pg2[:, m:m + 1], axis=0))
                for h in range(H):
                    E("vector", "tensor_tensor_scan", Cg[m][:, h:128 * H:H], ones_f[:], Lg[m][:, h:128 * H:H], 0.0,
                      ALU.mult, ALU.add, r=[f"Lg{m}", "ones_f"], w=[f"Cg{m}"])
                tot = Cg[m][:, 127 * H:128 * H]
                mm(m_ps[:, m * 16:m * 16 + 8], lt2[:], tot, r=["lt2", f"Cg{m}"], w=["m_ps"])
                mm(m_ps[:, m * 16 + 8:m * 16 + 16], bo2[:], tot, r=["bo2", f"Cg{m}"], w=["m_ps"])
                E("vector", "tensor_tensor", base[:, m, :], m_ps[:, m * 16 + 8:m * 16 + 16], lfn[:, m, :], ALU.add,
                  r=["m_ps", "lfn"], w=["base"])
                E("vector", "tensor_tensor", base[:, m, :], base[:, m, :], m_ps[:, m * 16:m * 16 + 8], ALU.subtract,
                  r=["m_ps", "base"], w=["base"])
                E("vector", "scalar_tensor_tensor", Cg[m][:].rearrange("p (r h) -> p r h", h=H),
                  Cg[m][:].rearrange("p (r h) -> p r h", h=H), -1.0,
                  base[:, m, :].unsqueeze(1).to_broadcast([128, 128, H]), ALU.mult, ALU.add,
                  r=[f"Cg{m}", "base"], w=[f"Cg{m}"])
                for h4 in range(2):
                    bp = bt_ps[h4]
                    for hh in range(4):
                        h = h4 * 4 + hh
                        E("tensor", "transpose", bp[:, hh * 128:(hh + 1) * 128], Cg[m][:, h:128 * H:H], ident_f[:],
                          r=[f"Cg{m}", "ident_f"], w=[f"bt_ps{h4}"])
                    E("vector", "tensor_copy", BT[:, m, h4 * 4:h4 * 4 + 4, :].rearrange("p h x -> p (h x)"), bp[:],
                      r=[f"bt_ps{h4}"], w=["BT"])
            S.barrier()
            S.emit()
        Sc = sb("Sc", [128, NS, NPAGES + 1, H], F32, pq)
        NKB = 4
        Kb = [sb(f"Kb{i}", [128, 4, AW], F32, pq) for i in range(NKB)]
        Vb = [sb(f"Vb{i}", [128, 4, AW], F32, pq) for i in range(NKB)]
        prod = sb("prod", [128, 4, AW], F32, pq)
        Kx = sb("Kx", [128, NS, AW], F32, pq)
        Vx = sb("Vx", [128, NS, AW], F32, pq)
        E("gpsimd", "memset", Kx[:], 0.0, w=["Kx"])
        E("gpsimd", "memset", Vx[:], 0.0, w=["Vx"])
        dma(Kx[0:1, :, :], k_s.rearrange("(o s) c -> o s c", o=1), writes=["Kx"])
        dma(Vx[0:1, :, :], v_s.rearrange("(o s) c -> o s c", o=1), writes=["Vx"])
        ng = 0
        for s_ in range(NS):
            m, s2 = s_ // 2, s_ % 2
            for g4 in range(NPAGES // 4):
                kb, kk = Kb[ng % NKB], f"Kb{ng % NKB}"
                ng += 1
                for pi in range(4):
                    pg = g4 * 4 + pi
                    dma(kb[:, pi, :], ck_d[:, :], reads=["rowi"], writes=[f"{kk}_{pi}"], queue="gpsimd",
                        gather=bass.IndirectOffsetOnAxis(ap=rowi[:, s_ * NPAGES + pg:s_ * NPAGES + pg + 1], axis=0))
                E("vector", "tensor_tensor", prod[:], kb[:], qb[:, s_, :].unsqueeze(1).to_broadcast([128, 4, AW]), ALU.mult,
                  r=[f"{kk}_{i_}" for i_ in range(4)] + ["qb"], w=["prod"])
                E("vector", "tensor_reduce", Sc[:, s_, g4 * 4:g4 * 4 + 4, :], prod[:].rearrange("p g (h d) -> p g h d", h=H),
                  AX.X, ALU.add, r=["prod"], w=[f"Sc{s_}"])
            E("vector", "tensor_tensor", prod[:, 0, :], Kx[:, s_, :], qb[:, s_, :], ALU.mult, r=["Kx", "qb"], w=["prod"])
            E("vector", "tensor_reduce", Sc[:, s_, NPAGES, :], prod[:, 0, :].rearrange("p (h d) -> p h d", h=H),
              AX.X, ALU.add, r=["prod"], w=[f"Sc{s_}"])
            E("vector", "tensor_scalar", Sc[:, s_, NPAGES, :], Sc[:, s_, NPAGES, :], mext[:, 0:1], None, ALU.add,
              r=[f"Sc{s_}", "mext"], w=[f"Sc{s_}"])
            btv = BT[:, m, :, s2 * 64:(s2 + 1) * 64].rearrange("p h g -> p g h")
            E("vector", "tensor_tensor", Sc[:, s_, 0:NPAGES, :], Sc[:, s_, 0:NPAGES, :], btv, ALU.add,
              r=[f"Sc{s_}", "BT"], w=[f"Sc{s_}"])
        SCK = [f"Sc{i}" for i in range(NS)]
        mx = sb("mx", [128, NS * H], F32, pq)
        E("vector", "tensor_reduce", mx[:].rearrange("p (s h) -> p s h", h=H), Sc[:].rearrange("p s g h -> p s h g"),
          AX.X, ALU.max, r=SCK, w=["mx"])
        E("tensor", "transpose", m_ps[0:32, 128:256], mx[:], ident_f[:], r=["mx", "ident_f"], w=["m_ps"])
        gm = sb("gm", [32, 1], F32, pq)
        E("vector", "tensor_reduce", gm[:], m_ps[0:32, 128:256], AX.X, ALU.max, r=["m_ps"], w=["gm"])
        dgm = sb("dgm", [32, 32], F32, pq)
        E("vector", "tensor_scalar", dgm[:], ident_f[0:32, 0:32], gm[:, 0:1], None, ALU.mult, r=["gm", "ident_f"], w=["dgm"])
        mm(m_ps[:, 256:288], ones_f[0:32, :], dgm[:], r=["ones_f", "dgm"], w=["m_ps2"])
        gmb = sb("gmb", [128, NS * H], F32, pq)
        E("vector", "tensor_copy", gmb[:], m_ps[:, 256:288], r=["m_ps2"], w=["gmb"])
        E("vector", "tensor_tensor", Sc[:], Sc[:],
          gmb[:].rearrange("p (s h) -> p s h", h=H).unsqueeze(2).to_broadcast([128, NS, NPAGES + 1, H]), ALU.subtract,
          r=SCK + ["gmb"], w=["ScA"])
        act(Sc[:].rearrange("p s g h -> p (s g h)"), Sc[:].rearrange("p s g h -> p (s g h)"), ACT.Exp, r=["ScA"], w=["ScP"])
        ls = sb("ls", [128, NS * H], F32, pq)
        E("vector", "tensor_reduce", ls[:].rearrange("p (s h) -> p s h", h=H), Sc[:].rearrange("p s g h -> p s h g"),
          AX.X, ALU.add, r=["ScP"], w=["ls"])
        mm(m_ps[:, 320:352], ones_f[:], ls[:], r=["ones_f", "ls"], w=["m_ps3"])
        rlb = sb("rlb", [128, NS * H], F32, pq)
        E("vector", "reciprocal", rlb[:], m_ps[:, 320:352], r=["m_ps3"], w=["rlb"])
        first = True
        for s_ in range(NS):
            for g4 in range(NPAGES // 4 + 1):
                if g4 < NPAGES // 4:
                    vb, vk = Vb[ng % NKB], f"Vb{ng % NKB}"
                    ng += 1
                    npg = 4
                    for pi in range(4):
                        pg = g4 * 4 + pi
                        dma(vb[:, pi, :], cv_d[:, :], reads=["rowi"], writes=[f"{vk}_{pi}"], queue="gpsimd",
                            gather=bass.IndirectOffsetOnAxis(ap=rowi[:, s_ * NPAGES + pg:s_ * NPAGES + pg + 1], axis=0))
                else:
                    npg = 1
                for pi in range(npg):
                    pg = g4 * 4 + pi
                    src = vb[:, pi, :] if g4 < NPAGES // 4 else Vx[:, s_, :]
                    sk = f"{vk}_{pi}" if g4 < NPAGES // 4 else "Vx"
                    for c4 in range(4):
                        last = (s_ == NS - 1 and g4 == NPAGES // 4 and c4 == 3)
                        o0 = (s_ * 4 + c4) * H
                        mm(o_ps[:, o0:o0 + H], src[:, c4 * 128:(c4 + 1) * 128], Sc[:, s_, pg, :], start=first, stop=last,
                           r=[sk, "ScP"], w=["os_ps"])
                        first = False
        ov = o_ps[:, 0:NS * 4 * H].rearrange("p (s c h) -> p s c h", s=NS, c=4)
        rv = rlb[:].rearrange("p (s h) -> p s h", h=H)
        for pr in range(4):
            for hh in range(2):
                rws = slice(hh * 64, (hh + 1) * 64)
                E("vector", "tensor_tensor", attnT[rws, pr, SEQ:SEQ + NS], ov[rws, :, pr, 2 * pr + hh], rv[rws, :, 2 * pr + hh],
                  ALU.mult, r=["os_ps", "rlb", "attnT_s"], w=["attnT_s"])
        S.barrier()
        S.emit()

    h2_d = nc.dram_tensor("h2_scratch", [TOK, D], F32, kind="Internal").ap()
    uv_d = nc.dram_tensor("uv_scratch", [16384, 2 * D], BF16, kind="Internal").ap()
    if stage >= 5:
      with contextlib.ExitStack() as p3:
        w_ao_d = k.din("w_ao", [AW, D]); w_ga_d = k.din("w_glu_a", [AW, D]); w_gb_d = k.din("w_glu_b", [AW, D])
        w_out_d = k.din("w_out", [D, D])
        w_ao = sb("w_aos", [128, 4, D], BF16, p3); w_ga = sb("w_gla", [128, 4, D], BF16, p3); w_gb = sb("w_glb", [128, 4, D], BF16, p3)
        w_gta = sb("w_gta", [128, 8, D], BF16, p3); w_gts = sb("w_gts", [128, 8, D], BF16, p3)
        w_o = sb("w_o", [128, 8, D], BF16, p3)
        for (dst, src, nm) in ((w_ao, w_ao_d, "w_ao"), (w_ga, w_ga_d, "w_gla"), (w_gb, w_gb_d, "w_glb")):
            sv = src.rearrange("(c p) n -> p c n", p=128)
            for c in range(4):
                dma(dst[:, c, :], sv[:, c, :], writes=[nm], queue="gpsimd", merge=True)
        w_out_c = w_out_d.rearrange("(c p) n -> p c n", p=128)
        for c in range(8):
            dma(w_gta[:, c, :], w_in_c[:, c, 2056:3080], writes=["w_gta"], queue="gpsimd", merge=True)
            dma(w_gts[:, c, :], w_in_c[:, c, 3080:4104], writes=["w_gts"], queue="gpsimd", merge=True)
            dma(w_o[:, c, :], w_out_c[:, c, :], writes=["w_o"], queue="gpsimd", merge=True)
        pu_d = k.din("peer_u", [16384, D]); pv_d = k.din("peer_v", [16384, D])
        uvs = [sb(f"uvs{i}", [128, 2, 2 * D], BF16, p3) for i in range(2)]
        for i in range(64):
            b = i % 2
            rs = slice(i * 256, (i + 1) * 256)
            dma(uvs[b][:, :, 0:D], pu_d[rs, :].rearrange("(p r) d -> p r d", r=2), writes=[f"uvs{b}u"], queue="gpsimd")
            dma(uvs[b][:, :, D:2 * D], pv_d[rs, :].rearrange("(p r) d -> p r d", r=2), writes=[f"uvs{b}v"], queue="gpsimd")
            dma(uv_d[rs, :].rearrange("(p r) d -> p r d", r=2), uvs[b][:], reads=[f"uvs{b}u", f"uvs{b}v"], writes=["uv_d"],
                queue="gpsimd", merge=True)
        mT = sb("mT", [128, 8, 512], BF16, p3)
        sg = [sb(f"sg{i}", [128, 512], F32, p3) for i in range(3)]
        tm = [sb(f"tm{i}", [128, 512], F32, p3) for i in range(2)]
        xr = [sb(f"xr{i}", [128, D], F32, p3) for i in range(2)]
        h2t = [sb(f"h2t{i}", [128, D], F32, p3) for i in range(2)]
        E("vector", "memset", xr[0][:], 0.0, w=["xr0"])
        E("vector", "memset", xr[1][:], 0.0, w=["xr1"])
        b_ps = [ps(f"b_ps{i}", [128, 512], F32, p3) for i in range(5)]
        h_ps = ps("h_ps", [128, 1024], F32, p3)
        ntile = 0
        for nb in (4, 0, 1, 2, 3):
            blk = slice(nb * 512, min((nb + 1) * 512, TOK))
            n = blk.stop - blk.start
            for oc in range(8):
                ocs = slice(oc * 128, (oc + 1) * 128)
                for (pi, wt, src, nk, wk, rk) in ((0, w_ao, attnT, 4, "w_ao", "attnT"), (1, w_ga, zsT, 4, "w_gla", "zsT"),
                                                  (2, w_gb, zsT, 4, "w_glb", "zsT"), (3, w_gta, hnT, 8, "w_gta", "hnT"),
                                                  (4, w_gts, hnT, 8, "w_gts", "hnT")):
                    for c in range(nk):
                        mm(b_ps[pi][:, 0:n], wt[:, c, ocs], src[:, c, blk], start=(c == 0), stop=(c == nk - 1),
                           r=[wk, rk], w=[f"b_ps{pi}"])
                act(sg[0][:, 0:n], b_ps[3][:, 0:n], ACT.Sigmoid, r=["b_ps3"], w=["sg0"])
                act(sg[1][:, 0:n], b_ps[4][:, 0:n], ACT.Sigmoid, r=["b_ps4"], w=["sg1"])
                act(sg[2][:, 0:n], b_ps[2][:, 0:n], ACT.Sigmoid, r=["b_ps2"], w=["sg2"])
                E("vector", "tensor_tensor", tm[0][:, 0:n], b_ps[1][:, 0:n], sg[2][:, 0:n], ALU.mult,
                  r=["b_ps1", "sg2"], w=["tm0"])
                E("vector", "tensor_tensor", tm[0][:, 0:n], tm[0][:, 0:n], sg[1][:, 0:n], ALU.mult, r=["tm0", "sg1"], w=["tm0"])
                E("vector", "tensor_tensor", tm[1][:, 0:n], b_ps[0][:, 0:n], sg[0][:, 0:n], ALU.mult,
                  r=["b_ps0", "sg0"], w=["tm1"])
                E("vector", "tensor_tensor", mT[:, oc, 0:n], tm[0][:, 0:n], tm[1][:, 0:n], ALU.add,
                  r=["tm0", "tm1"], w=[f"mT{oc}"])
            for ti in range(n // 128):
                tt = nb * 4 + ti
                b = ntile % 2
                ntile += 1
                if tt == 16:
                    dma(xr[b][0:NS, :], x_s[:, :], writes=[f"xr{b}"])
                else:
                    dma(xr[b][:], x_p[tt * 128:(tt + 1) * 128, :], writes=[f"xr{b}"])
                for half in range(2):
                    for c in range(8):
                        mm(h_ps[:, half * 512:(half + 1) * 512], mT[:, c, ti * 128:(ti + 1) * 128],
                           w_o[:, c, half * 512:(half + 1) * 512], start=(c == 0), stop=(c == 7),
                           r=[f"mT{c}", "w_o"], w=["h_ps"])
                E("vector", "tensor_tensor", h2t[b][:], h_ps[:], xr[b][:], ALU.add, r=["h_ps", f"xr{b}"], w=[f"h2t{b}"])
                tk = dma(h2_d[tt * 128:(tt + 1) * 128, :], h2t[b][:], reads=[f"h2t{b}"], writes=[f"h2d{tt}"])
                if dbg and dbg[0] == "h2":
                    S.mark_output(dma(dbg_t[tt * 128:(tt + 1) * 128, :], h2t[b][:], reads=[f"h2t{b}"]))
        S.barrier()
        S.emit()

    pA.close()
    pG.close()
    if stage >= 6:
      with contextlib.ExitStack() as p4:
        wq_d = k.din("peer_w_q", [D, 2048]); keys_d = k.din("peer_keys", [16, 128, 128])
        gffn_d = k.din("g_ffn", [1, D]); gple_d = k.din("g_ple", [1, D]); gfin_d = k.din("g_final", [1, D])
        wple_d = k.din("w_ple", [256, D]); wpg_d = k.din("w_ple_gate", [D, D])
        pp_d = k.din("p_p", [SEQ, 256]); ps_d = k.din("p_s", [NS, 256])
        y_p = k.dout("y_p", [SEQ, D]); y_s = k.dout("y_s", [NS, D])

        w_q = sb("w_q", [128, 8, 2048], BF16, p4)
        wq_c = wq_d.rearrange("(c p) n -> p c n", p=128)
        for c in range(8):
            dma(w_q[:, c, :], wq_c[:, c, :], writes=["w_q"], queue="gpsimd", merge=True)
        w_pg = sb("w_pg", [128, 8, D], BF16, p4)
        wpg_c = wpg_d.rearrange("(c p) n -> p c n", p=128)
        for c in range(8):
            dma(w_pg[:, c, :], wpg_c[:, c, :], writes=["w_pg"], queue="gpsimd", merge=True)
        w_pl = sb("w_pl", [128, 2, D], BF16, p4)
        wpl_c = wple_d.rearrange("(c p) n -> p c n", p=128)
        for c in range(2):
            dma(w_pl[:, c, :], wpl_c[:, c, :], writes=["w_pl"], queue="gpsimd", merge=True)
        gv = sb("gv", [128, 3, D], F32, p4)
        for i, gd in enumerate((gffn_d, gple_d, gfin_d)):
            dma(gv[:, i, :], gd.partition_broadcast(128), writes=["gv"], merge=True)
        io16 = sb("io16", [128, 16], F32, p4)
        E("gpsimd", "iota", io16[:], pattern=[[1, 16]], base=0, channel_multiplier=0,
          allow_small_or_imprecise_dtypes=True, w=["io16"])
        keysT = sb("keysT", [128, 16, 128], BF16, p4)
        A_ps = ps("A_ps", [128, 1024], BF16, p4)
        B_ps = ps("B_ps", [128, 512], F32, p4)
        C_ps = ps("C_ps", [128, 2048], F32, p4)
        D_ps = ps("D_ps", [128, 1024], F32, p4)
        with contextlib.ExitStack() as pk:
            kn = sb("kn", [128, 16, 128], BF16, pk)
            dma(kn[:], keys_d.rearrange("a k d -> k a d"), writes=["kn"], queue="gpsimd")
            for half in range(2):
                for a in range(8):
                    E("tensor", "transpose", A_ps[:, a * 128:(a + 1) * 128], kn[:, half * 8 + a, :], ident_bf[:],
                      r=["kn", "ident_bf"], w=["A_ps"])
                E("vector", "tensor_copy", keysT[:, half * 8:half * 8 + 8, :].rearrange("p a k -> p (a k)"), A_ps[:],
                  r=["A_ps"], w=["keysT"])
            S.barrier()
            S.emit()

        NU = 4
        uvb = [sb(f"uvb{i}", [128, 4, 2 * D], BF16, p4) for i in range(NU)]
        h2t = sb("h2t4", [128, D], F32, p4)
        hn2f = sb("hn2f", [128, D], F32, p4)
        hnb = sb("hnb", [128, D], BF16, p4)
        jk = sb("jk4", [128, D], BF16, p4)
        hT = sb("hT4", [128, 8, 128], BF16, p4)
        qpT = sb("qpT", [128, 16, 128], BF16, p4)
        S1 = sb("S1", [128, 4096], F32, p4)
        S2 = sb("S2", [128, 2048], F32, p4)
        vals = sb("vals", [128, 16, 16], F32, p4)
        ixu = sb("ixu", [128, 16, 16], U32, p4)
        ixf = sb("ixf", [128, 16, 16], F32, p4)
        tops = sb("tops", [128, 8, 16], F32, p4)
        posu = sb("posu", [128, 8, 16], U32, p4)
        abi = sb("abi", [128, 2, 8, 16], U32, p4)
        abf = sb("abf", [128, 2, 8, 16], F32, p4)
        ijf = sb("ijf", [128, 2, 8, 16], F32, p4)
        eidf = sb("eidf", [128, 128], F32, p4)
        eidx = sb("eidx", [128, 128], I32, p4)
        gat = sb("gat", [128, 8, 16], F32, p4)
        gsm = sb("gsm", [128, 8, 2], F32, p4)
        scr = sb("scr", [128, 128], F32, p4)
        wsl = sb("wsl", [128, 128], F32, p4)
        gt = [sb(f"gt{i}", [128, 128], F32, p4) for i in range(2)]
        dg4 = [sb(f"dg4_{i}", [128, 128], BF16, p4) for i in range(4)]
        g5 = [sb(f"g5_{i}", [128, 2, 4], F32, p4) for i in range(2)]
        st4 = sb("st4", [128, 3, 4], F32, p4)
        h3 = sb("h3", [128, D], F32, p4)
        gate = sb("gate", [128, D], F32, p4)
        pt_f = sb("pt_f", [128, 256], F32, p4)
        pt_b = sb("pt_b", [128, 256], BF16, p4)
        pT4 = sb("pT4", [128, 2, 128], BF16, p4)
        yo = sb("yo", [128, D], F32, p4)
        E("vector", "memset", pt_f[:], 0.0, w=["pt_f"])
        NEG = -1.0e30

        def rms(src, src_keys, gi, out_f, out_b, tag):
            st = st4[:, gi, :]
            act(jk[:], src, ACT.Square, accum_out=st[:, 0:1], r=src_keys, w=["jk4", f"st4{gi}"])
            E("vector", "tensor_scalar", st[:, 1:2], st[:, 0:1], 1.0 / D, EPS, ALU.mult, ALU.add, r=[f"st4{gi}"], w=[f"st4{gi}"])
            act(st[:, 2:3], st[:, 1:2], ACT.Sqrt, r=[f"st4{gi}"], w=[f"st4{gi}"])
            E("vector", "reciprocal", st[:, 3:4], st[:, 2:3], r=[f"st4{gi}"], w=[f"st4{gi}"])
            if out_f is not None:
                E("vector", "scalar_tensor_tensor", out_f[0], src, st[:, 3:4], gv[:, gi, :], ALU.mult, ALU.mult,
                  r=src_keys + [f"st4{gi}", "gv"], w=[out_f[1]])
            if out_b is not None:
                if out_f is not None:
                    E("gpsimd", "tensor_copy", out_b[0], out_f[0], r=[out_f[1]], w=[out_b[1]])
                else:
                    E("vector", "scalar_tensor_tensor", out_b[0], src, st[:, 3:4], gv[:, gi, :], ALU.mult, ALU.mult,
                      r=src_keys + [f"st4{gi}", "gv"], w=[out_b[1]])

        order = [16] + list(range(16))
        if stage == 6:
            order = [16, 0]
        for tt in order:
            rows = slice(tt * 128, (tt + 1) * 128)
            dma(h2t[:], h2_d[rows, :], reads=[f"h2d{tt}"], writes=["h2t4"])
            rms(h2t[:], ["h2t4"], 0, (hn2f[:], "hn2f"), (hnb[:], "hnb"), "a")
            for c in range(8):
                E("tensor", "transpose", A_ps[:, c * 128:(c + 1) * 128], hnb[:, c * 128:(c + 1) * 128], ident_bf[:],
                  r=["hnb", "ident_bf"], w=["A_ps"])
            act(hT[:].rearrange("p c n -> p (c n)"), A_ps[:], ACT.Copy, r=["A_ps"], w=["hT4"])
            for g4 in range(4):
                for a in range(4):
                    hp = g4 * 4 + a
                    for c in range(8):
                        mm(B_ps[:, a * 128:(a + 1) * 128], w_q[:, c, hp * 128:(hp + 1) * 128], hT[:, c, :],
                           start=(c == 0), stop=(c == 7), r=["w_q", "hT4"], w=["B_ps"])
                if g4 % 2 == 0:
                    act(qpT[:, g4 * 4:g4 * 4 + 4, :].rearrange("p a n -> p (a n)"), B_ps[:], ACT.Copy, r=["B_ps"], w=["qpT"])
                else:
                    E("vector", "tensor_copy", qpT[:, g4 * 4:g4 * 4 + 4, :].rearrange("p a n -> p (a n)"), B_ps[:],
                      r=["B_ps"], w=["qpT"])
            for hp in range(16):
                mm(C_ps[:, hp * 128:(hp + 1) * 128], qpT[:, hp, :], keysT[:, hp, :], r=["qpT", "keysT"], w=["C_ps"])
            sc = S1[:, 0:2048]; sc2 = S1[:, 2048:4096]
            act(sc, C_ps[:], ACT.Copy, r=["C_ps"], w=["S1a"])
            for hp in range(16):
                s_ = sc[:, hp * 128:(hp + 1) * 128]; s2_ = sc2[:, hp * 128:(hp + 1) * 128]
                E("vector", "max", vals[:, hp, 0:8], s_, r=["S1a"], w=["vals"])
                E("vector", "match_replace", s2_, vals[:, hp, 0:8], s_, NEG, r=["S1a", "vals"], w=["S1b"])
                E("vector", "max", vals[:, hp, 8:16], s2_, r=["S1b"], w=["vals"])
                E("vector", "max_index", ixu[:, hp, 0:8], vals[:, hp, 0:8], s_, r=["S1a", "vals"], w=["ixu"])
                E("vector", "max_index", ixu[:, hp, 8:16], vals[:, hp, 8:16], s2_, r=["S1b", "vals"], w=["ixu"])
            E("vector", "tensor_copy", ixf[:], ixu[:], r=["ixu"], w=["ixf"])
            v4 = vals[:].rearrange("p (h q) k -> p h q k", q=2)
            cand = S2[:].rearrange("p (h a b) -> p h a b", h=8, a=16)
            E("vector", "tensor_tensor", cand, v4[:, :, 0, :].unsqueeze(3).to_broadcast([128, 8, 16, 16]),
              v4[:, :, 1, :].unsqueeze(2).to_broadcast([128, 8, 16, 16]), ALU.add, r=["vals"], w=["S2"])
            c2 = S1[:, 0:2048]
            for h in range(8):
                c_ = S2[:, h * 256:(h + 1) * 256]; c2_ = c2[:, h * 256:(h + 1) * 256]
                E("vector", "max", tops[:, h, 0:8], c_, r=["S2"], w=["tops"])
                E("vector", "match_replace", c2_, tops[:, h, 0:8], c_, NEG, r=["S2", "tops", "S1a"], w=["S1a"])
                E("vector", "max", tops[:, h, 8:16], c2_, r=["S1a"], w=["tops"])
                E("vector", "max_index", posu[:, h, 0:8], tops[:, h, 0:8], c_, r=["S2", "tops"], w=["posu"])
                E("vector", "max_index", posu[:, h, 8:16], tops[:, h, 8:16], c2_, r=["S1a", "tops"], w=["posu"])
            E("vector", "tensor_single_scalar", abi[:, 0], posu[:], 4, ALU.logical_shift_right, r=["posu"], w=["abi"])
            E("vector", "tensor_single_scalar", abi[:, 1], posu[:], 15, ALU.bitwise_and, r=["posu"], w=["abi"])
            E("vector", "tensor_copy", abf[:], abi[:], r=["abi"], w=["abf"])
            eq = S1[:, 2048:4096].rearrange("p (h k a) -> p h k a", h=8, k=16)
            ix4 = ixf[:].rearrange("p (h q) k -> p h q k", q=2)
            io_b = io16[:].unsqueeze(1).unsqueeze(1).to_broadcast([128, 8, 16, 16])
            for w_ in range(2):
                E("vector", "tensor_tensor", eq, abf[:, w_].unsqueeze(3).to_broadcast([128, 8, 16, 16]), io_b, ALU.is_equal,
                  r=["abf", "io16", "S1b"], w=["S1b"])
                E("vector", "tensor_tensor", eq, eq, ix4[:, :, w_, :].unsqueeze(2).to_broadcast([128, 8, 16, 16]), ALU.mult,
                  r=["S1b", "ixf"], w=["S1b"])
                E("vector", "tensor_reduce", ijf[:, w_], eq, AX.X, ALU.add, r=["S1b"], w=["ijf"])
            E("vector", "scalar_tensor_tensor", eidf[:].rearrange("p (h k) -> p h k", h=8), ijf[:, 0], 128.0, ijf[:, 1],
              ALU.mult, ALU.add, r=["ijf"], w=["eidf"])
            E("vector", "tensor_copy", eidx[:], eidf[:], r=["eidf"], w=["eidx"])
            E("vector", "tensor_tensor", gat[:], tops[:], tops[:, :, 0:1].to_broadcast([128, 8, 16]), ALU.subtract,
              r=["tops"], w=["gat"])
            act(gat[:], gat[:], ACT.Exp, r=["gat"], w=["gat"])
            E("vector", "tensor_reduce", gsm[:, :, 0], gat[:], AX.X, ALU.add, r=["gat"], w=["gsm"])
            E("vector", "reciprocal", gsm[:, :, 1], gsm[:, :, 0], r=["gsm"], w=["gsm"])
            E("vector", "tensor_tensor", gat[:], gat[:], gsm[:, :, 1:2].to_broadcast([128, 8, 16]), ALU.mult,
              r=["gat", "gsm"], w=["gat"])
            gatf = gat[:].rearrange("p h k -> p (h k)")
            for grp in range(32):
                bi = grp % NU
                buf, bk = uvb[bi], f"uvb{bi}"
                sls = slice(grp * 4, grp * 4 + 4)
                for i in range(4):
                    sl = grp * 4 + i
                    dma(buf[:, i, :], uv_d[:, :], reads=["eidx", "uv_d"], writes=[f"{bk}_{i}"], queue="gpsimd",
                        gather=bass.IndirectOffsetOnAxis(ap=eidx[:, sl:sl + 1], axis=0))
                for i in range(4):
                    sl = grp * 4 + i
                    E("vector", "scalar_tensor_tensor", jk[:], buf[:, i, 0:D], 1.0, hn2f[:], ALU.mult, ALU.mult,
                      accum_out=scr[:, sl:sl + 1], r=[f"{bk}_{i}", "hn2f"], w=["jk4", f"scr{grp}"])
                ga_, gk = g5[grp % 2], f"g5_{grp % 2}"
                E("vector", "tensor_tensor", ga_[:, 0, :], scr[:, sls], scr[:, sls], ALU.mult, r=[f"scr{grp}"], w=[gk])
                E("vector", "tensor_scalar", ga_[:, 0, :], ga_[:, 0, :], 0.044715, 1.0, ALU.mult, ALU.add, r=[gk], w=[gk])
                E("vector", "tensor_tensor", ga_[:, 0, :], ga_[:, 0, :], scr[:, sls], ALU.mult, r=[gk, f"scr{grp}"], w=[gk])
                act(ga_[:, 1, :], ga_[:, 0, :], ACT.Sigmoid, scale=1.5957691216057308, r=[gk], w=[gk])
                E("vector", "tensor_tensor", ga_[:, 1, :], ga_[:, 1, :], scr[:, sls], ALU.mult, r=[gk, f"scr{grp}"], w=[gk])
                E("vector", "tensor_tensor", wsl[:, sls], ga_[:, 1, :], gatf[:, sls], ALU.mult, r=[gk, "gat"], w=[f"wsl{grp}"])
                for i in range(4):
                    sl = grp * 4 + i
                    d3 = sl % 4
                    act(dg4[d3][:], ident_f[:], ACT.Copy, scale=wsl[:, sl:sl + 1], r=["ident_f", f"wsl{grp}"], w=[f"dg4_{d3}"])
                    for half in range(2):
                        mm(D_ps[:, half * 512:(half + 1) * 512], dg4[d3][:], buf[:, i, D + half * 512:D + (half + 1) * 512],
                           start=(sl == 0), stop=(sl == 127), r=[f"dg4_{d3}", f"{bk}_{i}"], w=["D_ps"])
            E("vector", "tensor_tensor", h3[:], D_ps[:], h2t[:], ALU.add, r=["D_ps", "h2t4"], w=["h3"])
            if dbg and dbg[0] == "h3":
                S.mark_output(dma(dbg_t[rows, :], h3[:], reads=["h3"]))
            rms(h3[:], ["h3"], 1, None, (hnb[:], "hnb"), "b")
            for c in range(8):
                E("tensor", "transpose", A_ps[:, c * 128:(c + 1) * 128], hnb[:, c * 128:(c + 1) * 128], ident_bf[:],
                  r=["hnb", "ident_bf"], w=["A_ps"])
            act(hT[:].rearrange("p c n -> p (c n)"), A_ps[:], ACT.Copy, r=["A_ps"], w=["hT4"])
            for half in range(2):
                for c in range(8):
                    mm(C_ps[:, half * 512:(half + 1) * 512], hT[:, c, :], w_pg[:, c, half * 512:(half + 1) * 512],
                       start=(c == 0), stop=(c == 7), r=["hT4", "w_pg"], w=["C_ps"])
            act(gate[:], C_ps[:, 0:1024], ACT.Sigmoid, r=["C_ps"], w=["gate"])
            if tt == 16:
                dma(pt_f[0:NS, :], ps_d[:, :], writes=["pt_f"])
            else:
                dma(pt_f[:], pp_d[rows, :], writes=["pt_f"])
            E("vector", "tensor_copy", pt_b[:], pt_f[:], r=["pt_f"], w=["pt_b"])
            for c in range(2):
                E("tensor", "transpose", A_ps[:, c * 128:(c + 1) * 128], pt_b[:, c * 128:(c + 1) * 128], ident_bf[:],
                  r=["pt_b", "ident_bf"], w=["A_ps"])
            E("vector", "tensor_copy", pT4[:].rearrange("p c n -> p (c n)"), A_ps[:, 0:256], r=["A_ps"], w=["pT4"])
            for half in range(2):
                for c in range(2):
                    mm(C_ps[:, 1024 + half * 512:1024 + (half + 1) * 512], pT4[:, c, :], w_pl[:, c, half * 512:(half + 1) * 512],
                       start=(c == 0), stop=(c == 1), r=["pT4", "w_pl"], w=["C_ps2"])
            E("vector", "tensor_tensor", gate[:], C_ps[:, 1024:2048], gate[:], ALU.mult, r=["C_ps2", "gate"], w=["gate"])
            E("vector", "tensor_tensor", h3[:], h3[:], gate[:], ALU.add, r=["h3", "gate"], w=["h3"])
            rms(h3[:], ["h3"], 2, (yo[:], "yo"), None, "c")
            if tt == 16:
                S.mark_output(dma(y_s[:, :], yo[0:NS, :], reads=["yo"]))
            else:
                S.mark_output(dma(y_p[rows, :], yo[:], reads=["yo"]))
        S.barrier()
        S.emit()

    S.barrier()
    S.emit(final=True)

    k.stack.close()
    return k


def make_in_maps(inputs):
    maps = []
    for c in range(N_CORES):
        m = {
            "x_p": np.ascontiguousarray(inputs["x_prompt"][c]),
            "x_s": np.ascontiguousarray(inputs["x_sample"][4 * c:4 * c + 4, 0]),
            "w_in": np.ascontiguousarray(inputs["w_in"][0]),
            "b_forget": np.ascontiguousarray(inputs["b_forget"]),
            "g_mix": np.ascontiguousarray(inputs["g_mix"]),
            "lam_re": np.ascontiguousarray(inputs["ssm_lam_re"][0]),
            "lam_im": np.ascontiguousarray(inputs["ssm_lam_im"][0]),
            "log_dt": np.ascontiguousarray(inputs["ssm_log_dt"]),
            "b_re": np.ascontiguousarray(inputs["ssm_b_re"][0]),
            "b_im": np.ascontiguousarray(inputs["ssm_b_im"][0]),
            "c_re": np.ascontiguousarray(inputs["ssm_c_re"][0].reshape(512, 64)),
            "c_im": np.ascontiguousarray(inputs["ssm_c_im"][0].reshape(512, 64)),
            "ssm_d": np.ascontiguousarray(inputs["ssm_d"].reshape(512, 1)),
            "st_re": np.ascontiguousarray(inputs["state_ssm_re"][4 * c:4 * c + 4, 0].reshape(128, 64)),
            "st_im": np.ascontiguousarray(inputs["state_ssm_im"][4 * c:4 * c + 4, 0].reshape(128, 64)),
            "w_ao": np.ascontiguousarray(inputs["w_attn_out"][0]),
            "w_glu_a": np.ascontiguousarray(inputs["w_glu_a"][0]),
            "w_glu_b": np.ascontiguousarray(inputs["w_glu_b"][0]),
            "w_out": np.ascontiguousarray(inputs["w_out"][0]),
            "peer_w_q": np.ascontiguousarray(inputs["peer_w_q"][0]),
            "peer_keys": np.ascontiguousarray(inputs["peer_keys"][0].reshape(16, 128, 128)),
            "peer_u": inputs["peer_u"][0],
            "peer_v": inputs["peer_v"][0],
            "g_ffn": np.ascontiguousarray(inputs["g_ffn"]),
            "g_ple": np.ascontiguousarray(inputs["g_ple"]),
            "g_final": np.ascontiguousarray(inputs["g_final"].reshape(1, D)),
            "w_ple": np.ascontiguousarray(inputs["w_ple"][0]),
            "w_ple_gate": np.ascontiguousarray(inputs["w_ple_gate"][0]),
            "p_p": np.ascontiguousarray(inputs["p_prompt"][0, c]),
            "p_s": np.ascontiguousarray(inputs["p_sample"][0, 4 * c:4 * c + 4, 0]),
            "cache_k": inputs["cache_k"].reshape(NPHYS * 128, AW),
            "cache_v": inputs["cache_v"].reshape(NPHYS * 128, AW),
            "cache_logf": inputs["cache_logf"].reshape(NPHYS, 128 * H),
            "page_table": np.ascontiguousarray(inputs["page_table"][4 * c:4 * c + 4].reshape(1, NS * NPAGES)),
        }
        maps.append(m)
    return maps


def run(inputs, stage=99, trace=False, dbg=None):
    k = build(stage, dbg)
    names = set(k.io.keys())
    maps = [{n: v for n, v in m.items() if n in names} for m in make_in_maps(inputs)]
    res = run_bass_kernel_spmd(k.nc, maps, core_ids=list(range(N_CORES)), trace=trace)
    return res


def kernel(**inputs):
    inputs = {n: np.asarray(v) for n, v in inputs.items()}
    res = run(inputs).results
    B, DB = 8, 32
    f32 = np.float32

    def cat(name, shape):
        return np.stack([np.asarray(r[name]) for r in res], 0).reshape(shape).astype(f32, copy=False)

    return (cat("y_p", (B, SEQ, D)), cat("y_s", (DB, 1, D)),
            cat("k_p", (B, 1, SEQ, H, HD)), cat("v_p", (B, 1, SEQ, H, HD)), cat("lf_p", (B, 1, SEQ, H)),
            cat("hr_p", (B, 1, 32, 64)), cat("hi_p", (B, 1, 32, 64)),
            cat("k_s", (DB, 1, 1, H, HD)), cat("v_s", (DB, 1, 1, H, HD)), cat("lf_s", (DB, 1, 1, H)),
            cat("hr_s", (DB, 1, 32, 64)), cat("hi_s", (DB, 1, 32, 64)))
```

```python
import contextlib
import numpy as np
import concourse.bass as bass
import concourse.mybir as mybir
from concourse.bass_utils import run_bass_kernel_spmd

F32 = mybir.dt.float32
BF16 = mybir.dt.bfloat16
I32 = mybir.dt.int32
U32 = mybir.dt.uint32
ALU = mybir.AluOpType
ACT = mybir.ActivationFunctionType
AX = mybir.AxisListType

N_CORES = 8
NEEDED = ["x_prompt", "x_sample", "w_in", "b_forget", "g_mix", "ssm_lam_re", "ssm_lam_im", "ssm_log_dt", "ssm_b_re", "ssm_b_im", "ssm_c_re", "ssm_c_im", "ssm_d", "state_ssm_re", "state_ssm_im", "w_attn_out", "w_glu_a", "w_glu_b", "w_out", "peer_w_q", "peer_keys", "peer_u", "peer_v", "g_ffn", "g_ple", "g_final", "w_ple", "w_ple_gate", "p_prompt", "p_sample", "cache_k", "cache_v", "cache_logf", "page_table"]
D = 1024
SEQ = 2048
NT = 16
NTT = 17
TOK = NTT * 128
H = 8
HD = 64
AW = 512
IN_W = 4104
NS = 4
NPAGES = 64
NPHYS = 2560
EPS = 1e-6

COMPUTE = ("tensor", "vector", "scalar", "gpsimd")
SEM_SPAN = 30000
N_DMA_SEMS = 16


class Sched:
    def __init__(self, nc, stack):
        self.nc = nc
        self.stack = stack
        self.streams = {e: [] for e in ("tensor", "vector", "scalar", "gpsimd", "sync")}
        self.cnt = {e: 0 for e in self.streams}
        self.esems = {e: [] for e in COMPUTE}
        self.dq = ("sync", "gpsimd")
        self.dsems = [stack.enter_context(nc.semaphore(f"dq{i}")) for i in range(2 * N_DMA_SEMS)]
        self.dcnt = [0] * (2 * N_DMA_SEMS)
        self.dnext = {"sync": 0, "gpsimd": 0}
        self.waited = {}
        self.last_w = {}
        self.readers = {}
        self.n_inst = 0
        self.out_tokens = []

    def _sem_of(self, tok):
        if tok[0] == "e":
            _, eng, n = tok
            idx = (n - 1) // SEM_SPAN
            while len(self.esems[eng]) <= idx:
                self.esems[eng].append(self.stack.enter_context(
                    self.nc.semaphore(f"s_{eng}_{len(self.esems[eng])}")))
            return ("e", eng, idx), self.esems[eng][idx], n - idx * SEM_SPAN
        _, si, val = tok
        return ("d", si), self.dsems[si], val

    def _need_wait(self, eng, tok, same_ok):
        if tok is None:
            return None
        if tok[0] == "e" and tok[1] == eng and same_ok:
            return None
        key, sem, val = self._sem_of(tok)
        if self.waited.get((eng, key), 0) >= val:
            return None
        self.waited[(eng, key)] = val
        return (sem, val)

    def _deps(self, eng, reads, writes, merge=False):
        waits = []
        pe = eng == "tensor"
        for k in reads:
            for t in self.last_w.get(k, []):
                w = self._need_wait(eng, t, same_ok=pe)
                if w:
                    waits.append(w)
        for k in writes:
            if not merge:
                for t in self.last_w.get(k, []):
                    w = self._need_wait(eng, t, same_ok=pe)
                    if w:
                        waits.append(w)
            for r in self.readers.get(k, []):
                w = self._need_wait(eng, r, same_ok=pe)
                if w:
                    waits.append(w)
        return waits

    def _commit(self, tok, reads, writes, merge=False):
        for k in reads:
            self.readers.setdefault(k, []).append(tok)
        for k in writes:
            if merge:
                self.last_w.setdefault(k, []).append(tok)
            else:
                self.last_w[k] = [tok]
                self.readers[k] = []

    def op(self, eng, fn, reads=(), writes=()):
        waits = self._deps(eng, reads, writes)
        self.cnt[eng] += 1
        tok = ("e", eng, self.cnt[eng])
        _, sem, _ = self._sem_of(tok)
        self.streams[eng].append((waits, fn, sem, 1))
        self._commit(tok, reads, writes)
        self.n_inst += 1
        return tok

    def dma(self, out, in_, reads=(), writes=(), queue="sync", gather=None, merge=False, **kw):
        eng = queue
        waits = self._deps(eng, reads, writes, merge)
        si = self.dq.index(eng) * N_DMA_SEMS + self.dnext[eng]
        self.dnext[eng] = (self.dnext[eng] + 1) % N_DMA_SEMS
        if self.dcnt[si] > 0:
            w = self._need_wait(eng, ("d", si, self.dcnt[si]), same_ok=False)
            if w:
                waits.append(w)
        self.dcnt[si] += 16
        tok = ("d", si, self.dcnt[si])
        if gather is None:
            fn = lambda e, o=out, i=in_, kw=kw: e.dma_start(out=o, in_=i, **kw)
        else:
            fn = lambda e, o=out, i=in_, g=gather, kw=kw: e.indirect_dma_start(
                out=o, out_offset=None, in_=i, in_offset=g, **kw)
        self.streams[eng].append((waits, fn, self.dsems[si], 16))
        self._commit(tok, reads, writes, merge)
        self.n_inst += 1
        return tok

    def barrier(self):
        toks = [("e", o, self.cnt[o]) for o in COMPUTE if self.cnt[o] > 0]
        toks += [("d", si, self.dcnt[si]) for si in range(2 * N_DMA_SEMS) if self.dcnt[si] > 0]
        for eng in self.streams:
            waits = []
            for t in toks:
                w = self._need_wait(eng, t, same_ok=True)
                if w:
                    waits.append(w)
            if waits:
                self.streams[eng].append((waits, None, None, 0))
        self.last_w = {}
        self.readers = {}

    def mark_output(self, tok):
        self.out_tokens.append(tok)

    def emit(self, final=False):
        nc = self.nc
        fin = []
        if final:
            for tok in self.out_tokens:
                w = self._need_wait("sync", tok, same_ok=False)
                if w:
                    fin.append(w)
        streams = self.streams
        self.streams = {e: [] for e in streams}

        def run(e, name):
            for waits, fn, sem, inc in streams[name]:
                for (s, v) in waits:
                    e.wait_ge(s, v)
                if fn is not None:
                    fn(e).then_inc(sem, inc)
            if name == "sync":
                for (s, v) in fin:
                    e.wait_ge(s, v)

        with nc.Block() as block:
            @block.sync
            def _(e):
                run(e, "sync")

            @block.tensor
            def _(e):
                run(e, "tensor")

            @block.vector
            def _(e):
                run(e, "vector")

            @block.scalar
            def _(e):
                run(e, "scalar")

            @block.gpsimd
            def _(e):
                run(e, "gpsimd")


class K:
    def __init__(self, stage=99):
        self.stage = stage
        self.nc = bass.Bass("TRN2", target_bir_lowering=False)
        self.stack = contextlib.ExitStack()
        self.S = Sched(self.nc, self.stack)
        self.io = {}

    def din(self, name, shape, dt=F32):
        self.io[name] = self.nc.dram_tensor(name, list(shape), dt, kind="ExternalInput").ap()
        return self.io[name]

    def dout(self, name, shape, dt=F32):
        self.io[name] = self.nc.dram_tensor(name, list(shape), dt, kind="ExternalOutput").ap()
        return self.io[name]

    def sb(self, name, shape, dt=F32, stack=None):
        return (stack or self.stack).enter_context(self.nc.sbuf_tensor(name, list(shape), dt))

    def ps(self, name, shape, dt=F32, stack=None):
        return (stack or self.stack).enter_context(self.nc.psum_tensor(name, list(shape), dt))


def build(stage=99, dbg=None):
    k = K(stage)
    nc, S = k.nc, k.S
    op, dma = S.op, S.dma
    sb, ps = k.sb, k.ps

    def E(eng, method, *args, r=(), w=(), **kw):
        return op(eng, lambda e: getattr(e, method)(*args, **kw), r, w)

    def mm(out, lhsT, rhs, start=True, stop=True, r=(), w=()):
        return op("tensor", lambda e: e.matmul(out, lhsT, rhs, start=start, stop=stop), r, w)

    def act(out, in_, func, r=(), w=(), **kw):
        return op("scalar", lambda e: e.activation(out, in_, func, **kw), r, w)

    x_p = k.din("x_p", [SEQ, D])
    x_s = k.din("x_s", [NS, D])
    w_in = k.din("w_in", [D, IN_W])
    b_forget = k.din("b_forget", [1, H])
    g_mix = k.din("g_mix", [1, D])

    k_p = k.dout("k_p", [SEQ, AW])
    v_p = k.dout("v_p", [SEQ, AW])
    lf_p = k.dout("lf_p", [SEQ, H])
    k_s = k.dout("k_s", [NS, AW])
    v_s = k.dout("v_s", [NS, AW])
    lf_s = k.dout("lf_s", [NS, H])
    dbg_t = k.dout("dbg", list(dbg[1])) if dbg else None
    w_in_c = w_in.rearrange("(c p) n -> p c n", p=128)
    h2_d = nc.dram_tensor("h2_scratch", [TOK, D], F32, kind="Internal").ap()
    uv_d = nc.dram_tensor("uv_scratch", [16384, 2 * D], BF16, kind="Internal").ap()

    ident_bf = sb("ident_bf", [128, 128], BF16)
    ident_f = sb("ident_f", [128, 128], F32)
    iota_t = sb("iota_t", [128, 128], F32)
    tri = sb("tri", [128, 128], BF16)
    E("gpsimd", "iota", iota_t[:], pattern=[[1, 128]], base=0, channel_multiplier=-1,
      allow_small_or_imprecise_dtypes=True, w=["iota_t"])
    E("vector", "tensor_single_scalar", ident_f[:], iota_t[:], 0.0, ALU.is_equal, r=["iota_t"], w=["ident_f"])
    E("vector", "tensor_copy", ident_bf[:], ident_f[:], r=["ident_f"], w=["ident_bf"])
    E("vector", "tensor_single_scalar", tri[:], iota_t[:], 0.0, ALU.is_ge, r=["iota_t"], w=["tri"])

    pG = contextlib.ExitStack()
    hnT = sb("hnT", [128, 8, TOK], BF16, pG)
    lft = sb("lft", [128, NTT, 3 * H], F32, pG)
    zsT = sb("zsT", [128, 4, TOK], BF16, pG)

    with contextlib.ExitStack() as p1:
        gmix_b = sb("gmix_b", [128, D], F32, p1)
        dma(gmix_b[:], g_mix.partition_broadcast(128), writes=["gmix_b"])
        bf_b = sb("bf_b", [128, H], F32, p1)
        dma(bf_b[:], b_forget.partition_broadcast(128), writes=["bf_b"])
        w_kvf = sb("w_kvf", [128, 8, 1032], BF16, p1)
        for c in range(8):
            dma(w_kvf[:, c, :], w_in_c[:, c, 512:1544], writes=[f"w_kvf{c}"], queue="gpsimd")
        pu_d = k.din("peer_u", [16384, D]); pv_d = k.din("peer_v", [16384, D])
        uvs = [sb(f"uvs{i}", [128, 2, 2 * D], BF16, p1) for i in range(2)]
        for i in range(64):
            b = i % 2
            rs = slice(i * 256, (i + 1) * 256)
            dma(uvs[b][:, :, 0:D], pu_d[rs, :].rearrange("(p r) d -> p r d", r=2), writes=[f"uvs{b}u"], queue="gpsimd")
            dma(uvs[b][:, :, D:2 * D], pv_d[rs, :].rearrange("(p r) d -> p r d", r=2), writes=[f"uvs{b}v"], queue="gpsimd")
            dma(uv_d[rs, :].rearrange("(p r) d -> p r d", r=2), uvs[b][:], reads=[f"uvs{b}u", f"uvs{b}v"], writes=["uv_d"],
                queue="gpsimd", merge=True)
        NB = 2
        xt = [sb(f"xt{i}", [128, D], F32, p1) for i in range(NB)]
        hn = [sb(f"hn{i}", [128, D], BF16, p1) for i in range(NB)]
        junk = [sb(f"junk{i}", [128, D], BF16, p1) for i in range(NB)]
        kv = [sb(f"kv{i}", [128, 1024], F32, p1) for i in range(NB)]
        stat = sb("stat", [128, NTT, 4], F32, p1)
        tp_ps = [ps(f"tp_ps{i}", [128, 1024], BF16, p1) for i in range(2)]
        kv_ps = ps("kv_ps", [128, 1536], F32, p1)

        E("vector", "memset", xt[0][:], 0.0, w=["xt0"])
        for t in range(NTT):
            b = t % NB
            X, HN, J, KV = f"xt{b}", f"hn{b}", f"junk{b}", f"kv{b}"
            TP, KP = f"tp_ps{t % 2}", "kv_ps"
            tt = (t - 1) if t > 0 else 16
            cols = slice(tt * 128, (tt + 1) * 128)
            if tt == 16:
                dma(xt[b][0:NS, :], x_s[:, :], writes=[X])
            else:
                dma(xt[b][:], x_p[cols, :], writes=[X])
            st = stat[:, tt, :]
            act(junk[b][:], xt[b][:], ACT.Square, accum_out=st[:, 0:1], r=[X], w=[J, f"st{tt}a"])
            E("vector", "tensor_scalar", st[:, 1:2], st[:, 0:1], 1.0 / D, EPS, ALU.mult, ALU.add,
              r=[f"st{tt}a"], w=[f"st{tt}b"])
            act(st[:, 2:3], st[:, 1:2], ACT.Sqrt, r=[f"st{tt}b"], w=[f"st{tt}c"])
            E("vector", "reciprocal", st[:, 3:4], st[:, 2:3], r=[f"st{tt}c"], w=[f"st{tt}d"])
            E("vector", "scalar_tensor_tensor", hn[b][:], xt[b][:], st[:, 3:4], gmix_b[:], ALU.mult, ALU.mult,
              r=[X, f"st{tt}d", "gmix_b"], w=[HN])
            tp = tp_ps[t % 2]
            for c in range(8):
                E("tensor", "transpose", tp[:, c * 128:(c + 1) * 128], hn[b][:, c * 128:(c + 1) * 128],
                  ident_bf[:], r=[HN, "ident_bf"], w=[TP])
            act(hnT[:, :, cols], tp[:].rearrange("p (c n) -> p c n", c=8), ACT.Copy, r=[TP], w=[f"hnT{tt}"])
            for (lo, hi) in ((0, 512), (512, 1024), (1024, 1032)):
                for c in range(8):
                    mm(kv_ps[:, lo:hi], hnT[:, c, cols], w_kvf[:, c, lo:hi], start=(c == 0), stop=(c == 7),
                       r=[f"hnT{tt}", f"w_kvf{c}"], w=[KP])
            E("vector", "tensor_copy", kv[b][:], kv_ps[:, 0:1024], r=[KP], w=[KV])
            l3 = lft[:, tt, :]
            E("vector", "tensor_tensor", l3[:, 0:H], kv_ps[:, 1024:1032], bf_b[:], ALU.add,
              r=[KP, "bf_b"], w=[f"lf{tt}a"])
            act(l3[:, H:2 * H], l3[:, 0:H], ACT.Exp, scale=-1.0, r=[f"lf{tt}a"], w=[f"lf{tt}b"])
            act(l3[:, 2 * H:3 * H], l3[:, H:2 * H], ACT.Ln, bias=1.0, r=[f"lf{tt}b"], w=[f"lf{tt}c"])
            E("vector", "tensor_scalar", l3[:, 0:H], l3[:, 2 * H:3 * H], -1.0, None, ALU.mult,
              r=[f"lf{tt}c"], w=[f"lf{tt}d"])
            if tt == 16:
                S.mark_output(dma(k_s[:, :], kv[b][0:NS, 0:512], reads=[KV]))
                S.mark_output(dma(v_s[:, :], kv[b][0:NS, 512:1024], reads=[KV]))
                S.mark_output(dma(lf_s[:, :], l3[0:NS, 0:H], reads=[f"lf{tt}d"]))
            else:
                S.mark_output(dma(k_p[cols, :], kv[b][:, 0:512], reads=[KV]))
                S.mark_output(dma(v_p[cols, :], kv[b][:, 512:1024], reads=[KV]))
                S.mark_output(dma(lf_p[cols, :], l3[:, 0:H], reads=[f"lf{tt}d"]))
        S.barrier()
        S.emit()

    if stage >= 3:
      with contextlib.ExitStack() as pS:
        lam_re_d = k.din("lam_re", [32, 64]); lam_im_d = k.din("lam_im", [32, 64])
        log_dt_d = k.din("log_dt", [1, 32])
        b_re_d = k.din("b_re", [32, 64, 16]); b_im_d = k.din("b_im", [32, 64, 16])
        c_re_d = k.din("c_re", [512, 64]); c_im_d = k.din("c_im", [512, 64])
        ssm_d_d = k.din("ssm_d", [512, 1])
        st_re_d = k.din("st_re", [128, 64]); st_im_d = k.din("st_im", [128, 64])
        hr_p = k.dout("hr_p", [32, 64]); hi_p = k.dout("hi_p", [32, 64])
        hr_s = k.dout("hr_s", [128, 64]); hi_s = k.dout("hi_s", [128, 64])

        WS = sb("WS", [128, 4, 16, 2, 2, 64], BF16, pS)
        CA = sb("CA", [64, 17, 2, 32, 16], BF16, pS)
        KM = sb("KM", [128, 4, 16, 128], BF16, pS)
        AL = sb("AL", [64, 2, 2, 2, 32], F32, pS)
        with contextlib.ExitStack() as pW:
            def T(name, shape, dt=F32):
                return sb(name, shape, dt, pW)
            TWO_PI = 2.0 * np.pi
            lnat = T("lnat", [32, 2, 64])
            dma(lnat[:, 0, :], lam_re_d[:, :], writes=["lnat"])
            dma(lnat[:, 1, :], lam_im_d[:, :], writes=["lnat"])
            ldt = T("ldt", [64, 32])
            dma(ldt[:], log_dt_d.partition_broadcast(64), writes=["ldt"])
            Bt = T("Bt", [64, 2, 32, 16])
            dma(Bt[:, 0, :, :], b_re_d.rearrange("g n c -> n g c"), writes=["Bt"])
            dma(Bt[:, 1, :, :], b_im_d.rearrange("g n c -> n g c"), writes=["Bt"])
            Cnat = T("Cnat", [128, 2, 4, 64])
            dma(Cnat[:, 0, :, :], c_re_d.rearrange("(q p) n -> p q n", p=128), writes=["Cnat"])
            dma(Cnat[:, 1, :, :], c_im_d.rearrange("(q p) n -> p q n", p=128), writes=["Cnat"])
            dcol = T("dcol", [128, 4])
            for q in range(4):
                dma(dcol[:, q:q + 1], ssm_d_d[q * 128:(q + 1) * 128, :], writes=["dcol"])
            wps = [ps(f"wps{i}", [128, 512], F32, pW) for i in range(2)]
            wpb = [ps(f"wpb{i}", [128, 1024], BF16, pW) for i in range(2)]
            lam = T("lam", [64, 2, 32])
            for ri in range(2):
                E("tensor", "transpose", wps[0][0:64, ri * 32:(ri + 1) * 32], lnat[:, ri, :], ident_f[0:32, 0:32],
                  r=["lnat", "ident_f"], w=["wps0"])
            E("vector", "tensor_copy", lam[:].rearrange("p a g -> p (a g)"), wps[0][0:64, 0:64], r=["wps0"], w=["lam"])
            CT = T("CT", [64, 2, 512])
            for ri in range(2):
                for q in range(4):
                    E("tensor", "transpose", wps[1][0:64, q * 128:(q + 1) * 128], Cnat[:, ri, q, :], ident_f[:],
                      r=["Cnat", "ident_f"], w=["wps1"])
                E("vector", "tensor_copy", CT[:, ri, :], wps[1][0:64, :], r=["wps1"], w=["CT"])
            sc = T("sc", [64, 16, 32])
            _n = [0]

            def V2(out, a, b, o, r, w):
                E("vector", "tensor_tensor", out, a, b, o, r=r, w=w)

            lr, li = lam[:, 0, :], lam[:, 1, :]
            dt_, lrdt, mag, ang, yv, kk, tmp, rr, sn, cs_, are, aim, den, nre, cre, cim = [sc[:, i, :] for i in range(16)]
            KS = ["sc"]
            act(dt_, ldt[:], ACT.Exp, r=["ldt"], w=KS)
            V2(lrdt, lr, dt_, ALU.mult, ["lam"] + KS, KS)
            act(mag, lrdt, ACT.Exp, r=KS, w=KS)
            V2(ang, li, dt_, ALU.mult, ["lam"] + KS, KS)
            E("vector", "tensor_scalar", yv, ang, 1.0 / TWO_PI, None, ALU.mult, r=KS, w=KS)
            E("vector", "tensor_scalar", kk, yv, 0.5, None, ALU.is_ge, r=KS, w=KS)
            for m in range(2, 8):
                E("vector", "tensor_scalar", tmp, yv, m - 0.5, None, ALU.is_ge, r=KS, w=KS)
                V2(kk, kk, tmp, ALU.add, KS, KS)
            for m in range(1, 3):
                E("vector", "tensor_scalar", tmp, yv, -(m - 0.5), None, ALU.is_le, r=KS, w=KS)
                V2(kk, kk, tmp, ALU.subtract, KS, KS)
            V2(rr, yv, kk, ALU.subtract, KS, KS)
            act(sn, rr, ACT.Sin, scale=TWO_PI, r=KS, w=KS)
            act(tmp, rr, ACT.Abs, r=KS, w=KS)
            hpi = T("hpi", [64, 1])
            E("vector", "memset", hpi[:], float(np.pi / 2), w=["hpi"])
            act(cs_, tmp, ACT.Sin, scale=-TWO_PI, bias=hpi[:, 0:1], r=KS + ["hpi"], w=KS)
            V2(are, mag, cs_, ALU.mult, KS, KS)
            V2(aim, mag, sn, ALU.mult, KS, KS)
            V2(den, lr, lr, ALU.mult, ["lam"] + KS, KS)
            V2(tmp, li, li, ALU.mult, ["lam"] + KS, KS)
            V2(den, den, tmp, ALU.add, KS, KS)
            E("vector", "reciprocal", den, den, r=KS, w=KS)
            E("vector", "tensor_scalar", nre, are, -1.0, None, ALU.add, r=KS, w=KS)
            V2(cre, nre, lr, ALU.mult, ["lam"] + KS, KS)
            V2(tmp, aim, li, ALU.mult, ["lam"] + KS, KS)
            V2(cre, cre, tmp, ALU.add, KS, KS)
            V2(cre, cre, den, ALU.mult, KS, KS)
            V2(cim, aim, lr, ALU.mult, ["lam"] + KS, KS)
            V2(tmp, nre, li, ALU.mult, ["lam"] + KS, KS)
            V2(cim, cim, tmp, ALU.subtract, KS, KS)
            V2(cim, cim, den, ALU.mult, KS, KS)

            PW = T("PW", [64, 17, 2, 32])
            tA = T("tA", [64, 8, 32]); tB = T("tB", [64, 8, 32])
            E("vector", "memset", PW[:, 0, 0, :], 1.0, w=["PW"])
            E("vector", "memset", PW[:, 0, 1, :], 0.0, w=["PW"])
            E("vector", "tensor_copy", PW[:, 1, 0, :], are, r=KS, w=["PW"])
            E("vector", "tensor_copy", PW[:, 1, 1, :], aim, r=KS, w=["PW"])
            for m in (1, 2, 4, 8):
                xr, xi = PW[:, 1:m + 1, 0, :], PW[:, 1:m + 1, 1, :]
                yr = PW[:, m:m + 1, 0, :].to_broadcast([64, m, 32]); yi = PW[:, m:m + 1, 1, :].to_broadcast([64, m, 32])
                orr, oi = PW[:, m + 1:2 * m + 1, 0, :], PW[:, m + 1:2 * m + 1, 1, :]
                P_ = ["PW", "tA", "tB"]
                V2(tA[:, 0:m, :], xr, yr, ALU.mult, P_, ["tA"])
                V2(tB[:, 0:m, :], xi, yi, ALU.mult, P_, ["tB"])
                V2(orr, tA[:, 0:m, :], tB[:, 0:m, :], ALU.subtract, P_, ["PW"])
                V2(tA[:, 0:m, :], xr, yi, ALU.mult, P_, ["tA"])
                V2(tB[:, 0:m, :], xi, yr, ALU.mult, P_, ["tB"])
                V2(oi, tA[:, 0:m, :], tB[:, 0:m, :], ALU.add, P_, ["PW"])
            for wi, e_ in ((0, 16), (1, 1)):
                E("vector", "tensor_copy", AL[:, wi, 0, 0, :], PW[:, e_, 0, :], r=["PW"], w=["AL"])
                E("vector", "tensor_copy", AL[:, wi, 0, 1, :], PW[:, e_, 0, :], r=["PW"], w=["AL"])
                E("vector", "tensor_scalar", AL[:, wi, 1, 0, :], PW[:, e_, 1, :], -1.0, None, ALU.mult, r=["PW"], w=["AL"])
                E("vector", "tensor_copy", AL[:, wi, 1, 1, :], PW[:, e_, 1, :], r=["PW"], w=["AL"])

            BB = T("BB", [64, 2, 32, 16])
            t5 = T("t5", [64, 2, 32, 16]); t6 = T("t6", [64, 2, 32, 16])
            creb = cre.unsqueeze(2).to_broadcast([64, 32, 16]); cimb = cim.unsqueeze(2).to_broadcast([64, 32, 16])
            Q_ = KS + ["Bt", "t5", "t6"]
            V2(t5[:, 0], Bt[:, 0], creb, ALU.mult, Q_, ["t5"])
            V2(t6[:, 0], Bt[:, 1], cimb, ALU.mult, Q_, ["t6"])
            V2(BB[:, 0], t5[:, 0], t6[:, 0], ALU.subtract, Q_, ["BB"])
            V2(t5[:, 0], Bt[:, 1], creb, ALU.mult, Q_, ["t5"])
            V2(t6[:, 0], Bt[:, 0], cimb, ALU.mult, Q_, ["t6"])
            V2(BB[:, 0 + 1], t5[:, 0], t6[:, 0], ALU.add, Q_, ["BB"])

            BA = T("BA", [64, 16, 2, 32, 16], BF16)
            CT4 = CT[:].rearrange("p a (g c) -> p a g c", c=16)

            def cprod(dst, src_re, src_im, e0, ne, neg_im, keys_r, key_w):
                pr = PW[:, e0:e0 + ne, 0, :].unsqueeze(3).to_broadcast([64, ne, 32, 16])
                pi_ = PW[:, e0:e0 + ne, 1, :].unsqueeze(3).to_broadcast([64, ne, 32, 16])
                sr = src_re.unsqueeze(1).to_broadcast([64, ne, 32, 16])
                si = src_im.unsqueeze(1).to_broadcast([64, ne, 32, 16])
                R_ = ["PW", "t5", "t6"] + keys_r
                V2(t5[:, 0:ne], sr, pr, ALU.mult, R_, ["t5"])
                V2(t6[:, 0:ne], si, pi_, ALU.mult, R_, ["t6"])
                V2(dst[:, e0:e0 + ne, 0], t5[:, 0:ne], t6[:, 0:ne], ALU.subtract, R_, [key_w])
                V2(t5[:, 0:ne], sr, pi_, ALU.mult, R_, ["t5"])
                V2(t6[:, 0:ne], si, pr, ALU.mult, R_, ["t6"])
                if neg_im:
                    E("vector", "scalar_tensor_tensor", dst[:, e0:e0 + ne, 1], t5[:, 0:ne], -1.0, t6[:, 0:ne],
                      ALU.mult, ALU.subtract, r=R_, w=[key_w])
                else:
                    V2(dst[:, e0:e0 + ne, 1], t5[:, 0:ne], t6[:, 0:ne], ALU.add, R_, [key_w])

            for e0 in range(0, 16, 2):
                cprod(BA, BB[:, 0], BB[:, 1], e0, 2, False, ["BB"], "BA")
            for e0 in range(0, 16, 2):
                cprod(CA, CT4[:, 0], CT4[:, 1], e0, 2, True, ["CT"], "CA")
            cprod(CA, CT4[:, 0], CT4[:, 1], 16, 1, True, ["CT"], "CA")

            pmask = T("pmask", [128, 2])
            pidx = T("pidx", [128, 1])
            E("gpsimd", "iota", pidx[:], pattern=[[0, 1]], base=0, channel_multiplier=1,
              allow_small_or_imprecise_dtypes=True, w=["pidx"])
            pi32 = T("pi32", [128, 2], I32)
            E("vector", "tensor_copy", pi32[:, 0:1], pidx[:], r=["pidx"], w=["pi32"])
            E("vector", "tensor_scalar", pi32[:, 1:2], pi32[:, 0:1], 4, 1, ALU.logical_shift_right, ALU.bitwise_and,
              r=["pi32"], w=["pi32b"])
            E("vector", "tensor_copy", pmask[:, 1:2], pi32[:, 1:2], r=["pi32b"], w=["pmask1"])
            E("vector", "tensor_scalar", pmask[:, 0:1], pmask[:, 1:2], -1.0, 1.0, ALU.mult, ALU.add,
              r=["pmask1"], w=["pmask"])
            nb_ = 0
            for q in range(4):
                for j0 in range(0, 16, 8):
                    wp, wk = wpb[nb_ % 2], f"wpb{nb_ % 2}"
                    nb_ += 1
                    for jj in range(8):
                        j = j0 + jj
                        for ri in range(2):
                            E("tensor", "transpose", wp[:, (jj * 2 + ri) * 64:(jj * 2 + ri + 1) * 64],
                              BA[:, 15 - j, ri, 8 * q:8 * q + 8, :].rearrange("p g c -> p (g c)"), ident_bf[0:64, 0:64],
                              r=["BA", "ident_bf"], w=[wk])
                    src = wp[:].rearrange("p (j r n) -> p j r n", j=8, r=2)
                    for par in range(2):
                        E("vector" if par == 0 else "gpsimd" if False else "vector", "tensor_scalar",
                          WS[:, q, j0:j0 + 8, :, par, :], src, pmask[:, par:par + 1], None, ALU.mult,
                          r=[wk, "pmask", "pmask1"], w=["WS"])
            CTb = T("CTb", [64, 2, 512], BF16)
            E("vector", "tensor_copy", CTb[:, 0, :], CT[:, 0, :], r=["CT"], w=["CTb"])
            E("vector", "tensor_scalar", CTb[:, 1, :], CT[:, 1, :], -1.0, None, ALU.mult, r=["CT"], w=["CTb"])
            bdm = T("bdm", [128, 128])
            io2 = T("io2", [128, 128], I32)
            E("gpsimd", "iota", io2[:], pattern=[[1, 128]], base=0, channel_multiplier=0, w=["io2"])
            E("vector", "tensor_scalar", io2[:], io2[:], 4, None, ALU.logical_shift_right, r=["io2"], w=["io2"])
            gcol = T("gcol", [128, 128])
            E("vector", "tensor_copy", gcol[:], io2[:], r=["io2"], w=["gcol"])
            prow = T("prow", [128, 2], I32)
            E("vector", "tensor_scalar", prow[:, 0:1], pi32[:, 0:1], 4, None, ALU.logical_shift_right, r=["pi32"], w=["prow"])
            prowf = T("prowf", [128, 1])
            E("vector", "tensor_copy", prowf[:], prow[:, 0:1], r=["prow"], w=["prowf"])
            E("vector", "tensor_scalar", bdm[:], gcol[:], prowf[:, 0:1], None, ALU.is_equal, r=["gcol", "prowf"], w=["bdm"])
            bdm4 = bdm[:].unsqueeze(1).to_broadcast([128, 4, 128])
            for q in range(4):
                for d0 in range(0, 16, 4):
                    wp, wk = wps[nb_ % 2], f"wps{nb_ % 2}"
                    nb_ += 1
                    for dd in range(4):
                        for ri in range(2):
                            mm(wp[:, dd * 128:(dd + 1) * 128],
                               BA[:, d0 + dd, ri, 8 * q:8 * q + 8, :].rearrange("p g c -> p (g c)"),
                               CTb[:, ri, q * 128:(q + 1) * 128], start=(ri == 0), stop=(ri == 1),
                               r=["BA", "CTb"], w=[wk])
                    V2(KM[:, q, d0:d0 + 4, :], wp[:].rearrange("p (d n) -> p d n", d=4), bdm4, ALU.mult,
                       [wk, "bdm"], [f"KM{q}"])
                dg = T(f"dg{q}", [128, 128])
                E("vector", "tensor_scalar", dg[:], ident_f[:], dcol[:, q:q + 1], None, ALU.mult,
                  r=["ident_f", "dcol"], w=[f"dg{q}"])
                V2(KM[:, q, 0, :], KM[:, q, 0, :], dg[:], ALU.add, [f"KM{q}", f"dg{q}"], [f"KM{q}"])
            S.barrier()
            S.emit()

        uT = sb("uT", [128, 4, TOK], BF16, pS)
        Hb = sb("Hb", [64, 129, 2, 32], BF16, pS)
        with contextlib.ExitStack() as pU:
            w_u = sb("w_u", [128, 8, 512], BF16, pU)
            for c in range(8):
                dma(w_u[:, c, :], w_in_c[:, c, 1544:2056], writes=[f"w_u{c}"], queue="gpsimd")
            u_ps = [ps(f"u_ps{i}", [128, 512], F32, pU) for i in range(2)]
            nu = 0
            for q in range(4):
                for nb in range(5):
                    blk = slice(nb * 512, min((nb + 1) * 512, TOK))
                    n = blk.stop - blk.start
                    up, uk = u_ps[nu % 2], f"u_ps{nu % 2}"
                    for c in range(8):
                        mm(up[:, 0:n], w_u[:, c, q * 128:(q + 1) * 128], hnT[:, c, blk], start=(c == 0), stop=(c == 7),
                           r=[f"w_u{c}"], w=[uk])
                    if nu % 2 == 0:
                        act(uT[:, q, blk], up[:, 0:n], ACT.Copy, r=[uk], w=[f"uT{q}"])
                    else:
                        E("vector", "tensor_copy", uT[:, q, blk], up[:, 0:n], r=[uk], w=[f"uT{q}"])
                    nu += 1
            S.barrier()
            S.emit()

        E("gpsimd", "memset", Hb[:, 0, :, :], 0.0, w=["Hb0"])
        uTj = [uT[:, q, 0:SEQ].rearrange("p (k j) -> p j k", j=16) for q in range(4)]
        h0b = sb("h0b", [64, 2, 32, NS], BF16, pS)
        with contextlib.ExitStack() as pH:
          s_ps = [ps(f"ss_ps{i}", [128, 512], F32, pH) for i in range(2)]
          fps = ps("fps", [128, 512], F32, pH)
          with contextlib.ExitStack() as pHi:
            Hf = sb("Hf", [64, 128, 2, 32], F32, pHi)
            nsp = 0
            for gp in range(16):
                q, pp = gp // 4, gp % 4
                sp_, sk = s_ps[nsp % 2], f"ss_ps{nsp % 2}"
                nsp += 1
                for ri in range(2):
                    for par in range(2):
                        o = (ri * 2 + par) * 128
                        for j in range(16):
                            op("tensor", lambda e, o=o, sp_=sp_, q=q, pp=pp, j=j, ri=ri, par=par: e.matmul(
                                sp_[0:64, o:o + 128], WS[32 * pp:32 * pp + 32, q, j, ri, par, :],
                                uTj[q][32 * pp:32 * pp + 32, j, :], start=(j == 0), stop=(j == 15),
                                tile_position=(32 * pp, 0)), [f"uT{q}", "WS"], [sk])
                E("vector" if gp % 2 == 0 else "scalar", "tensor_copy" if gp % 2 == 0 else "activation",
                  Hf[:, :, :, 2 * gp:2 * gp + 2].rearrange("p k r g -> p r g k"),
                  sp_[0:64, :].rearrange("p (r g k) -> p r g k", r=2, g=2),
                  *(() if gp % 2 == 0 else (ACT.Copy,)), r=[sk], w=[f"Hf_g{gp}"])
            rt = [sb(f"rt{i}", [64, 2, 32], F32, pHi) for i in range(2)]
            allg = [f"Hf_g{gp}" for gp in range(16)]
            prev_key = allg
            for kc in range(1, 128):
                cur = f"Hk{kc}"
                P_ = Hf[:, kc - 1, :, :]
                V2(rt[0][:], P_, AL[:, 0, 0, :, :], ALU.mult, prev_key + ["AL", "rt0"], ["rt0"])
                V2(rt[1][:, 0, :], P_[:, 1, :], AL[:, 0, 1, 0, :], ALU.mult, prev_key + ["AL", "rt1"], ["rt1"])
                V2(rt[1][:, 1, :], P_[:, 0, :], AL[:, 0, 1, 1, :], ALU.mult, prev_key + ["AL", "rt1"], ["rt1"])
                V2(rt[0][:], rt[0][:], rt[1][:], ALU.add, ["rt0", "rt1"], ["rt0"])
                V2(Hf[:, kc, :, :], Hf[:, kc, :, :], rt[0][:], ALU.add, ["rt0"] + (allg if kc == 1 else []), [cur])
                prev_key = [cur]
            E("vector", "tensor_copy", Hb[:, 1:129, :, :], Hf[:], r=prev_key + allg, w=["Hb"])
            fin = sb("fin", [32, 2, 64], F32, pHi)
            for ri in range(2):
                E("tensor", "transpose", fps[0:32, ri * 64:(ri + 1) * 64], Hf[:, 127, ri, :], ident_f[0:64, 0:64],
                  r=prev_key + ["ident_f"], w=["fps"])
            E("vector", "tensor_copy", fin[:].rearrange("p a n -> p (a n)"), fps[0:32, 0:128], r=["fps"], w=["fin"])
            S.mark_output(dma(hr_p[:, :], fin[:, 0, :], reads=["fin"]))
            S.mark_output(dma(hi_p[:, :], fin[:, 1, :], reads=["fin"]))
            S.barrier()
            S.emit()
          if True:

            h0n = sb("h0n", [128, 2, 64], F32, pH)
            dma(h0n[:, 0, :], st_re_d[:, :], writes=["h0n"])
            dma(h0n[:, 1, :], st_im_d[:, :], writes=["h0n"])
            h0 = sb("h0", [64, 2, NS, 32], F32, pH)
            h1 = sb("h1", [64, 2, NS, 32], F32, pH)
            for ri in range(2):
                E("tensor", "transpose", fps[0:64, 128 + ri * 128:256 + ri * 128], h0n[:, ri, :], ident_f[:],
                  r=["h0n", "ident_f"], w=["fps2"])
            E("vector", "tensor_copy", h0[:].rearrange("p r s g -> p (r s g)"), fps[0:64, 128:384], r=["fps2"], w=["h0"])
            E("vector", "tensor_copy", h0b[:].rearrange("p r g s -> p r s g"), h0[:], r=["h0"], w=["h0b"])
            ssp = s_ps[0]
            for g in range(32):
                q, pp, par = g // 8, (g % 8) // 2, g % 2
                for ri in range(2):
                    o = (ri * 32 + g) * NS
                    op("tensor", lambda e, o=o, q=q, pp=pp, ri=ri, par=par: e.matmul(
                        ssp[0:64, o:o + NS], WS[32 * pp:32 * pp + 32, q, 15, ri, par, :],
                        uT[32 * pp:32 * pp + 32, q, SEQ:SEQ + NS], start=True, stop=True,
                        tile_position=(32 * pp, 0)), [f"uT{q}", "WS"], ["ss_ps0"])
            a1b = [AL[:, 1, i, :, :].unsqueeze(2).to_broadcast([64, 2, NS, 32]) for i in range(2)]
            t7 = sb("t7", [64, 2, NS, 32], F32, pH); t8 = sb("t8", [64, 2, NS, 32], F32, pH)
            V2(t7[:], h0[:], a1b[0], ALU.mult, ["h0", "AL"], ["t7"])
            V2(t8[:, 0], h0[:, 1], a1b[1][:, 0], ALU.mult, ["h0", "AL"], ["t8"])
            V2(t8[:, 1], h0[:, 0], a1b[1][:, 1], ALU.mult, ["h0", "AL"], ["t8"])
            V2(t7[:], t7[:], t8[:], ALU.add, ["t7", "t8"], ["t7"])
            V2(h1[:], t7[:], ssp[0:64, 0:2 * 32 * NS].rearrange("p (r g s) -> p r s g", r=2, g=32), ALU.add,
               ["t7", "ss_ps0"], ["h1"])
            for ri in range(2):
                E("tensor", "transpose", fps[:, 384 + ri * 64:448 + ri * 64], h1[:, ri, :, :].rearrange("p s g -> p (s g)"),
                  ident_f[0:64, 0:64], r=["h1", "ident_f"], w=["fps3"])
            h1o = sb("h1o", [128, 2, 64], F32, pH)
            E("vector", "tensor_copy", h1o[:].rearrange("p a n -> p (a n)"), fps[:, 384:512], r=["fps3"], w=["h1o"])
            S.mark_output(dma(hr_s[:, :], h1o[:, 0, :], reads=["h1o"]))
            S.mark_output(dma(hi_s[:, :], h1o[:, 1, :], reads=["h1o"]))
            S.barrier()
            S.emit()

        with contextlib.ExitStack() as pY:
            y_ps = ps("y_ps", [128, 2048], F32, pY)
            ys_ps = ps("ys_ps", [128, 512], F32, pY)
            t_ps2 = ps("t_ps2", [128, 2560], BF16, pY)
            ysb = [sb(f"ysb{i}", [128, 2048], BF16, pY) for i in range(2)]
            yss = sb("yss", [128, 512], BF16, pY)
            g1 = sb("g1", [128, 2176], F32, pY); g2 = sb("g2", [128, 2176], F32, pY)
            for q in range(4):
                mm(ys_ps[0:NS, q * 128:(q + 1) * 128], uT[:, q, SEQ:SEQ + NS], KM[:, q, 0, :], start=(q == 0), stop=False,
                   r=[f"uT{q}", f"KM{q}"], w=["ys_ps"])
            for g in range(32):
                for ri in range(2):
                    mm(ys_ps[0:NS, g * 16:(g + 1) * 16], h0b[:, ri, g, :], CA[:, 1, ri, g, :], start=False,
                       stop=(g == 31 and ri == 1), r=["h0b", "CA"], w=["ys_ps"])
            E("vector", "memset", yss[:], 0.0, w=["yss"])
            E("vector", "tensor_copy", yss[0:NS, :], ys_ps[0:NS, :], r=["ys_ps"], w=["yss"])
            for q in range(4):
                yk = f"y_ps"
                for j in range(16):
                    for i in range(j + 1):
                        mm(y_ps[:, j * 128:(j + 1) * 128], uTj[q][:, i, :], KM[:, q, j - i, :], start=(i == 0), stop=False,
                           r=[f"uT{q}", f"KM{q}"], w=[yk])
                    for gl in range(8):
                        g = 8 * q + gl
                        for ri in range(2):
                            mm(y_ps[:, j * 128 + gl * 16:j * 128 + (gl + 1) * 16], Hb[:, 0:128, ri, g], CA[:, j + 1, ri, g, :],
                               start=False, stop=(gl == 7 and ri == 1), r=["Hb", "Hb0", "CA"], w=[yk])
                yb, ybk = ysb[q % 2], f"ysb{q % 2}"
                act(yb[:], y_ps[:], ACT.Copy, r=[yk], w=[ybk])
                for j in range(16):
                    E("tensor", "transpose", t_ps2[:, j * 128:(j + 1) * 128], yb[:, j * 128:(j + 1) * 128], ident_bf[:],
                      r=[ybk, "ident_bf"], w=["t_ps2"])
                E("tensor", "transpose", t_ps2[:, 2048:2176], yss[:, q * 128:(q + 1) * 128], ident_bf[:],
                  r=["yss", "ident_bf"], w=["t_ps2"])
                xin = t_ps2[:, 0:2176]
                act(g1[:], xin, ACT.Square, r=["t_ps2"], w=["g1"])
                E("vector", "tensor_scalar", g1[:], g1[:], 0.044715, 1.0, ALU.mult, ALU.add, r=["g1"], w=["g1"])
                E("vector", "tensor_tensor", g1[:], g1[:], xin, ALU.mult, r=["g1", "t_ps2"], w=["g1"])
                act(g2[:], g1[:], ACT.Sigmoid, scale=1.5957691216057308, r=["g1"], w=["g2"])
                E("vector", "tensor_tensor", zsT[:, q, 0:SEQ].rearrange("p (k j) -> p j k", j=16),
                  g2[:, 0:SEQ].rearrange("p (j k) -> p j k", j=16), t_ps2[:, 0:SEQ].rearrange("p (j k) -> p j k", j=16),
                  ALU.mult, r=["g2", "t_ps2"], w=[f"zsT{q}"])
                E("vector", "tensor_tensor", zsT[:, q, SEQ:TOK], g2[:, SEQ:TOK], t_ps2[:, SEQ:TOK], ALU.mult,
                  r=["g2", "t_ps2"], w=[f"zsT{q}"])
            if dbg and dbg[0] == "zs":
                dt_ = sb("dbgt", [128, TOK], F32, pY)
                dv = dbg_t.rearrange("p (a n) -> p a n", a=4)
                for a in range(4):
                    E("vector", "tensor_copy", dt_[:], zsT[:, a, :], r=[f"zsT{a}"], w=["dbgt"])
                    S.mark_output(dma(dv[:, a, :], dt_[:], reads=["dbgt"]))
            S.barrier()
            S.emit()

    pA = contextlib.ExitStack()
    attnT = sb("attnT", [128, 4, TOK], BF16, pA)
    E("gpsimd", "memset", attnT[:, :, SEQ:TOK], 0.0, w=["attnT_s"])
    if stage == 2 or stage >= 4:
      with contextlib.ExitStack() as p2:
        v_bf = sb("v_bf", [128, NT, AW], BF16, p2)
        dma(v_bf[:], v_p.rearrange("(t p) c -> p t c", p=128), writes=["v_bf"], queue="gpsimd")
        w_qk = sb("w_qk", [128, 8, 1024], BF16, p2)
        for c in range(8):
            dma(w_qk[:, c, :], w_in_c[:, c, 0:1024], writes=[f"w_qk{c}"], queue="gpsimd")
        w_f = sb("w_f", [128, 8, 8], BF16, p2)
        dma(w_f[:], w_in_c[:, :, 1536:1544], writes=["w_f"], queue="gpsimd")
        negb8 = sb("negb8", [8, 1], F32, p2)
        dma(negb8[:], b_forget.rearrange("o h -> h o"), writes=["negb8"])
        E("vector", "tensor_scalar", negb8[:], negb8[:], -1.0, None, ALU.mult, r=["negb8"], w=["negb8"])
        ones8 = sb("ones8", [8, SEQ], F32, p2)
        E("gpsimd", "memset", ones8[:], 1.0, w=["ones8"])
        spl = sb("spl", [8, SEQ], F32, p2)
        cs = sb("cs", [8, SEQ], F32, p2)
        e1 = [sb(f"e1_{i}", [8, 512], F32, p2) for i in range(2)]
        c_split = sb("c_split", [8, 3, SEQ], BF16, p2)
        tmpf = [sb(f"tmpf{i}", [8, SEQ], F32, p2) for i in range(3)]
        negc_tok = sb("negc_tok", [128, NT * H], F32, p2)
        with contextlib.ExitStack() as p2a:
            f_ps = [ps(f"f_ps{i}", [128, 512], F32, p2a) for i in range(2)]
            t_ps = ps("t_ps", [128, 512], F32, p2a)
            for nb in range(4):
                fp = f_ps[nb % 2]
                blk = slice(nb * 512, (nb + 1) * 512)
                for c in range(8):
                    mm(fp[0:8, :], w_f[:, c, :], hnT[:, c, blk], start=(c == 0), stop=(c == 7),
                       r=["w_f"], w=[f"f_ps{nb % 2}"])
                act(e1[nb % 2][:], fp[0:8, :], ACT.Exp, scale=-1.0, bias=negb8[:, 0:1],
                    r=[f"f_ps{nb % 2}", "negb8"], w=[f"e1_{nb % 2}"])
                act(spl[:, blk], e1[nb % 2][:], ACT.Ln, bias=1.0, r=[f"e1_{nb % 2}"], w=[f"spl{nb}"])
            E("vector", "tensor_tensor_scan", cs[:], ones8[:], spl[:], 0.0, ALU.mult, ALU.add,
              r=["ones8"] + [f"spl{i}" for i in range(4)], w=["cs"])
            E("vector", "tensor_scalar", c_split[:, 0, :], cs[:], -8.0, None, ALU.mult, r=["cs"], w=["c_hi"])
            E("vector", "tensor_copy", tmpf[0][:], c_split[:, 0, :], r=["c_hi"], w=["tmpf0"])
            E("vector", "scalar_tensor_tensor", tmpf[1][:], cs[:], -8.0, tmpf[0][:], ALU.mult, ALU.subtract,
              r=["cs", "tmpf0"], w=["tmpf1"])
            E("vector", "tensor_copy", c_split[:, 1, :], tmpf[1][:], r=["tmpf1"], w=["c_mid"])
            E("vector", "tensor_copy", tmpf[2][:], c_split[:, 1, :], r=["c_mid"], w=["tmpf2"])
            E("vector", "tensor_tensor", tmpf[0][:], tmpf[1][:], tmpf[2][:], ALU.subtract,
              r=["tmpf1", "tmpf2"], w=["tmpf0"])
            E("vector", "tensor_copy", c_split[:, 2, :], tmpf[0][:], r=["tmpf0"], w=["c_lo"])
            for t in range(NT):
                E("tensor", "transpose", t_ps[:, t * 8:(t + 1) * 8], cs[0:8, t * 128:(t + 1) * 128],
                  ident_f[0:8, 0:8], r=["cs", "ident_f"], w=["t_ps"])
            E("vector", "tensor_copy", negc_tok[:], t_ps[:, 0:NT * H], r=["t_ps"], w=["negc_tok"])
            S.barrier()
            S.emit()

        qa = [sb(f"qa{i}", [128, SEQ], BF16, p2) for i in range(2)]
        ka = [sb(f"ka{i}", [128, SEQ], BF16, p2) for i in range(2)]
        vp = [sb(f"vp{i}", [128, NT, 128], BF16, p2) for i in range(2)]
        onesp = [sb(f"onesp{i}", [128, 128], BF16, p2) for i in range(2)]
        pT = [sb(f"pT{i}", [128, 512], BF16, p2) for i in range(3)]
        rl = [sb(f"rl{i}", [128, 512], F32, p2) for i in range(2)]
        for i in range(2):
            E("gpsimd", "memset", ka[i][64:67, :], 1.0, w=[f"ka{i}"])
            E("gpsimd", "memset", vp[i][:], 0.0, w=[f"vp{i}"])
            E("gpsimd", "memset", onesp[i][:], 0.0, w=[f"onesp{i}"])
            E("gpsimd", "memset", onesp[i][:, i * 64:(i + 1) * 64], 1.0, w=[f"onesp{i}"])
        with contextlib.ExitStack() as p2b:
            pj_ps = [ps(f"pj_ps{i}", [128, 512], F32, p2b) for i in range(2)]
            s_ps = [ps(f"s_ps{i}", [128, 512], F32, p2b) for i in range(2)]
            o_ps = [ps(f"o_ps{i}", [128, 512], F32, p2b) for i in range(2)]
            l_ps = [ps(f"l_ps{i}", [128, 512], F32, p2b) for i in range(2)]
            npj = 0
            nsc = 0
            ngr = 0
            for pr in range(4):
                for hh in range(2):
                    h = 2 * pr + hh
                    for (dst, dk, col0) in ((qa[hh], f"qa{hh}", h * 64), (ka[hh], f"ka{hh}", 512 + h * 64)):
                        for nb in range(4):
                            blk = slice(nb * 512, (nb + 1) * 512)
                            pp, pk = pj_ps[npj % 2], f"pj_ps{npj % 2}"
                            for c in range(8):
                                mm(pp[0:64, :], w_qk[:, c, col0:col0 + 64], hnT[:, c, blk],
                                   start=(c == 0), stop=(c == 7), r=[f"w_qk{c}"], w=[pk])
                            if npj % 2 == 0:
                                act(dst[0:64, blk], pp[0:64, :], ACT.Copy, r=[pk], w=[dk])
                            else:
                                E("vector", "tensor_copy", dst[0:64, blk], pp[0:64, :], r=[pk], w=[dk])
                            npj += 1
                    for i in range(3):
                        dma(qa[hh][64 + i:65 + i, :], c_split[h:h + 1, i, :], reads=["c_hi", "c_mid", "c_lo"],
                            writes=[f"qa{hh}"])
                    E("vector", "tensor_copy", vp[hh][:, :, hh * 64:(hh + 1) * 64], v_bf[:, :, h * 64:(h + 1) * 64],
                      r=["v_bf"], w=[f"vp{hh}"])
                for g in range(4):
                    gb = ngr % 2
                    OP, LP = f"o_ps{gb}", f"l_ps{gb}"
                    items = [(hh, j) for hh in range(2) for j in range(4 * g + 4)]
                    pend = None

                    def qk_exp(hh, j):
                        nonlocal nsc
                        h = 2 * pr + hh
                        rr = j - 4 * g
                        c0 = max(rr, 0) * 128
                        sp_, sk = s_ps[nsc % 2], f"s_ps{nsc % 2}"
                        pt, pk = pT[nsc % 3], f"pT{nsc % 3}"
                        nsc += 1
                        mm(sp_[:, c0:512], ka[hh][0:67, j * 128:(j + 1) * 128],
                           qa[hh][0:67, g * 512 + c0:(g + 1) * 512], r=[f"ka{hh}", f"qa{hh}"], w=[sk])
                        act(pt[:, c0:512], sp_[:, c0:512], ACT.Exp, scale=0.125,
                            bias=negc_tok[:, j * H + h:j * H + h + 1], r=[sk, "negc_tok"], w=[pk])
                        if rr >= 0:
                            E("gpsimd", "tensor_tensor", pt[:, c0:c0 + 128], pt[:, c0:c0 + 128], tri[:], ALU.mult,
                              r=[pk, "tri"], w=[pk])
                        return (hh, j, c0, pt, pk)

                    def pv(it, first, last):
                        hh, j, c0, pt, pk = it
                        mm(o_ps[gb][:, c0:512], vp[hh][:, j, :], pt[:, c0:512], start=first, stop=last,
                           r=[f"vp{hh}", pk], w=[OP])
                        mm(l_ps[gb][:, c0:512], onesp[hh][:], pt[:, c0:512], start=first, stop=last,
                           r=[f"onesp{hh}", pk], w=[LP])

                    for idx, (hh, j) in enumerate(items):
                        cur = qk_exp(hh, j)
                        if pend is not None:
                            pv(pend, first=(idx == 1), last=False)
                        pend = cur
                    pv(pend, first=(len(items) == 1), last=True)
                    E("vector", "reciprocal", rl[gb][:], l_ps[gb][:], r=[LP], w=[f"rl{gb}"])
                    E("vector", "tensor_tensor", attnT[:, pr, g * 512:(g + 1) * 512], o_ps[gb][:], rl[gb][:], ALU.mult,
                      r=[OP, f"rl{gb}"], w=[f"attnT{pr}_{g}"])
                    ngr += 1
            if dbg and dbg[0] == "attn":
                dt_ = sb("dbgt", [128, SEQ], F32, p2b)
                dv = dbg_t.rearrange("p (a n) -> p a n", a=4)
                for a in range(4):
                    E("vector", "tensor_copy", dt_[:], attnT[:, a, 0:SEQ],
                      r=[f"attnT{a}_{b_}" for b_ in range(4)], w=["dbgt"])
                    S.mark_output(dma(dv[:, a, :], dt_[:], reads=["dbgt"]))
            S.barrier()
            S.emit()

    if stage >= 4:
      with contextlib.ExitStack() as pq:
        ck_d = k.din("cache_k", [NPHYS * 128, AW]); cv_d = k.din("cache_v", [NPHYS * 128, AW])
        cl_d = k.din("cache_logf", [NPHYS, 128 * H])
        pt_d = k.din("page_table", [1, NS * NPAGES], I32)
        NEG = -1.0e30
        w_qs = sb("w_qs", [128, 8, AW], BF16, pq)
        for c in range(8):
            dma(w_qs[:, c, :], w_in_c[:, c, 0:512], writes=["w_qs"], queue="gpsimd", merge=True)
        ptb = sb("ptb", [128, NS * NPAGES], I32, pq)
        dma(ptb[:], pt_d.partition_broadcast(128), writes=["ptb"])
        ptf = sb("ptf", [128, NS * NPAGES], F32, pq)
        pidx = sb("pidx_q", [128, 1], F32, pq)
        E("gpsimd", "iota", pidx[:], pattern=[[0, 1]], base=0, channel_multiplier=1,
          allow_small_or_imprecise_dtypes=True, w=["pidx"])
        E("vector", "tensor_copy", ptf[:], ptb[:], r=["ptb"], w=["ptf"])
        E("vector", "tensor_scalar", ptf[:], ptf[:], 128.0, None, ALU.mult, r=["ptf"], w=["ptf"])
        E("vector", "tensor_scalar", ptf[:], ptf[:], pidx[:, 0:1], None, ALU.add, r=["ptf", "pidx"], w=["ptf"])
        rowi = sb("rowi", [128, NS * NPAGES], I32, pq)
        E("vector", "tensor_copy", rowi[:], ptf[:], r=["ptf"], w=["rowi"])
        mext = sb("mext", [128, 1], F32, pq)
        E("vector", "tensor_scalar", mext[:], pidx[:], 0.0, NEG, ALU.is_gt, ALU.mult, r=["pidx"], w=["mext"])
        pg2 = sb("pg2", [128, 2], I32, pq)
        for m in range(2):
            dma(pg2[:, m:m + 1], pt_d[0:1, m * 128:(m + 1) * 128].rearrange("o n -> n o"), writes=["pg2"])
        ones_f = sb("ones_f", [128, 128], F32, pq)
        E("gpsimd", "memset", ones_f[:], 1.0, w=["ones_f"])
        lt2 = sb("lt2", [128, 128], F32, pq)
        bo2 = sb("bo2", [128, 128], F32, pq)
        E("vector", "tensor_single_scalar", lt2[:], iota_t[:], 0.0, ALU.is_gt, r=["iota_t"], w=["lt2"])
        E("vector", "memset", lt2[0:64, 64:128], 0.0, w=["lt2"])
        E("vector", "memset", bo2[:], 0.0, w=["bo2"])
        E("vector", "memset", bo2[0:64, 0:64], 1.0, w=["bo2"])
        E("vector", "memset", bo2[64:128, 64:128], 1.0, w=["bo2"])
        q_ps = ps("q_ps", [128, 512], F32, pq)
        m_ps = ps("m_ps", [128, 512], F32, pq)
        bt_ps = [ps(f"bt_ps{i}", [128, 512], F32, pq) for i in range(2)]
        o_ps = ps("os_ps", [128, 512], F32, pq)
        qb = sb("qb", [128, NS, AW], F32, pq)
        hb = [sb(f"hb{i}", [128, 8, 128], BF16, pq) for i in range(2)]
        for s_ in range(NS):
            col = SEQ + s_
            E("vector", "tensor_copy", hb[s_ % 2][:], hnT[:, :, col:col + 1].to_broadcast([128, 8, 128]), w=[f"hb{s_ % 2}"])
            for c in range(8):
                mm(q_ps[:], hb[s_ % 2][:, c, :], w_qs[:, c, :], start=(c == 0), stop=(c == 7), r=[f"hb{s_ % 2}", "w_qs"], w=["q_ps"])
            act(qb[:, s_, :], q_ps[:], ACT.Copy, scale=0.125, r=["q_ps"], w=["qb"])
        BT = sb("BT", [128, 2, H, 128], F32, pq)
        lfn = sb("lfn", [128, 2, H], F32, pq)
        for m in range(2):
            for s2 in range(2):
                dma(lfn[s2 * 64:(s2 + 1) * 64, m, :], lf_s[2 * m + s2:2 * m + s2 + 1, :].partition_broadcast(64),
                    writes=["lfn"])
        with contextlib.ExitStack() as pl:
            Lg = [sb(f"Lg{i}", [128, 128 * H], F32, pl) for i in range(2)]
            Cg = [sb(f"Cg{i}", [128, 128 * H], F32, pl) for i in range(2)]
            base = sb("base", [128, 2, H], F32, pl)
            for m in range(2):
                dma(Lg[m][:], cl_d[:, :], reads=["pg2"], writes=[f"Lg{m}"], queue="gpsimd",
                    gather=bass.IndirectOffsetOnAxis(ap=pg2[:, m:m + 1], axis=0))
                for h in range(H):
                    E("vector", "tensor_tensor_scan", Cg[m][:, h:128 * H:H], ones_f[:], Lg[m][:, h:128 * H:H], 0.0,
                      ALU.mult, ALU.add, r=[f"Lg{m}", "ones_f"], w=[f"Cg{m}"])
                tot = Cg[m][:, 127 * H:128 * H]
                mm(m_ps[:, m * 16:m * 16 + 8], lt2[:], tot, r=["lt2", f"Cg{m}"], w=["m_ps"])
                mm(m_ps[:, m * 16 + 8:m * 16 + 16], bo2[:], tot, r=["bo2", f"Cg{m}"], w=["m_ps"])
                E("vector", "tensor_tensor", base[:, m, :], m_ps[:, m * 16 + 8:m * 16 + 16], lfn[:, m, :], ALU.add,
                  r=["m_ps", "lfn"], w=["base"])
                E("vector", "tensor_tensor", base[:, m, :], base[:, m, :], m_ps[:, m * 16:m * 16 + 8], ALU.subtract,
                  r=["m_ps", "base"], w=["base"])
                E("vector", "scalar_tensor_tensor", Cg[m][:].rearrange("p (r h) -> p r h", h=H),
                  Cg[m][:].rearrange("p (r h) -> p r h", h=H), -1.0,
                  base[:, m, :].unsqueeze(1).to_broadcast([128, 128, H]), ALU.mult, ALU.add,
                  r=[f"Cg{m}", "base"], w=[f"Cg{m}"])
                for h4 in range(2):
                    bp = bt_ps[h4]
                    for hh in range(4):
                        h = h4 * 4 + hh
                        E("tensor", "transpose", bp[:, hh * 128:(hh + 1) * 128], Cg[m][:, h:128 * H:H], ident_f[:],
                          r=[f"Cg{m}", "ident_f"], w=[f"bt_ps{h4}"])
                    E("vector", "tensor_copy", BT[:, m, h4 * 4:h4 * 4 + 4, :].rearrange("p h x -> p (h x)"), bp[:],
                      r=[f"bt_ps{h4}"], w=["BT"])
            S.barrier()
            S.emit()
        Sc = sb("Sc", [128, NS, NPAGES + 1, H], F32, pq)
        NKB = 4
        Kb = [sb(f"Kb{i}", [128, 4, AW], F32, pq) for i in range(NKB)]
        Vb = [sb(f"Vb{i}", [128, 4, AW], F32, pq) for i in range(NKB)]
        prod = sb("prod", [128, 4, AW], F32, pq)
        Kx = sb("Kx", [128, NS, AW], F32, pq)
        Vx = sb("Vx", [128, NS, AW], F32, pq)
        E("gpsimd", "memset", Kx[:], 0.0, w=["Kx"])
        E("gpsimd", "memset", Vx[:], 0.0, w=["Vx"])
        dma(Kx[0:1, :, :], k_s.rearrange("(o s) c -> o s c", o=1), writes=["Kx"])
        dma(Vx[0:1, :, :], v_s.rearrange("(o s) c -> o s c", o=1), writes=["Vx"])
        ng = 0
        for s_ in range(NS):
            m, s2 = s_ // 2, s_ % 2
            for g4 in range(NPAGES // 4):
                kb, kk = Kb[ng % NKB], f"Kb{ng % NKB}"
                ng += 1
                for pi in range(4):
                    pg = g4 * 4 + pi
                    dma(kb[:, pi, :], ck_d[:, :], reads=["rowi"], writes=[f"{kk}_{pi}"], queue="gpsimd",
                        gather=bass.IndirectOffsetOnAxis(ap=rowi[:, s_ * NPAGES + pg:s_ * NPAGES + pg + 1], axis=0))
                E("vector", "tensor_tensor", prod[:], kb[:], qb[:, s_, :].unsqueeze(1).to_broadcast([128, 4, AW]), ALU.mult,
                  r=[f"{kk}_{i_}" for i_ in range(4)] + ["qb"], w=["prod"])
                E("vector", "tensor_reduce", Sc[:, s_, g4 * 4:g4 * 4 + 4, :], prod[:].rearrange("p g (h d) -> p g h d", h=H),
                  AX.X, ALU.add, r=["prod"], w=[f"Sc{s_}"])
            E("vector", "tensor_tensor", prod[:, 0, :], Kx[:, s_, :], qb[:, s_, :], ALU.mult, r=["Kx", "qb"], w=["prod"])
            E("vector", "tensor_reduce", Sc[:, s_, NPAGES, :], prod[:, 0, :].rearrange("p (h d) -> p h d", h=H),
              AX.X, ALU.add, r=["prod"], w=[f"Sc{s_}"])
            E("vector", "tensor_scalar", Sc[:, s_, NPAGES, :], Sc[:, s_, NPAGES, :], mext[:, 0:1], None, ALU.add,
              r=[f"Sc{s_}", "mext"], w=[f"Sc{s_}"])
            btv = BT[:, m, :, s2 * 64:(s2 + 1) * 64].rearrange("p h g -> p g h")
            E("vector", "tensor_tensor", Sc[:, s_, 0:NPAGES, :], Sc[:, s_, 0:NPAGES, :], btv, ALU.add,
              r=[f"Sc{s_}", "BT"], w=[f"Sc{s_}"])
        SCK = [f"Sc{i}" for i in range(NS)]
        mx = sb("mx", [128, NS * H], F32, pq)
        E("vector", "tensor_reduce", mx[:].rearrange("p (s h) -> p s h", h=H), Sc[:].rearrange("p s g h -> p s h g"),
          AX.X, ALU.max, r=SCK, w=["mx"])
        E("tensor", "transpose", m_ps[0:32, 128:256], mx[:], ident_f[:], r=["mx", "ident_f"], w=["m_ps"])
        gm = sb("gm", [32, 1], F32, pq)
        E("vector", "tensor_reduce", gm[:], m_ps[0:32, 128:256], AX.X, ALU.max, r=["m_ps"], w=["gm"])
        dgm = sb("dgm", [32, 32], F32, pq)
        E("vector", "tensor_scalar", dgm[:], ident_f[0:32, 0:32], gm[:, 0:1], None, ALU.mult, r=["gm", "ident_f"], w=["dgm"])
        mm(m_ps[:, 256:288], ones_f[0:32, :], dgm[:], r=["ones_f", "dgm"], w=["m_ps2"])
        gmb = sb("gmb", [128, NS * H], F32, pq)
        E("vector", "tensor_copy", gmb[:], m_ps[:, 256:288], r=["m_ps2"], w=["gmb"])
        E("vector", "tensor_tensor", Sc[:], Sc[:],
          gmb[:].rearrange("p (s h) -> p s h", h=H).unsqueeze(2).to_broadcast([128, NS, NPAGES + 1, H]), ALU.subtract,
          r=SCK + ["gmb"], w=["ScA"])
        act(Sc[:].rearrange("p s g h -> p (s g h)"), Sc[:].rearrange("p s g h -> p (s g h)"), ACT.Exp, r=["ScA"], w=["ScP"])
        ls = sb("ls", [128, NS * H], F32, pq)
        E("vector", "tensor_reduce", ls[:].rearrange("p (s h) -> p s h", h=H), Sc[:].rearrange("p s g h -> p s h g"),
          AX.X, ALU.add, r=["ScP"], w=["ls"])
        mm(m_ps[:, 320:352], ones_f[:], ls[:], r=["ones_f", "ls"], w=["m_ps3"])
        rlb = sb("rlb", [128, NS * H], F32, pq)
        E("vector", "reciprocal", rlb[:], m_ps[:, 320:352], r=["m_ps3"], w=["rlb"])
        first = True
        for s_ in range(NS):
            for g4 in range(NPAGES // 4 + 1):
                if g4 < NPAGES // 4:
                    vb, vk = Vb[ng % NKB], f"Vb{ng % NKB}"
                    ng += 1
                    npg = 4
                    for pi in range(4):
                        pg = g4 * 4 + pi
                        dma(vb[:, pi, :], cv_d[:, :], reads=["rowi"], writes=[f"{vk}_{pi}"], queue="gpsimd",
                            gather=bass.IndirectOffsetOnAxis(ap=rowi[:, s_ * NPAGES + pg:s_ * NPAGES + pg + 1], axis=0))
                else:
                    npg = 1
                for pi in range(npg):
                    pg = g4 * 4 + pi
                    src = vb[:, pi, :] if g4 < NPAGES // 4 else Vx[:, s_, :]
                    sk = f"{vk}_{pi}" if g4 < NPAGES // 4 else "Vx"
                    for c4 in range(4):
                        last = (s_ == NS - 1 and g4 == NPAGES // 4 and c4 == 3)
                        o0 = (s_ * 4 + c4) * H
                        mm(o_ps[:, o0:o0 + H], src[:, c4 * 128:(c4 + 1) * 128], Sc[:, s_, pg, :], start=first, stop=last,
                           r=[sk, "ScP"], w=["os_ps"])
                        first = False
        ov = o_ps[:, 0:NS * 4 * H].rearrange("p (s c h) -> p s c h", s=NS, c=4)
        rv = rlb[:].rearrange("p (s h) -> p s h", h=H)
        for pr in range(4):
            for hh in range(2):
                rws = slice(hh * 64, (hh + 1) * 64)
                E("vector", "tensor_tensor", attnT[rws, pr, SEQ:SEQ + NS], ov[rws, :, pr, 2 * pr + hh], rv[rws, :, 2 * pr + hh],
                  ALU.mult, r=["os_ps", "rlb", "attnT_s"], w=["attnT_s"])
        S.barrier()
        S.emit()

    if stage >= 5:
      with contextlib.ExitStack() as p3:
        w_ao_d = k.din("w_ao", [AW, D]); w_ga_d = k.din("w_glu_a", [AW, D]); w_gb_d = k.din("w_glu_b", [AW, D])
        w_out_d = k.din("w_out", [D, D])
        w_ao = sb("w_aos", [128, 4, D], BF16, p3); w_ga = sb("w_gla", [128, 4, D], BF16, p3); w_gb = sb("w_glb", [128, 4, D], BF16, p3)
        w_gta = sb("w_gta", [128, 8, D], BF16, p3); w_gts = sb("w_gts", [128, 8, D], BF16, p3)
        w_o = sb("w_o", [128, 8, D], BF16, p3)
        for (dst, src, nm) in ((w_ao, w_ao_d, "w_ao"), (w_ga, w_ga_d, "w_gla"), (w_gb, w_gb_d, "w_glb")):
            sv = src.rearrange("(c p) n -> p c n", p=128)
            for c in range(4):
                dma(dst[:, c, :], sv[:, c, :], writes=[nm], queue="gpsimd", merge=True)
        w_out_c = w_out_d.rearrange("(c p) n -> p c n", p=128)
        for c in range(8):
            dma(w_gta[:, c, :], w_in_c[:, c, 2056:3080], writes=["w_gta"], queue="gpsimd", merge=True)
            dma(w_gts[:, c, :], w_in_c[:, c, 3080:4104], writes=["w_gts"], queue="gpsimd", merge=True)
            dma(w_o[:, c, :], w_out_c[:, c, :], writes=["w_o"], queue="gpsimd", merge=True)
        mT = sb("mT", [128, 8, 512], BF16, p3)
        sg = [sb(f"sg{i}", [128, 512], F32, p3) for i in range(3)]
        tm = [sb(f"tm{i}", [128, 512], F32, p3) for i in range(2)]
        xr = [sb(f"xr{i}", [128, D], F32, p3) for i in range(2)]
        h2t = [sb(f"h2t{i}", [128, D], F32, p3) for i in range(2)]
        E("vector", "memset", xr[0][:], 0.0, w=["xr0"])
        E("vector", "memset", xr[1][:], 0.0, w=["xr1"])
        b_ps = [ps(f"b_ps{i}", [128, 512], F32, p3) for i in range(5)]
        h_ps = ps("h_ps", [128, 1024], F32, p3)
        ntile = 0
        for nb in (4, 0, 1, 2, 3):
            blk = slice(nb * 512, min((nb + 1) * 512, TOK))
            n = blk.stop - blk.start
            for oc in range(8):
                ocs = slice(oc * 128, (oc + 1) * 128)
                for (pi, wt, src, nk, wk, rk) in ((0, w_ao, attnT, 4, "w_ao", "attnT"), (1, w_ga, zsT, 4, "w_gla", "zsT"),
                                                  (2, w_gb, zsT, 4, "w_glb", "zsT"), (3, w_gta, hnT, 8, "w_gta", "hnT"),
                                                  (4, w_gts, hnT, 8, "w_gts", "hnT")):
                    for c in range(nk):
                        mm(b_ps[pi][:, 0:n], wt[:, c, ocs], src[:, c, blk], start=(c == 0), stop=(c == nk - 1),
                           r=[wk, rk], w=[f"b_ps{pi}"])
                act(sg[0][:, 0:n], b_ps[3][:, 0:n], ACT.Sigmoid, r=["b_ps3"], w=["sg0"])
                act(sg[1][:, 0:n], b_ps[4][:, 0:n], ACT.Sigmoid, r=["b_ps4"], w=["sg1"])
                act(sg[2][:, 0:n], b_ps[2][:, 0:n], ACT.Sigmoid, r=["b_ps2"], w=["sg2"])
                E("vector", "tensor_tensor", tm[0][:, 0:n], b_ps[1][:, 0:n], sg[2][:, 0:n], ALU.mult,
                  r=["b_ps1", "sg2"], w=["tm0"])
                E("vector", "tensor_tensor", tm[0][:, 0:n], tm[0][:, 0:n], sg[1][:, 0:n], ALU.mult, r=["tm0", "sg1"], w=["tm0"])
                E("vector", "tensor_tensor", tm[1][:, 0:n], b_ps[0][:, 0:n], sg[0][:, 0:n], ALU.mult,
                  r=["b_ps0", "sg0"], w=["tm1"])
                E("vector", "tensor_tensor", mT[:, oc, 0:n], tm[0][:, 0:n], tm[1][:, 0:n], ALU.add,
                  r=["tm0", "tm1"], w=[f"mT{oc}"])
            for ti in range(n // 128):
                tt = nb * 4 + ti
                b = ntile % 2
                ntile += 1
                if tt == 16:
                    dma(xr[b][0:NS, :], x_s[:, :], writes=[f"xr{b}"])
                else:
                    dma(xr[b][:], x_p[tt * 128:(tt + 1) * 128, :], writes=[f"xr{b}"])
                for half in range(2):
                    for c in range(8):
                        mm(h_ps[:, half * 512:(half + 1) * 512], mT[:, c, ti * 128:(ti + 1) * 128],
                           w_o[:, c, half * 512:(half + 1) * 512], start=(c == 0), stop=(c == 7),
                           r=[f"mT{c}", "w_o"], w=["h_ps"])
                E("vector", "tensor_tensor", h2t[b][:], h_ps[:], xr[b][:], ALU.add, r=["h_ps", f"xr{b}"], w=[f"h2t{b}"])
                tk = dma(h2_d[tt * 128:(tt + 1) * 128, :], h2t[b][:], reads=[f"h2t{b}"], writes=[f"h2d{tt}"])
                if dbg and dbg[0] == "h2":
                    S.mark_output(dma(dbg_t[tt * 128:(tt + 1) * 128, :], h2t[b][:], reads=[f"h2t{b}"]))
        S.barrier()
        S.emit()

    pA.close()
    pG.close()
    if stage >= 6:
      with contextlib.ExitStack() as p4:
        wq_d = k.din("peer_w_q", [D, 2048]); keys_d = k.din("peer_keys", [16, 128, 128])
        gffn_d = k.din("g_ffn", [1, D]); gple_d = k.din("g_ple", [1, D]); gfin_d = k.din("g_final", [1, D])
        wple_d = k.din("w_ple", [256, D]); wpg_d = k.din("w_ple_gate", [D, D])
        pp_d = k.din("p_p", [SEQ, 256]); ps_d = k.din("p_s", [NS, 256])
        y_p = k.dout("y_p", [SEQ, D]); y_s = k.dout("y_s", [NS, D])

        w_q = sb("w_q", [128, 8, 2048], BF16, p4)
        wq_c = wq_d.rearrange("(c p) n -> p c n", p=128)
        for c in range(8):
            dma(w_q[:, c, :], wq_c[:, c, :], writes=["w_q"], queue="gpsimd", merge=True)
        w_pg = sb("w_pg", [128, 8, D], BF16, p4)
        wpg_c = wpg_d.rearrange("(c p) n -> p c n", p=128)
        for c in range(8):
            dma(w_pg[:, c, :], wpg_c[:, c, :], writes=["w_pg"], queue="gpsimd", merge=True)
        w_pl = sb("w_pl", [128, 2, D], BF16, p4)
        wpl_c = wple_d.rearrange("(c p) n -> p c n", p=128)
        for c in range(2):
            dma(w_pl[:, c, :], wpl_c[:, c, :], writes=["w_pl"], queue="gpsimd", merge=True)
        gv = sb("gv", [128, 3, D], F32, p4)
        for i, gd in enumerate((gffn_d, gple_d, gfin_d)):
            dma(gv[:, i, :], gd.partition_broadcast(128), writes=["gv"], merge=True)
        io16 = sb("io16", [128, 16], F32, p4)
        E("gpsimd", "iota", io16[:], pattern=[[1, 16]], base=0, channel_multiplier=0,
          allow_small_or_imprecise_dtypes=True, w=["io16"])
        keysT = sb("keysT", [128, 16, 128], BF16, p4)
        A_ps = ps("A_ps", [128, 1024], BF16, p4)
        B_ps = ps("B_ps", [128, 512], F32, p4)
        C_ps = ps("C_ps", [128, 2048], F32, p4)
        D_ps = ps("D_ps", [128, 1024], F32, p4)
        with contextlib.ExitStack() as pk:
            kn = sb("kn", [128, 16, 128], BF16, pk)
            dma(kn[:], keys_d.rearrange("a k d -> k a d"), writes=["kn"], queue="gpsimd")
            for half in range(2):
                for a in range(8):
                    E("tensor", "transpose", A_ps[:, a * 128:(a + 1) * 128], kn[:, half * 8 + a, :], ident_bf[:],
                      r=["kn", "ident_bf"], w=["A_ps"])
                E("vector", "tensor_copy", keysT[:, half * 8:half * 8 + 8, :].rearrange("p a k -> p (a k)"), A_ps[:],
                  r=["A_ps"], w=["keysT"])
            S.barrier()
            S.emit()

        NU = 4
        uvb = [sb(f"uvb{i}", [128, 4, 2 * D], BF16, p4) for i in range(NU)]
        h2t = sb("h2t4", [128, D], F32, p4)
        hn2f = sb("hn2f", [128, D], F32, p4)
        hnb = sb("hnb", [128, D], BF16, p4)
        jk = sb("jk4", [128, D], BF16, p4)
        hT = sb("hT4", [128, 8, 128], BF16, p4)
        qpT = sb("qpT", [128, 16, 128], BF16, p4)
        S1 = sb("S1", [128, 4096], F32, p4)
        S2 = sb("S2", [128, 2048], F32, p4)
        vals = sb("vals", [128, 16, 16], F32, p4)
        ixu = sb("ixu", [128, 16, 16], U32, p4)
        ixf = sb("ixf", [128, 16, 16], F32, p4)
        tops = sb("tops", [128, 8, 16], F32, p4)
        posu = sb("posu", [128, 8, 16], U32, p4)
        abi = sb("abi", [128, 2, 8, 16], U32, p4)
        abf = sb("abf", [128, 2, 8, 16], F32, p4)
        ijf = sb("ijf", [128, 2, 8, 16], F32, p4)
        eidf = sb("eidf", [128, 128], F32, p4)
        eidx = sb("eidx", [128, 128], I32, p4)
        gat = sb("gat", [128, 8, 16], F32, p4)
        gsm = sb("gsm", [128, 8, 2], F32, p4)
        scr = sb("scr", [128, 128], F32, p4)
        wsl = sb("wsl", [128, 128], F32, p4)
        gt = [sb(f"gt{i}", [128, 128], F32, p4) for i in range(2)]
        dg4 = [sb(f"dg4_{i}", [128, 128], BF16, p4) for i in range(4)]
        g5 = [sb(f"g5_{i}", [128, 2, 8], F32, p4) for i in range(2)]
        st4 = sb("st4", [128, 3, 4], F32, p4)
        h3 = sb("h3", [128, D], F32, p4)
        gate = sb("gate", [128, D], F32, p4)
        pt_f = sb("pt_f", [128, 256], F32, p4)
        pt_b = sb("pt_b", [128, 256], BF16, p4)
        pT4 = sb("pT4", [128, 2, 128], BF16, p4)
        yo = sb("yo", [128, D], F32, p4)
        E("vector", "memset", pt_f[:], 0.0, w=["pt_f"])
        NEG = -1.0e30

        def rms(src, src_keys, gi, out_f, out_b, tag):
            st = st4[:, gi, :]
            act(jk[:], src, ACT.Square, accum_out=st[:, 0:1], r=src_keys, w=["jk4", f"st4{gi}"])
            E("vector", "tensor_scalar", st[:, 1:2], st[:, 0:1], 1.0 / D, EPS, ALU.mult, ALU.add, r=[f"st4{gi}"], w=[f"st4{gi}"])
            act(st[:, 2:3], st[:, 1:2], ACT.Sqrt, r=[f"st4{gi}"], w=[f"st4{gi}"])
            E("vector", "reciprocal", st[:, 3:4], st[:, 2:3], r=[f"st4{gi}"], w=[f"st4{gi}"])
            if out_f is not None:
                E("vector", "scalar_tensor_tensor", out_f[0], src, st[:, 3:4], gv[:, gi, :], ALU.mult, ALU.mult,
                  r=src_keys + [f"st4{gi}", "gv"], w=[out_f[1]])
            if out_b is not None:
                if out_f is not None:
                    E("gpsimd", "tensor_copy", out_b[0], out_f[0], r=[out_f[1]], w=[out_b[1]])
                else:
                    E("vector", "scalar_tensor_tensor", out_b[0], src, st[:, 3:4], gv[:, gi, :], ALU.mult, ALU.mult,
                      r=src_keys + [f"st4{gi}", "gv"], w=[out_b[1]])

        order = [16] + list(range(16))
        if stage == 6:
            order = [16, 0]
        for tt in order:
            rows = slice(tt * 128, (tt + 1) * 128)
            dma(h2t[:], h2_d[rows, :], reads=[f"h2d{tt}"], writes=["h2t4"])
            rms(h2t[:], ["h2t4"], 0, (hn2f[:], "hn2f"), (hnb[:], "hnb"), "a")
            for c in range(8):
                E("tensor", "transpose", A_ps[:, c * 128:(c + 1) * 128], hnb[:, c * 128:(c + 1) * 128], ident_bf[:],
                  r=["hnb", "ident_bf"], w=["A_ps"])
            act(hT[:].rearrange("p c n -> p (c n)"), A_ps[:], ACT.Copy, r=["A_ps"], w=["hT4"])
            for g4 in range(4):
                for a in range(4):
                    hp = g4 * 4 + a
                    for c in range(8):
                        mm(B_ps[:, a * 128:(a + 1) * 128], w_q[:, c, hp * 128:(hp + 1) * 128], hT[:, c, :],
                           start=(c == 0), stop=(c == 7), r=["w_q", "hT4"], w=["B_ps"])
                if g4 % 2 == 0:
                    act(qpT[:, g4 * 4:g4 * 4 + 4, :].rearrange("p a n -> p (a n)"), B_ps[:], ACT.Copy, r=["B_ps"], w=["qpT"])
                else:
                    E("vector", "tensor_copy", qpT[:, g4 * 4:g4 * 4 + 4, :].rearrange("p a n -> p (a n)"), B_ps[:],
                      r=["B_ps"], w=["qpT"])
            for hp in range(16):
                mm(C_ps[:, hp * 128:(hp + 1) * 128], qpT[:, hp, :], keysT[:, hp, :], r=["qpT", "keysT"], w=["C_ps"])
            sc = S1[:, 0:2048]; sc2 = S1[:, 2048:4096]
            act(sc, C_ps[:], ACT.Copy, r=["C_ps"], w=["S1a"])
            for hp in range(16):
                s_ = sc[:, hp * 128:(hp + 1) * 128]; s2_ = sc2[:, hp * 128:(hp + 1) * 128]
                E("vector", "max", vals[:, hp, 0:8], s_, r=["S1a"], w=["vals"])
                E("vector", "match_replace", s2_, vals[:, hp, 0:8], s_, NEG, r=["S1a", "vals"], w=["S1b"])
                E("vector", "max", vals[:, hp, 8:16], s2_, r=["S1b"], w=["vals"])
                E("vector", "max_index", ixu[:, hp, 0:8], vals[:, hp, 0:8], s_, r=["S1a", "vals"], w=["ixu"])
                E("vector", "max_index", ixu[:, hp, 8:16], vals[:, hp, 8:16], s2_, r=["S1b", "vals"], w=["ixu"])
            E("vector", "tensor_copy", ixf[:], ixu[:], r=["ixu"], w=["ixf"])
            v4 = vals[:].rearrange("p (h q) k -> p h q k", q=2)
            cand = S2[:].rearrange("p (h a b) -> p h a b", h=8, a=16)
            E("vector", "tensor_tensor", cand, v4[:, :, 0, :].unsqueeze(3).to_broadcast([128, 8, 16, 16]),
              v4[:, :, 1, :].unsqueeze(2).to_broadcast([128, 8, 16, 16]), ALU.add, r=["vals"], w=["S2"])
            c2 = S1[:, 0:2048]
            for h in range(8):
                c_ = S2[:, h * 256:(h + 1) * 256]; c2_ = c2[:, h * 256:(h + 1) * 256]
                E("vector", "max", tops[:, h, 0:8], c_, r=["S2"], w=["tops"])
                E("vector", "match_replace", c2_, tops[:, h, 0:8], c_, NEG, r=["S2", "tops", "S1a"], w=["S1a"])
                E("vector", "max", tops[:, h, 8:16], c2_, r=["S1a"], w=["tops"])
                E("vector", "max_index", posu[:, h, 0:8], tops[:, h, 0:8], c_, r=["S2", "tops"], w=["posu"])
                E("vector", "max_index", posu[:, h, 8:16], tops[:, h, 8:16], c2_, r=["S1a", "tops"], w=["posu"])
            E("vector", "tensor_single_scalar", abi[:, 0], posu[:], 4, ALU.logical_shift_right, r=["posu"], w=["abi"])
            E("vector", "tensor_single_scalar", abi[:, 1], posu[:], 15, ALU.bitwise_and, r=["posu"], w=["abi"])
            E("vector", "tensor_copy", abf[:], abi[:], r=["abi"], w=["abf"])
            eq = S1[:, 2048:4096].rearrange("p (h k a) -> p h k a", h=8, k=16)
            ix4 = ixf[:].rearrange("p (h q) k -> p h q k", q=2)
            io_b = io16[:].unsqueeze(1).unsqueeze(1).to_broadcast([128, 8, 16, 16])
            for w_ in range(2):
                E("vector", "tensor_tensor", eq, abf[:, w_].unsqueeze(3).to_broadcast([128, 8, 16, 16]), io_b, ALU.is_equal,
                  r=["abf", "io16", "S1b"], w=["S1b"])
                E("vector", "tensor_tensor", eq, eq, ix4[:, :, w_, :].unsqueeze(2).to_broadcast([128, 8, 16, 16]), ALU.mult,
                  r=["S1b", "ixf"], w=["S1b"])
                E("vector", "tensor_reduce", ijf[:, w_], eq, AX.X, ALU.add, r=["S1b"], w=["ijf"])
            E("vector", "scalar_tensor_tensor", eidf[:].rearrange("p (h k) -> p h k", h=8), ijf[:, 0], 128.0, ijf[:, 1],
              ALU.mult, ALU.add, r=["ijf"], w=["eidf"])
            E("vector", "tensor_copy", eidx[:], eidf[:], r=["eidf"], w=["eidx"])
            E("vector", "tensor_tensor", gat[:], tops[:], tops[:, :, 0:1].to_broadcast([128, 8, 16]), ALU.subtract,
              r=["tops"], w=["gat"])
            act(gat[:], gat[:], ACT.Exp, r=["gat"], w=["gat"])
            E("vector", "tensor_reduce", gsm[:, :, 0], gat[:], AX.X, ALU.add, r=["gat"], w=["gsm"])
            E("vector", "reciprocal", gsm[:, :, 1], gsm[:, :, 0], r=["gsm"], w=["gsm"])
            E("vector", "tensor_tensor", gat[:], gat[:], gsm[:, :, 1:2].to_broadcast([128, 8, 16]), ALU.mult,
              r=["gat", "gsm"], w=["gat"])
            gatf = gat[:].rearrange("p h k -> p (h k)")

            def gather_score(grp):
                bi = grp % NU
                buf, bk = uvb[bi], f"uvb{bi}"
                for i in range(4):
                    sl = grp * 4 + i
                    dma(buf[:, i, :], uv_d[:, :], reads=["eidx", "uv_d"], writes=[f"{bk}_{i}"], queue="gpsimd",
                        gather=bass.IndirectOffsetOnAxis(ap=eidx[:, sl:sl + 1], axis=0))
                for i in range(4):
                    sl = grp * 4 + i
                    E("vector", "scalar_tensor_tensor", jk[:], buf[:, i, 0:D], 1.0, hn2f[:], ALU.mult, ALU.mult,
                      accum_out=scr[:, sl:sl + 1], r=[f"{bk}_{i}", "hn2f"], w=["jk4", f"scr{grp}"])

            def accumulate(grp, wk):
                bi = grp % NU
                buf, bk = uvb[bi], f"uvb{bi}"
                for i in range(4):
                    sl = grp * 4 + i
                    d3 = sl % 4
                    act(dg4[d3][:], ident_f[:], ACT.Copy, scale=wsl[:, sl:sl + 1], r=["ident_f", wk], w=[f"dg4_{d3}"])
                    for half in range(2):
                        mm(D_ps[:, half * 512:(half + 1) * 512], dg4[d3][:], buf[:, i, D + half * 512:D + (half + 1) * 512],
                           start=(sl == 0), stop=(sl == 127), r=[f"dg4_{d3}", f"{bk}_{i}"], w=["D_ps"])

            for g2 in range(16):
                ga_, gk = g5[g2 % 2], f"g5_{g2 % 2}"
                gather_score(2 * g2)
                gather_score(2 * g2 + 1)
                sls = slice(g2 * 8, g2 * 8 + 8)
                sk_ = [f"scr{2 * g2}", f"scr{2 * g2 + 1}"]
                act(ga_[:, 0, :], scr[:, sls], ACT.Square, r=sk_, w=[gk])
                E("vector", "tensor_scalar", ga_[:, 0, :], ga_[:, 0, :], 0.044715, 1.0, ALU.mult, ALU.add, r=[gk], w=[gk])
                E("vector", "tensor_tensor", ga_[:, 0, :], ga_[:, 0, :], scr[:, sls], ALU.mult, r=[gk] + sk_, w=[gk])
                act(ga_[:, 1, :], ga_[:, 0, :], ACT.Sigmoid, scale=1.5957691216057308, r=[gk], w=[gk])
                E("vector", "tensor_tensor", ga_[:, 1, :], ga_[:, 1, :], scr[:, sls], ALU.mult, r=[gk] + sk_, w=[gk])
                E("vector", "tensor_tensor", wsl[:, sls], ga_[:, 1, :], gatf[:, sls], ALU.mult, r=[gk, "gat"], w=[f"wsl{g2}"])
                accumulate(2 * g2, f"wsl{g2}")
                accumulate(2 * g2 + 1, f"wsl{g2}")
            E("vector", "tensor_tensor", h3[:], D_ps[:], h2t[:], ALU.add, r=["D_ps", "h2t4"], w=["h3"])
            if dbg and dbg[0] == "h3":
                S.mark_output(dma(dbg_t[rows, :], h3[:], reads=["h3"]))
            rms(h3[:], ["h3"], 1, None, (hnb[:], "hnb"), "b")
            for c in range(8):
                E("tensor", "transpose", A_ps[:, c * 128:(c + 1) * 128], hnb[:, c * 128:(c + 1) * 128], ident_bf[:],
                  r=["hnb", "ident_bf"], w=["A_ps"])
            act(hT[:].rearrange("p c n -> p (c n)"), A_ps[:], ACT.Copy, r=["A_ps"], w=["hT4"])
            for half in range(2):
                for c in range(8):
                    mm(C_ps[:, half * 512:(half + 1) * 512], hT[:, c, :], w_pg[:, c, half * 512:(half + 1) * 512],
                       start=(c == 0), stop=(c == 7), r=["hT4", "w_pg"], w=["C_ps"])
            act(gate[:], C_ps[:, 0:1024], ACT.Sigmoid, r=["C_ps"], w=["gate"])
            if tt == 16:
                dma(pt_f[0:NS, :], ps_d[:, :], writes=["pt_f"])
            else:
                dma(pt_f[:], pp_d[rows, :], writes=["pt_f"])
            E("vector", "tensor_copy", pt_b[:], pt_f[:], r=["pt_f"], w=["pt_b"])
            for c in range(2):
                E("tensor", "transpose", A_ps[:, c * 128:(c + 1) * 128], pt_b[:, c * 128:(c + 1) * 128], ident_bf[:],
                  r=["pt_b", "ident_bf"], w=["A_ps"])
            E("vector", "tensor_copy", pT4[:].rearrange("p c n -> p (c n)"), A_ps[:, 0:256], r=["A_ps"], w=["pT4"])
            for half in range(2):
                for c in range(2):
                    mm(C_ps[:, 1024 + half * 512:1024 + (half + 1) * 512], pT4[:, c, :], w_pl[:, c, half * 512:(half + 1) * 512],
                       start=(c == 0), stop=(c == 1), r=["pT4", "w_pl"], w=["C_ps2"])
            E("vector", "tensor_tensor", gate[:], C_ps[:, 1024:2048], gate[:], ALU.mult, r=["C_ps2", "gate"], w=["gate"])
            E("vector", "tensor_tensor", h3[:], h3[:], gate[:], ALU.add, r=["h3", "gate"], w=["h3"])
            rms(h3[:], ["h3"], 2, (yo[:], "yo"), None, "c")
            if tt == 16:
                S.mark_output(dma(y_s[:, :], yo[0:NS, :], reads=["yo"]))
            else:
                S.mark_output(dma(y_p[rows, :], yo[:], reads=["yo"]))
        S.barrier()
        S.emit()

    S.barrier()
    S.emit(final=True)

    k.stack.close()
    return k


def make_in_maps(inputs):
    maps = []
    for c in range(N_CORES):
        m = {
            "x_p": np.ascontiguousarray(inputs["x_prompt"][c]),
            "x_s": np.ascontiguousarray(inputs["x_sample"][4 * c:4 * c + 4, 0]),
            "w_in": np.ascontiguousarray(inputs["w_in"][0]),
            "b_forget": np.ascontiguousarray(inputs["b_forget"]),
            "g_mix": np.ascontiguousarray(inputs["g_mix"]),
            "lam_re": np.ascontiguousarray(inputs["ssm_lam_re"][0]),
            "lam_im": np.ascontiguousarray(inputs["ssm_lam_im"][0]),
            "log_dt": np.ascontiguousarray(inputs["ssm_log_dt"]),
            "b_re": np.ascontiguousarray(inputs["ssm_b_re"][0]),
            "b_im": np.ascontiguousarray(inputs["ssm_b_im"][0]),
            "c_re": np.ascontiguousarray(inputs["ssm_c_re"][0].reshape(512, 64)),
            "c_im": np.ascontiguousarray(inputs["ssm_c_im"][0].reshape(512, 64)),
            "ssm_d": np.ascontiguousarray(inputs["ssm_d"].reshape(512, 1)),
            "st_re": np.ascontiguousarray(inputs["state_ssm_re"][4 * c:4 * c + 4, 0].reshape(128, 64)),
            "st_im": np.ascontiguousarray(inputs["state_ssm_im"][4 * c:4 * c + 4, 0].reshape(128, 64)),
            "w_ao": np.ascontiguousarray(inputs["w_attn_out"][0]),
            "w_glu_a": np.ascontiguousarray(inputs["w_glu_a"][0]),
            "w_glu_b": np.ascontiguousarray(inputs["w_glu_b"][0]),
            "w_out": np.ascontiguousarray(inputs["w_out"][0]),
            "peer_w_q": np.ascontiguousarray(inputs["peer_w_q"][0]),
            "peer_keys": np.ascontiguousarray(inputs["peer_keys"][0].reshape(16, 128, 128)),
            "peer_u": inputs["peer_u"][0],
            "peer_v": inputs["peer_v"][0],
            "g_ffn": np.ascontiguousarray(inputs["g_ffn"]),
            "g_ple": np.ascontiguousarray(inputs["g_ple"]),
            "g_final": np.ascontiguousarray(inputs["g_final"].reshape(1, D)),
            "w_ple": np.ascontiguousarray(inputs["w_ple"][0]),
            "w_ple_gate": np.ascontiguousarray(inputs["w_ple_gate"][0]),
            "p_p": np.ascontiguousarray(inputs["p_prompt"][0, c]),
            "p_s": np.ascontiguousarray(inputs["p_sample"][0, 4 * c:4 * c + 4, 0]),
            "cache_k": inputs["cache_k"].reshape(NPHYS * 128, AW),
            "cache_v": inputs["cache_v"].reshape(NPHYS * 128, AW),
            "cache_logf": inputs["cache_logf"].reshape(NPHYS, 128 * H),
            "page_table": np.ascontiguousarray(inputs["page_table"][4 * c:4 * c + 4].reshape(1, NS * NPAGES)),
        }
        maps.append(m)
    return maps


def run(inputs, stage=99, trace=False, dbg=None):
    k = build(stage, dbg)
    names = set(k.io.keys())
    maps = [{n: v for n, v in m.items() if n in names} for m in make_in_maps(inputs)]
    res = run_bass_kernel_spmd(k.nc, maps, core_ids=list(range(N_CORES)), trace=trace)
    return res


def kernel(**inputs):
    inputs = {n: np.asarray(v) for n, v in inputs.items()}
    res = run(inputs).results
    B, DB = 8, 32
    f32 = np.float32

    def cat(name, shape):
        return np.stack([np.asarray(r[name]) for r in res], 0).reshape(shape).astype(f32, copy=False)

    return (cat("y_p", (B, SEQ, D)), cat("y_s", (DB, 1, D)),
            cat("k_p", (B, 1, SEQ, H, HD)), cat("v_p", (B, 1, SEQ, H, HD)), cat("lf_p", (B, 1, SEQ, H)),
            cat("hr_p", (B, 1, 32, 64)), cat("hi_p", (B, 1, 32, 64)),
            cat("k_s", (DB, 1, 1, H, HD)), cat("v_s", (DB, 1, 1, H, HD)), cat("lf_s", (DB, 1, 1, H)),
            cat("hr_s", (DB, 1, 32, 64)), cat("hi_s", (DB, 1, 32, 64)))
```
